# Optimizing a Trainium2 kernel written in Bass

```python
import math
import jax, jax.numpy as jnp
from jax import lax
import numpy as np

D_MODEL = 2048
BATCH = 4
SEQ = 4096
DEPTH = 1

N_Q_HEADS = 16
N_KV_HEADS = 4
HEAD_DIM = 64
WINDOW = 128
ATTN_BLOCK = 128
ATTN_WIDTH = N_Q_HEADS * HEAD_DIM
KV_WIDTH = N_KV_HEADS * HEAD_DIM
REL_BUCKETS = 32
REL_MAX_DIST = 128
HG_HEADS = 8
HG_KEY_DIM = 128
HG_VAL_DIM = 128
HG_KEY_WIDTH = HG_HEADS * HG_KEY_DIM
HG_VAL_WIDTH = HG_HEADS * HG_VAL_DIM
HG_CHUNK = 64
N_GROUPS = 4
EXPERTS_PER_GROUP = 8
N_EXPERTS = N_GROUPS * EXPERTS_PER_GROUP
TOP_K = 2
EXPERT_FF = 512
MOE_BLOCK = 128
RMS_EPS = 1e-6
IN_SECTIONS = (ATTN_WIDTH, KV_WIDTH, KV_WIDTH, HG_KEY_WIDTH, HG_KEY_WIDTH, HG_VAL_WIDTH, HG_VAL_WIDTH, D_MODEL, D_MODEL)
IN_WIDTH = ATTN_WIDTH + 2 * KV_WIDTH + 2 * HG_KEY_WIDTH + 2 * HG_VAL_WIDTH + 2 * D_MODEL

kernel_name = 'hybrid_swa_sink_hgrn2_hiermoe'


def rms_norm(x, g):
    xf = x.astype(jnp.float32)
    y = xf * lax.rsqrt(jnp.mean(xf * xf, axis=-1, keepdims=True) + RMS_EPS)
    return (y * g.astype(jnp.float32)).astype(x.dtype)


def t5_causal_bucket(n):
    max_exact = REL_BUCKETS // 2
    nf = jnp.maximum(n, 1).astype(jnp.float32)
    large = max_exact + (jnp.log(nf / max_exact) / math.log(REL_MAX_DIST / max_exact)
                         * (REL_BUCKETS - max_exact)).astype(jnp.int32)
    large = jnp.minimum(large, REL_BUCKETS - 1)
    return jnp.where(n < max_exact, n, large)


def sliding_window_sink_attention(q, k, v, sinks, rel_table):
    bsz, s_len, _ = q.shape
    nb = s_len // ATTN_BLOCK
    grp = N_Q_HEADS // N_KV_HEADS
    q = q.reshape(bsz, nb, ATTN_BLOCK, N_KV_HEADS, grp, HEAD_DIM)
    k = k.reshape(bsz, nb, ATTN_BLOCK, N_KV_HEADS, HEAD_DIM)
    v = v.reshape(bsz, nb, ATTN_BLOCK, N_KV_HEADS, HEAD_DIM)
    pad_k = jnp.zeros_like(k[:, :1])
    kk = jnp.concatenate([jnp.concatenate([pad_k, k[:, :-1]], axis=1), k], axis=2)
    vv = jnp.concatenate([jnp.concatenate([pad_k, v[:, :-1]], axis=1), v], axis=2)
    s = jnp.einsum('bnqhgd,bnkhd->bnhgqk', q, kk).astype(jnp.float32) * (HEAD_DIM ** -0.5)
    qi = jnp.arange(ATTN_BLOCK)[:, None]
    kj = jnp.arange(2 * ATTN_BLOCK)[None, :]
    dist = qi + ATTN_BLOCK - kj
    bias = rel_table[t5_causal_bucket(jnp.clip(dist, 0))]
    bias = bias.transpose(2, 0, 1).reshape(N_KV_HEADS, grp, ATTN_BLOCK, 2 * ATTN_BLOCK).astype(jnp.float32)
    valid = (dist >= 0) & (dist < WINDOW)
    in_range = (jnp.arange(nb)[:, None, None] > 0) | (kj >= ATTN_BLOCK)[None]
    valid = valid[None] & in_range
    s = jnp.where(valid[None, :, None, None], s + bias, -jnp.inf)
    sink = sinks.astype(jnp.float32).reshape(N_KV_HEADS, grp, 1, 1)
    m = jnp.maximum(jnp.max(s, axis=-1, keepdims=True), sink)
    p = jnp.exp(s - m)
    denom = jnp.sum(p, axis=-1, keepdims=True) + jnp.exp(sink - m)
    o = jnp.einsum('bnhgqk,bnkhd->bnqhgd', (p / denom).astype(vv.dtype), vv)
    return o.reshape(bsz, s_len, ATTN_WIDTH)


def hgrn2(q, f_raw, i_in, g_out, lb, gn):
    bsz, s_len, _ = q.shape
    nc = s_len // HG_CHUNK
    f = lb + (1.0 - lb) * jax.nn.sigmoid(f_raw.astype(jnp.float32))
    logf = jnp.log(f)
    kf = 1.0 - f

    def heads(t, d):
        return t.astype(jnp.float32).reshape(bsz, nc, HG_CHUNK, HG_HEADS, d).transpose(1, 0, 3, 2, 4)

    qc, kc, gc = heads(q, HG_KEY_DIM), heads(kf, HG_KEY_DIM), heads(logf, HG_KEY_DIM)
    vc = heads(i_in, HG_VAL_DIM)
    tri = jnp.tril(jnp.ones((HG_CHUNK, HG_CHUNK), dtype=bool))

    def step(state, inp):
        qt, kt, vt, gt = inp
        b = jnp.cumsum(gt, axis=2)
        o_inter = jnp.einsum('bhtk,bhkv->bhtv', qt * jnp.exp(b), state)
        decay = jnp.exp(jnp.where(tri[None, None, :, :, None], b[:, :, :, None, :] - b[:, :, None, :, :], -jnp.inf))
        att = jnp.einsum('bhtk,bhsk,bhtsk->bhts', qt, kt, decay)
        o = o_inter + jnp.einsum('bhts,bhsv->bhtv', att, vt)
        b_last = b[:, :, -1]
        state = jnp.exp(b_last)[..., None] * state + jnp.einsum('bhsk,bhsv->bhkv', kt * jnp.exp(b_last[:, :, None] - b), vt)
        return state, o

    s0 = jnp.zeros((bsz, HG_HEADS, HG_KEY_DIM, HG_VAL_DIM), jnp.float32)
    _, o = lax.scan(step, s0, (qc, kc, vc, gc))
    o = o.transpose(1, 0, 3, 2, 4).reshape(bsz, s_len, HG_HEADS, HG_VAL_DIM)
    o = o * lax.rsqrt(jnp.mean(o * o, axis=-1, keepdims=True) + RMS_EPS) * gn.astype(jnp.float32)
    o = o * jax.nn.silu(g_out.astype(jnp.float32).reshape(bsz, s_len, HG_HEADS, HG_VAL_DIM))
    return o.reshape(bsz, s_len, HG_VAL_WIDTH).astype(q.dtype)


def hier_moe(h, w_gr, b_gr, w_er, b_er, w_gate, w_up, w_down):
    bsz, s_len, d = h.shape
    n_tok = bsz * s_len
    t = h.reshape(n_tok, d)
    gl = (t @ w_gr).astype(jnp.float32) + b_gr.astype(jnp.float32)
    gp = jax.nn.softmax(gl, axis=-1)
    g_idx = jnp.argmax(gl, axis=-1)
    g_w = jnp.take_along_axis(gp, g_idx[:, None], axis=1)[:, 0]
    el = ((t @ w_er).astype(jnp.float32) + b_er.astype(jnp.float32)).reshape(n_tok, N_GROUPS, EXPERTS_PER_GROUP)
    el_sel = jnp.take_along_axis(el, g_idx[:, None, None], axis=1)[:, 0]
    ep = jax.nn.softmax(el_sel, axis=-1)
    top_p, top_i = lax.top_k(ep, TOP_K)
    wts = g_w[:, None] * top_p / jnp.sum(top_p, axis=-1, keepdims=True)
    e_id = (g_idx[:, None] * EXPERTS_PER_GROUP + top_i).astype(jnp.int32)
    n_asg = n_tok * TOP_K
    e_flat = e_id.reshape(n_asg)
    w_flat = wts.reshape(n_asg)
    tok = jnp.repeat(jnp.arange(n_tok, dtype=jnp.int32), TOP_K)
    order = jnp.argsort(e_flat)
    e_s, tok_s, w_s = e_flat[order], tok[order], w_flat[order]
    counts = jnp.bincount(e_flat, length=N_EXPERTS)
    padded = (counts + MOE_BLOCK - 1) // MOE_BLOCK * MOE_BLOCK
    starts = jnp.cumsum(counts) - counts
    pends = jnp.cumsum(padded)
    pstarts = pends - padded
    dest = pstarts[e_s] + jnp.arange(n_asg, dtype=jnp.int32) - starts[e_s]
    n_blocks = -(-n_asg // MOE_BLOCK) + N_EXPERTS
    n_rows = n_blocks * MOE_BLOCK
    row_tok = jnp.full((n_rows,), n_tok, jnp.int32).at[dest].set(tok_s)
    row_w = jnp.zeros((n_rows,), jnp.float32).at[dest].set(w_s)
    block_e = jnp.minimum(jnp.searchsorted(pends, jnp.arange(n_blocks) * MOE_BLOCK, side='right'), N_EXPERTS - 1)
    t_pad = jnp.concatenate([t, jnp.zeros((1, d), t.dtype)], axis=0)

    def expert_block(args):
        idx, eid = args
        xb = t_pad[idx]
        hb = jax.nn.silu(xb @ w_gate[eid]) * (xb @ w_up[eid])
        return hb @ w_down[eid]

    yb = lax.map(expert_block, (row_tok.reshape(n_blocks, MOE_BLOCK), block_e))
    yb = yb.reshape(n_rows, d) * row_w[:, None].astype(yb.dtype)
    y = jax.ops.segment_sum(yb, row_tok, num_segments=n_tok + 1)[:n_tok]
    return y.reshape(bsz, s_len, d)


def setup_inputs(seed: int = 0) -> dict:
    key = jax.random.key(seed)
    ks = jax.random.split(key, 20)
    f32 = jnp.float32
    nrm = lambda k, shp, sc: jax.random.normal(k, shp, f32) * sc
    return {
        'x': nrm(ks[0], (BATCH, SEQ, D_MODEL), 1.0),
        'norm1_g': 1.0 + nrm(ks[1], (DEPTH, D_MODEL), 0.02),
        'w_in': nrm(ks[2], (DEPTH, D_MODEL, IN_WIDTH), D_MODEL ** -0.5),
        'attn_sinks': nrm(ks[3], (DEPTH, N_Q_HEADS), 0.5),
        'rel_bias': nrm(ks[4], (REL_BUCKETS, N_Q_HEADS), 0.5),
        'hg_lb_logits': nrm(ks[5], (DEPTH + 1, HG_KEY_WIDTH), 0.5),
        'hg_norm_g': 1.0 + nrm(ks[6], (DEPTH, HG_VAL_DIM), 0.02),
        'w_attn_branch': nrm(ks[7], (DEPTH, ATTN_WIDTH, D_MODEL), ATTN_WIDTH ** -0.5),
        'w_hg_branch': nrm(ks[8], (DEPTH, HG_VAL_WIDTH, D_MODEL), HG_VAL_WIDTH ** -0.5),
        'w_out': nrm(ks[9], (DEPTH, D_MODEL, D_MODEL), D_MODEL ** -0.5),
        'norm2_g': 1.0 + nrm(ks[10], (DEPTH, D_MODEL), 0.02),
        'w_group_router': nrm(ks[11], (DEPTH, D_MODEL, N_GROUPS), D_MODEL ** -0.5),
        'b_group_router': nrm(ks[12], (DEPTH, N_GROUPS), 0.01),
        'w_expert_router': nrm(ks[13], (DEPTH, D_MODEL, N_EXPERTS), D_MODEL ** -0.5),
        'b_expert_router': nrm(ks[14], (DEPTH, N_EXPERTS), 0.01),
        'w_gate': nrm(ks[15], (DEPTH, N_EXPERTS, D_MODEL, EXPERT_FF), D_MODEL ** -0.5),
        'w_up': nrm(ks[16], (DEPTH, N_EXPERTS, D_MODEL, EXPERT_FF), D_MODEL ** -0.5),
        'w_down': nrm(ks[17], (DEPTH, N_EXPERTS, EXPERT_FF, D_MODEL), EXPERT_FF ** -0.5),
        'final_g': 1.0 + nrm(ks[18], (D_MODEL,), 0.02),
    }


def reference(x, norm1_g, w_in, attn_sinks, rel_bias, hg_lb_logits, hg_norm_g, w_attn_branch, w_hg_branch,
              w_out, norm2_g, w_group_router, b_group_router, w_expert_router, b_expert_router,
              w_gate, w_up, w_down, final_g):
    lb_all = jnp.cumsum(jax.nn.softmax(hg_lb_logits.astype(jnp.float32), axis=0), axis=0)
    offsets = [int(v) for v in np.cumsum(IN_SECTIONS)[:-1]]
    for l in range(DEPTH):
        h = rms_norm(x, norm1_g[l])
        proj = h @ w_in[l]
        q_a, k_a, v_a, q_h, f_h, i_h, g_h, gate_a, gate_h = jnp.split(proj, offsets, axis=-1)
        a = sliding_window_sink_attention(q_a, k_a, v_a, attn_sinks[l], rel_bias)
        r = hgrn2(q_h, f_h, i_h, g_h, lb_all[l], hg_norm_g[l])
        mixed = jax.nn.sigmoid(gate_a) * (a @ w_attn_branch[l]) + jax.nn.sigmoid(gate_h) * (r @ w_hg_branch[l])
        x = x + mixed @ w_out[l]
        h2 = rms_norm(x, norm2_g[l])
        x = x + hier_moe(h2, w_group_router[l], b_group_router[l], w_expert_router[l], b_expert_router[l],
                         w_gate[l], w_up[l], w_down[l])
    return rms_norm(x, final_g)
```

```python
import numpy as np
from contextlib import ExitStack
import concourse.bass as bass
import concourse.mybir as mybir
from concourse.bass_utils import run_bass_kernel_spmd

F32 = mybir.dt.float32
BF16 = mybir.dt.bfloat16
I32 = mybir.dt.int32
AF = mybir.ActivationFunctionType
ALU = mybir.AluOpType
AX = mybir.AxisListType

SAME_ENG_SYNC = True
D = 2048
KC = 16
TOK = 2048
G = 512
NG = TOK // G
INW = 9728
NE = 32
CAP = 256
EPS = 1e-6
NEG = -30000.0
WPD = 2
DBG_NGP = NG
DBG_NGO = NG
DBG_NEX = NE
DBG_NT3 = 16
DBG_LIMIT = 10 ** 9
DBG_MARKS = []
NRING = 3

O_QA, O_KA, O_VA, O_QH, O_FH, O_IH, O_GH, O_GA, O_GB = 0, 1024, 1280, 1536, 2560, 3584, 4608, 5632, 7680


class T:
    __slots__ = ("name", "w", "r")

    def __init__(self, name):
        self.name = name
        self.w = {}
        self.r = {}


class Sched:
    ENG = ("pe", "act", "dve", "pool", "sp")
    NDMA = 8

    def __init__(self, nc, es):
        self.nc = nc
        self.nrec = 0
        self.marks = []
        self.prog = {k: [] for k in self.ENG}
        self.sems = {}
        self.cnt = {}
        self.seen = {k: {} for k in self.ENG}
        for k in self.ENG:
            self.sems[k] = es.enter_context(nc.semaphore("s_" + k))
            self.cnt[k] = 0
        self.breg = es.enter_context(nc.gpsimd.register("bnd"))
        self.dma_pool = {}
        self.dma_i = {}
        for q in ("sp", "act", "pool"):
            keys = []
            for i in range(self.NDMA):
                k = "d_%s%d" % (q, i)
                self.sems[k] = es.enter_context(nc.semaphore(k))
                self.cnt[k] = 0
                keys.append(k)
            self.dma_pool[q] = keys
            self.dma_i[q] = 0

    def _deps(self, e, reads, writes, merge):
        need = {}
        for t in reads:
            for k, v in t.w.items():
                need[k] = max(need.get(k, 0), v)
        for t in writes:
            if not merge:
                for k, v in t.w.items():
                    need[k] = max(need.get(k, 0), v)
            for k, v in t.r.items():
                need[k] = max(need.get(k, 0), v)
        waits = []
        for k, v in need.items():
            if k == e and (e == "pe" or not SAME_ENG_SYNC):
                continue
            if self.seen[e].get(k, 0) < v:
                self.seen[e][k] = v
                waits.append((k, v))
        return waits

    def mark(self, name):
        self.marks.append((name, self.nrec))

    def op(self, e, fn, reads=(), writes=()):
        if self.nrec >= DBG_LIMIT:
            return
        self.nrec += 1
        waits = self._deps(e, reads, writes, False)
        self.cnt[e] += 1
        v = self.cnt[e]
        self.prog[e].append((waits, fn, (e, 1)))
        for t in reads:
            t.r[e] = v
        for t in writes:
            t.w = {e: v}
            t.r = {}

    def dma(self, q, fn, reads=(), writes=(), merge=False):
        if self.nrec >= DBG_LIMIT:
            return
        self.nrec += 1
        i = self.dma_i[q]
        self.dma_i[q] += 1
        k = self.dma_pool[q][i % self.NDMA]
        waits = self._deps(q, reads, writes, merge)
        if self.cnt[k] > self.seen[q].get(k, 0):
            waits.append((k, self.cnt[k]))
            self.seen[q][k] = self.cnt[k]
        self.cnt[k] += 16
        v = self.cnt[k]
        self.prog[q].append((waits, fn, (k, 16)))
        for t in reads:
            t.r[k] = v
        for t in writes:
            if merge:
                t.w[k] = v
            else:
                t.w = {k: v}
                t.r = {}

    def barrier(self):
        for e in self.ENG:
            waits = []
            for k, v in self.cnt.items():
                if k != e and v > self.seen[e].get(k, 0):
                    waits.append((k, v))
                    self.seen[e][k] = v
            self.prog[e].append((waits, None, None))

    def emit(self, final=False):
        sems = self.sems
        prog = self.prog
        fin = [(k, v) for k, v in self.cnt.items() if v > 0 and k != "sp"] if final else []

        def mk(e):
            plist = prog[e]

            def body(engobj):
                if e == "pool" and self.breg is not None:
                    engobj.reg_mov(self.breg, NE * CAP - 1)
                for waits, fn, inc in plist:
                    for (wk, wv) in waits:
                        engobj.wait_ge(sems[wk], wv)
                    if fn is not None:
                        ins = fn(engobj)
                        ins.then_inc(sems[inc[0]], inc[1])
                if e == "sp":
                    for (wk, wv) in fin:
                        engobj.wait_ge(sems[wk], wv)
            return body

        with self.nc.Block() as block:
            block.tensor(mk("pe"))
            block.scalar(mk("act"))
            block.vector(mk("dve"))
            block.gpsimd(mk("pool"))
            block.sync(mk("sp"))
        self.prog = {k: [] for k in self.ENG}


class B:
    def __init__(self, S):
        self.S = S
        self.flip = 0

    def mm(self, groups, reads, writes):
        groups = [(o, list(ops)) for o, ops in groups]

        def fn(pe):
            ins = None
            for o, ops in groups:
                n = len(ops)
                for i, (l, r) in enumerate(ops):
                    ins = pe.matmul(o, l, r, start=(i == 0), stop=(i == n - 1))
            return ins
        self.S.op("pe", fn, reads, writes)

    def tr(self, items, reads, writes):
        items = list(items)

        def fn(pe):
            ins = None
            for o, i_, idt in items:
                ins = pe.transpose(o, i_, idt)
            return ins
        self.S.op("pe", fn, reads, writes)

    def act(self, out, in_, func, reads, writes, **kw):
        def fn(e):
            return e.activation(out=out, in_=in_, func=func, **kw)
        self.S.op("act", fn, reads, writes)

    def tt(self, out, in0, in1, op, reads, writes, eng="dve"):
        def fn(e):
            return e.tensor_tensor(out=out, in0=in0, in1=in1, op=op)
        self.S.op(eng, fn, reads, writes)

    def ts(self, out, in0, s1, s2, op0, op1, reads, writes, eng="dve"):
        def fn(e):
            if s2 is None:
                return e.tensor_scalar(out=out, in0=in0, scalar1=s1, scalar2=None, op0=op0)
            return e.tensor_scalar(out=out, in0=in0, scalar1=s1, scalar2=s2, op0=op0, op1=op1)
        self.S.op(eng, fn, reads, writes)

    def stt(self, out, in0, scalar, in1, op0, op1, reads, writes):
        def fn(e):
            return e.scalar_tensor_tensor(out=out, in0=in0, scalar=scalar, in1=in1, op0=op0, op1=op1)
        self.S.op("dve", fn, reads, writes)

    def copy(self, eng, out, in_, reads, writes):
        if eng == "act":
            def fn(e):
                return e.activation(out=out, in_=in_, func=AF.Copy)
        else:
            def fn(e):
                return e.tensor_copy(out=out, in_=in_)
        self.S.op(eng, fn, reads, writes)

    def evac(self, out, in_, reads, writes):
        self.flip ^= 1
        self.copy("act" if self.flip else "dve", out, in_, reads, writes)

    def reduce(self, out, in_, op, reads, writes):
        def fn(e):
            return e.tensor_reduce(out=out, in_=in_, axis=AX.X, op=op)
        self.S.op("dve", fn, reads, writes)

    def recip(self, out, in_, reads, writes):
        def fn(e):
            return e.reciprocal(out=out, in_=in_)
        self.S.op("dve", fn, reads, writes)

    def memset(self, eng, ap, val, writes):
        def fn(e):
            return e.memset(ap, val)
        self.S.op(eng, fn, (), writes)

    def dma(self, q, out, in_, reads, writes, merge=False):
        def fn(e):
            return e.dma_start(out=out, in_=in_)
        self.S.dma(q, fn, reads, writes, merge)

    def scan(self, out, d0, d1, reads, writes):
        def fn(e):
            return e.tensor_tensor_scan(out=out, data0=d0, data1=d1, initial=0.0, op0=ALU.mult, op1=ALU.add)
        self.S.op("dve", fn, reads, writes)


def build_program(dbg=False):
    nc = bass.Bass("TRN2", target_bir_lowering=False)

    def din(name, shape, dt=F32):
        return nc.dram_tensor(name, shape, dt, kind="ExternalInput").ap()

    x_own = din("x_own", [TOK, D])
    x_pre = din("x_pre", [TOK, D])
    w_in = din("w_in", [D, INW])
    w_a = din("w_a", [1024, D])
    w_r = din("w_r", [1024, D])
    w_o = din("w_o", [D, D])
    w_gate = din("w_gate", [NE, D, 512])
    w_up = din("w_up", [NE, D, 512])
    w_down = din("w_down", [NE, 512, D])
    g1_bc = din("g1_bc", [128, D])
    g2_bc = din("g2_bc", [128, D])
    gf_bc = din("gf_bc", [128, D])
    gn_in = din("gn_bc", [128, 1024])
    bias_in = din("biasT", [128, 16, 256])
    flag_in = din("flag0", [128, 1])
    sinks_in = din("sinks_bc", [128, 16])
    lbl_in = din("lbl", [128, 8, 2])
    wr_in = din("wr", [D, 36])
    bb_in = din("b_bc", [128, 36])
    c_ident = din("c_ident", [128, 128])
    c_triu = din("c_triu", [128, 64])
    c_lstrict = din("c_lstrict", [128, 128])
    c_scanmask = din("c_scanmask", [128, 512])
    c_ebase = din("c_ebase", [128, 32])
    out_d = nc.dram_tensor("out", [TOK, D], F32, kind="ExternalOutput").ap()
    if dbg:
        xmid = nc.dram_tensor("xmid", [TOK, D], F32, kind="ExternalOutput").ap()
    else:
        xmid = nc.dram_tensor("xmid", [TOK, D], F32).ap()
    Xg = nc.dram_tensor("Xg", [NE * CAP, D], BF16).ap()
    Yg = nc.dram_tensor("Yg", [NE * CAP, D], F32).ap()
    tXm = [T("xm%d" % i) for i in range(16)]
    tXg = T("Xg")
    tYg = T("Yg")

    w_in_v = w_in.rearrange("(kc p) n -> p kc n", p=128)
    w_a_v = w_a.rearrange("(kc p) n -> p kc n", p=128)
    w_r_v = w_r.rearrange("(kc p) n -> p kc n", p=128)
    w_o_v = w_o.rearrange("(kc p) n -> p kc n", p=128)

    with ExitStack() as ea:
        S = Sched(nc, ea)
        b = B(S)

        def sb(es, name, shape, dt):
            return es.enter_context(nc.sbuf_tensor("sb_" + name, shape, dt))

        ident_bf = sb(ea, "ident_bf", [128, 128], BF16)
        ident_f = sb(ea, "ident_f", [128, 128], F32)
        triu = sb(ea, "triu", [128, 64], BF16)
        lstrict = sb(ea, "lstrict", [128, 128], BF16)
        ones_bf = sb(ea, "ones_bf", [128, 128], BF16)
        ebase = sb(ea, "ebase", [128, 32], F32)
        neghalf = sb(ea, "neghalf", [128, 1], F32)
        dest_i = sb(ea, "dest_i", [128, 16, 2], I32)
        wts = sb(ea, "wts", [128, 16, 2], F32)
        tC = T("consts")
        tDest = [T("dest%d" % i) for i in range(16)]
        ps = []
        tP = []
        for i in range(6):
            ps.append(ea.enter_context(nc.psum_tensor("ps%d" % i, [128, 512], F32)))
            tP.append(T("ps%d" % i))
        pb = []
        tPb = []
        for i in range(2):
            pb.append(ea.enter_context(nc.psum_tensor("pb%d" % i, [128, 8, 128], BF16)))
            tPb.append(T("pb%d" % i))

        b.dma("pool", ident_bf[:], c_ident, (), [tC])
        b.dma("sp", ident_f[:], c_ident, (), [tC], merge=True)
        b.dma("pool", triu[:], c_triu, (), [tC], merge=True)
        b.dma("pool", lstrict[:], c_lstrict, (), [tC], merge=True)
        b.dma("sp", ebase[:], c_ebase, (), [tC], merge=True)
        tC2 = T("consts2")
        b.memset("pool", ones_bf[:], 1.0, [tC2])
        b.memset("pool", neghalf[:], -0.5, [tC2])

        with ExitStack() as e1:
            scanmask = sb(e1, "scanmask", [128, 512], F32)
            biasT = sb(e1, "biasT", [128, 16, 256], F32)
            gn_bc = sb(e1, "gn_bc", [128, 1024], F32)
            wr_sb = sb(e1, "wr_sb", [128, 16, 36], F32)
            b_bc = sb(e1, "b_bc", [128, 36], F32)
            esink = sb(e1, "esink", [128, 16], F32)
            flag0 = sb(e1, "flag0", [128, 1], F32)
            lbl = sb(e1, "lbl", [128, 8, 2], F32)
            lb = sb(e1, "lb", [128, 8], F32)
            oml = sb(e1, "oml", [128, 8], F32)
            ln_oml = sb(e1, "ln_oml", [128, 8], F32)
            macc = sb(e1, "macc", [128, 32], BF16)
            tK = T("consts_p1")
            b.dma("sp", scanmask[:], c_scanmask, (), [tK])
            b.dma("sp", biasT[:], bias_in, (), [tK], merge=True)
            b.dma("sp", gn_bc[:], gn_in, (), [tK], merge=True)
            b.dma("sp", wr_sb[:], wr_in.rearrange("(kc p) n -> p kc n", p=128), (), [tK], merge=True)
            b.dma("sp", b_bc[:], bb_in, (), [tK], merge=True)
            b.dma("sp", esink[:], sinks_in, (), [tK], merge=True)
            b.dma("sp", flag0[:], flag_in, (), [tK], merge=True)
            b.dma("sp", lbl[:], lbl_in, (), [tK], merge=True)
            tL = T("lb")
            b.tt(lb[:], lbl[:, :, 1], lbl[:, :, 0], ALU.subtract, [tK], [tL])
            b.act(lb[:], lb[:], AF.Exp, [tL], [tL])
            b.ts(lb[:], lb[:], 1.0, None, ALU.add, None, [tL], [tL])
            b.recip(lb[:], lb[:], [tL], [tL])
            b.ts(oml[:], lb[:], -1.0, 1.0, ALU.mult, ALU.add, [tL], [tL])
            b.act(ln_oml[:], oml[:], AF.Ln, [tL], [tL])
            tEs = T("esink")
            b.act(esink[:], esink[:], AF.Exp, [tK], [tEs])
            tMacc = T("macc")
            b.memset("pool", macc[:], 0.0, [tMacc])

            NR = NRING
            wring = [sb(e1, "wring%d" % i, [128, 4096], BF16) for i in range(NR)]
            tW = [T("wring%d" % i) for i in range(NR)]
            hT = sb(e1, "hT", [128, KC, G], BF16)
            tHT = T("hT")
            qaT = sb(e1, "qaT", [128, 8, G], BF16)
            tQA = [T("qa%d" % i) for i in range(4)]
            kbuf = sb(e1, "kbuf", [128, 4, 128 + G], BF16)
            tKb = T("kbuf")
            vbuf = sb(e1, "vbuf", [128, 5, 4, 65], BF16)
            tVb = T("vbuf")
            QtT = sb(e1, "QtT", [128, 8, G], BF16)
            tQt = [T("Qt%d" % i) for i in range(4)]
            KtT = sb(e1, "KtT", [128, 8, G], BF16)
            tKt = T("KtT")
            vh = sb(e1, "vh", [128, 4, 1024], BF16)
            tVh = T("vh")
            gsg = sb(e1, "gsg", [128, 4, 1024], BF16)
            tGs = T("gsg")
            sgA = sb(e1, "sgA", [128, 16, G], BF16)
            sgH = sb(e1, "sgH", [128, 16, G], BF16)
            tSgA = T("sgA")
            tSgH = T("sgH")
            S32 = sb(e1, "S32", [128, 8, 128], F32)
            Sbf = sb(e1, "Sbf", [128, 8, 128], BF16)
            tS32 = T("S32")
            tSbf = T("Sbf")
            dec = sb(e1, "dec", [128, 8, 8], F32)
            tDec = T("dec")
            xt0 = sb(e1, "xt0", [128, D], F32)
            xt1 = sb(e1, "xt1", [128, D], F32)
            gbc = sb(e1, "gbc", [128, D], F32)
            hb = sb(e1, "hb", [128, D], BF16)
            tX0, tX1, tGbc, tHb = T("xt0"), T("xt1"), T("gbc"), T("hb")
            arena = sb(e1, "arena", [128, 6 * 512], F32)
            tA = [T("ar%d" % i) for i in range(6)]
            small = sb(e1, "small", [128, 64], F32)
            tSm = T("small")
            smallr = sb(e1, "smallr", [128, 160], F32)
            tSr = T("smallr")

            def ar(i, n=1):
                return arena[:, i * 512:(i + n) * 512]

            b.memset("pool", S32[:], 0.0, [tS32])
            b.memset("pool", Sbf[:], 0.0, [tSbf])
            b.memset("pool", vbuf[:], 1.0, [tVb])
            b.memset("pool", kbuf[:], 0.0, [tKb])

            b.memset("pool", hb[:], 0.0, [tHb])
            for i in range(NE * CAP // 128):
                b.dma("sp", Xg[i * 128:(i + 1) * 128, :], hb[:], [tHb], [tXg], merge=True)

            wlist = []

            def wq_win(col0):
                wlist.append((w_in_v[:, :, col0:col0 + 256], 16, 256))
                return len(wlist) - 1
            wstate = {"issued": 0}

            def wissue(upto):
                while wstate["issued"] <= min(upto, len(wlist) - 1):
                    i = wstate["issued"]
                    src, kc, n = wlist[i]
                    slot = wring[i % NR][:, 0:kc * n].rearrange("p (k n) -> p k n", k=kc)
                    b.dma("pool", slot, src, (), [tW[i % NR]])
                    wstate["issued"] += 1

            def wget(i):
                wissue(i + WPD)
                src, kc, n = wlist[i]
                return wring[i % NR][:, 0:kc * n].rearrange("p (k n) -> p k n", k=kc), tW[i % NR]

            pstate = {"i": 0}

            def nextps():
                pstate["i"] ^= 1
                return ps[pstate["i"]], tP[pstate["i"]]

            def rms_tile(xt, tXt, gtile, tG, out, tOut, junk, tJunk):
                b.act(junk, xt, AF.Square, [tXt], [tJunk, tSm], accum_out=small[:, 0:1])
                b.ts(small[:, 1:2], small[:, 0:1], 1.0 / D, EPS, ALU.mult, ALU.add, [tSm], [tSm])
                b.act(small[:, 1:2], small[:, 1:2], AF.Ln, [tSm], [tSm])
                b.act(small[:, 2:3], small[:, 1:2], AF.Exp, [tSm], [tSm], scale=-0.5)
                b.stt(out, xt, small[:, 2:3], gtile, ALU.mult, ALU.mult, [tXt, tSm, tG, tJunk], [tOut])

            def stage_norm(xsrc, grp):
                b.dma("sp", gbc[:], g1_bc, (), [tGbc])
                for tt in range(4):
                    xt, tXt = (xt0, tX0) if tt % 2 == 0 else (xt1, tX1)
                    r0 = grp * G + tt * 128
                    b.dma("sp", xt[:], xsrc[r0:r0 + 128, :], (), [tXt])
                    rms_tile(xt[:], tXt, gbc[:], tGbc, hb[:], tHb, hb[:], tHb)
                    for rd in range(2):
                        p, tp = pb[rd], tPb[rd]
                        b.tr([(p[:, j, :], hb[:, (rd * 8 + j) * 128:(rd * 8 + j + 1) * 128], ident_bf[:])
                              for j in range(8)], [tHb, tC], [tp])
                        b.evac(hT[:, rd * 8:rd * 8 + 8, tt * 128:(tt + 1) * 128], p[:], [tp], [tHT])

            def proj_fm(slot, tSlot, c):
                p, tp = nextps()
                b.mm([(p[:], [(slot[:, kc, c * 128:(c + 1) * 128], hT[:, kc, :]) for kc in range(KC)])],
                     [tSlot, tHT], [tp])
                return p, tp

            def proj_tm(slot, tSlot, tt, n):
                p, tp = nextps()
                b.mm([(p[:, 0:n], [(hT[:, kc, tt * 128:(tt + 1) * 128], slot[:, kc, 0:n]) for kc in range(KC)])],
                     [tSlot, tHT], [tp])
                return p, tp

            def chain(h, p, tp, own):
                E, L1, L2, bb_ = ar(0), ar(1), ar(2), ar(3)
                b.act(E, p[:], AF.Exp, [tp], [tA[0]])
                b.act(L1, E, AF.Ln, [tA[0], tL], [tA[1]], bias=lb[:, h:h + 1])
                b.act(L2, E, AF.Ln, [tA[0]], [tA[2]], bias=1.0)
                b.tt(L1, L1, L2, ALU.subtract, [tA[1], tA[2]], [tA[1]])
                b.scan(bb_, scanmask[:], L1, [tK, tA[1]], [tA[3]])
                b.act(E, bb_, AF.Exp, [tA[3]], [tA[0]])
                if own:
                    b.tt(QtT[:, h, :], QtT[:, h, :], E, ALU.mult, [tA[0]] + tQt, tQt)
                b.tt(L2, L2, bb_, ALU.add, [tA[2], tA[3]], [tA[2]])
                b.act(KtT[:, h, :], L2, AF.Exp, [tA[2], tL], [tKt], scale=-1.0, bias=ln_oml[:, h:h + 1])
                b.copy("pool", dec[:, h, :], E.rearrange("p (c t) -> p c t", t=64)[:, :, 63], [tA[0]], [tDec])

            def stage_proj(own, last_prefix, wbase):
                wi = wbase
                if own:
                    for blk in range(4):
                        slot, tsl = wget(wi); wi += 1
                        for c in range(2):
                            p, tp = proj_fm(slot, tsl, c)
                            b.act(qaT[:, blk * 2 + c, :], p[:], AF.Copy, [tp], tQA, scale=0.125)
                if own or last_prefix:
                    slot, tsl = wget(wi); wi += 1
                    for g in range(4):
                        p, tp = nextps()
                        b.mm([(p[0:64, :], [(slot[:, kc, g * 64:(g + 1) * 64], hT[:, kc, :]) for kc in range(KC)]),
                              (p[64:128, :], [(slot[:, kc, g * 64:(g + 1) * 64], hT[:, kc, :]) for kc in range(KC)])],
                             [tsl, tHT], [tp])
                        b.evac(kbuf[:, g, 128:128 + G], p[:], [tp], [tKb])
                    slot, tsl = wget(wi); wi += 1
                    for tt in range(4):
                        p, tp = proj_tm(slot, tsl, tt, 256)
                        b.evac(vbuf[:, 1 + tt, :, 0:64], p[:, 0:256].rearrange("p (g d) -> p g d", g=4), [tp], [tVb])
                if own:
                    for blk in range(4):
                        slot, tsl = wget(wi); wi += 1
                        for c in range(2):
                            p, tp = proj_fm(slot, tsl, c)
                            b.evac(QtT[:, blk * 2 + c, :], p[:], [tp], tQt)
                for blk in range(4):
                    slot, tsl = wget(wi); wi += 1
                    for c in range(2):
                        p, tp = proj_fm(slot, tsl, c)
                        chain(blk * 2 + c, p, tp, own)
                for blk in range(4):
                    slot, tsl = wget(wi); wi += 1
                    for tt in range(4):
                        p, tp = proj_tm(slot, tsl, tt, 256)
                        b.evac(vh[:, tt, blk * 256:(blk + 1) * 256], p[:, 0:256], [tp], [tVh])
                if own:
                    for blk in range(4):
                        slot, tsl = wget(wi); wi += 1
                        for tt in range(4):
                            p, tp = proj_tm(slot, tsl, tt, 256)
                            tmp = ar(4)[:, 0:256]
                            b.act(tmp, p[:, 0:256], AF.Silu, [tp], [tA[4]])
                            b.tt(gsg[:, tt, blk * 256:(blk + 1) * 256], tmp, gn_bc[:, blk * 256:(blk + 1) * 256],
                                 ALU.mult, [tA[4], tK], [tGs])
                    for blk in range(8):
                        slot, tsl = wget(wi); wi += 1
                        for c in range(2):
                            p, tp = proj_fm(slot, tsl, c)
                            b.act(sgA[:, blk * 2 + c, :], p[:], AF.Sigmoid, [tp], [tSgA])
                    for blk in range(8):
                        slot, tsl = wget(wi); wi += 1
                        for c in range(2):
                            p, tp = proj_fm(slot, tsl, c)
                            b.act(sgH[:, blk * 2 + c, :], p[:], AF.Sigmoid, [tp], [tSgH])
                return wi

            def stage_attn(first_group):
                sE = ar(0, 2).rearrange("p (a n) -> p a n", a=2)
                pT = ar(2).bitcast(BF16).rearrange("p (a h q) -> p a h q", a=2, h=4)
                a_tok = ar(3).bitcast(BF16)
                for tt in range(4):
                    for g in range(4):
                        banks = [[(ps[2], tP[2]), (ps[3], tP[3])], [(ps[0], tP[0]), (ps[1], tP[1])]]
                        sE4 = sE.rearrange("p a (j two q) -> p a j two q", j=2, two=2)
                        for kb in range(2):
                            for par in range(2):
                                pbk, tpbk = banks[kb][par]
                                p0 = par * 64
                                groups = []
                                for jj in range(2):
                                    h = g * 4 + jj * 2 + par
                                    groups.append((pbk[:, jj * 128:(jj + 1) * 128],
                                                   [(kbuf[p0:p0 + 64, g, (tt + kb) * 128:(tt + kb + 1) * 128],
                                                     qaT[p0:p0 + 64, h // 2, tt * 128:(tt + 1) * 128])]))
                                b.mm(groups, [tKb, tQA[tt]], [tpbk])
                                bcol = slice(128, 256) if kb == 0 else slice(0, 128)
                                bsl = biasT[:, g * 4:(g + 1) * 4, bcol].rearrange("p (j two) q -> p j two q", two=2)[:, :, par, :]
                                b.stt(sE4[:, kb, :, par, :], pbk[:, 0:256].rearrange("p (j q) -> p j q", j=2), 60.0, bsl,
                                      ALU.min, ALU.add, [tpbk, tK], [tA[kb]])
                        b.act(pT, sE.rearrange("p a (h q) -> p a h q", h=4), AF.Exp, [tA[0], tA[1]], [tA[2]])
                        if first_group and tt == 0:
                            b.ts(pT[:, 0], pT[:, 0], flag0[:, 0:1], None, ALU.mult, None, [tA[2], tK], [tA[2]])
                        po, tpo = ps[4], tP[4]
                        groups = []
                        for hh in range(4):
                            groups.append((po[:, hh * 65:(hh + 1) * 65],
                                           [(pT[:, kb, hh, :], vbuf[:, tt + kb, g, :]) for kb in range(2)]))
                        b.mm(groups, [tA[2], tVb], [tpo])
                        pov = po[:, 0:260].rearrange("p (h d) -> p h d", h=4)
                        b.tt(smallr[:, 0:4], pov[:, :, 64], esink[:, g * 4:(g + 1) * 4], ALU.add, [tpo, tEs], [tSr])
                        b.recip(smallr[:, 4:8], smallr[:, 0:4], [tSr], [tSr])
                        b.tt(a_tok[:, g * 256:(g + 1) * 256].rearrange("p (h d) -> p h d", h=4), pov[:, :, 0:64],
                             smallr[:, 4:8].unsqueeze(2).broadcast_to([128, 4, 64]), ALU.mult, [tpo, tSr], [tA[3]])
                    p, tp = pb[tt % 2], tPb[tt % 2]
                    b.tr([(p[:, j, :], a_tok[:, j * 128:(j + 1) * 128], ident_bf[:]) for j in range(8)],
                         [tA[3], tC], [tp])
                    b.evac(qaT[:, :, tt * 128:(tt + 1) * 128], p[:], [tp], [tQA[tt]])
                b.copy("pool", kbuf[:, :, 0:128], kbuf[:, :, G:G + 128], [tKb], [tKb])
                b.copy("pool", vbuf[:, 0], vbuf[:, 4], [tVb], [tVb])

            def stage_hloop(own):
                attm = ar(0).bitcast(BF16)[:, 0:512].rearrange("p (h t) -> p h t", h=8)
                r_tok = ar(1).bitcast(BF16)
                junk = ar(2).bitcast(BF16)
                Kt = ar(3).bitcast(BF16).rearrange("p (h k) -> p h k", h=8)
                for tt in range(4):
                    p, tp = pb[tt % 2], tPb[tt % 2]
                    b.tr([(p[:, h, :], KtT[:, h, tt * 128:(tt + 1) * 128], ident_bf[:]) for h in range(8)],
                         [tKt, tC], [tp])
                    b.evac(Kt, p[:], [tp], [tA[3]])
                    for cc in range(2):
                        c = tt * 2 + cc
                        p0 = cc * 64
                        cs = slice(c * 64, (c + 1) * 64)
                        if own:
                            pa, tpa = ps[4], tP[4]
                            b.mm([(pa[p0:p0 + 64, h * 64:(h + 1) * 64], [(KtT[:, h, cs], QtT[:, h, cs])])
                                  for h in range(8)], [tKt, tQt[tt]], [tpa])
                            b.tt(attm[p0:p0 + 64], pa[p0:p0 + 64, :].rearrange("p (h t) -> p h t", h=8),
                                 triu[p0:p0 + 64, :].unsqueeze(1).broadcast_to([64, 8, 64]), ALU.mult,
                                 [tpa, tC], [tA[0]])
                            groups = []
                            for h in range(8):
                                po = ps[2] if h < 4 else ps[3]
                                o_ap = po[p0:p0 + 64, (h % 4) * 128:(h % 4 + 1) * 128]
                                groups.append((o_ap, [(QtT[:, h, cs], Sbf[:, h, :]),
                                                      (attm[p0:p0 + 64, h, :], vh[p0:p0 + 64, tt, h * 128:(h + 1) * 128])]))
                            b.mm(groups, [tQt[tt], tSbf, tA[0], tVh], [tP[2], tP[3]])
                        groups = []
                        for h in range(8):
                            po = ps[0] if h < 4 else ps[1]
                            groups.append((po[:, (h % 4) * 128:(h % 4 + 1) * 128],
                                           [(Kt[p0:p0 + 64, h, :], vh[p0:p0 + 64, tt, h * 128:(h + 1) * 128])]))
                        b.mm(groups, [tA[3], tVh], [tP[0], tP[1]])
                        b.tt(S32[:, 0:4, :], ps[0][:].rearrange("p (h v) -> p h v", h=4), S32[:, 0:4, :], ALU.add,
                             [tP[0], tS32], [tS32])
                        b.tt(S32[:, 4:8, :], ps[1][:].rearrange("p (h v) -> p h v", h=4), S32[:, 4:8, :], ALU.add,
                             [tP[1], tS32], [tS32])
                        b.tt(S32[:], S32[:], dec[:, :, c].unsqueeze(2).broadcast_to([128, 8, 128]), ALU.mult,
                             [tS32, tDec], [tS32])
                        b.copy("act", Sbf[:], S32[:], [tS32], [tSbf])
                    if own:
                        for h in range(8):
                            po = ps[2] if h < 4 else ps[3]
                            b.act(junk[:, 0:128], po[:, (h % 4) * 128:(h % 4 + 1) * 128], AF.Square,
                                  [tP[2], tP[3]], [tA[2], tSr], accum_out=smallr[:, 16 + h:17 + h])
                        b.ts(smallr[:, 32:40], smallr[:, 16:24], 1.0 / 128, EPS, ALU.mult, ALU.add, [tSr], [tSr])
                        b.act(smallr[:, 32:40], smallr[:, 32:40], AF.Ln, [tSr], [tSr])
                        b.act(smallr[:, 40:48], smallr[:, 32:40], AF.Exp, [tSr], [tSr], scale=-0.5)
                        for h in range(8):
                            po = ps[2] if h < 4 else ps[3]
                            b.stt(r_tok[:, h * 128:(h + 1) * 128], po[:, (h % 4) * 128:(h % 4 + 1) * 128],
                                  smallr[:, 40 + h:41 + h], gsg[:, tt, h * 128:(h + 1) * 128], ALU.mult, ALU.mult,
                                  [tP[2], tP[3], tSr, tGs], [tA[1]])
                        p, tp = pb[(tt + 1) % 2], tPb[(tt + 1) % 2]
                        b.tr([(p[:, j, :], r_tok[:, j * 128:(j + 1) * 128], ident_bf[:]) for j in range(8)],
                             [tA[1], tC], [tp])
                        b.evac(QtT[:, :, tt * 128:(tt + 1) * 128], p[:], [tp], [tQt[tt]])

            def stage_branch(wi):
                m2 = ar(1)
                for nb in range(4):
                    sa, tsa = wget(wi); wi += 1
                    for c in range(4):
                        j = nb * 4 + c
                        p, tp = ps[2 + c % 2], tP[2 + c % 2]
                        b.mm([(p[:], [(sa[:, kc, c * 128:(c + 1) * 128], qaT[:, kc, :]) for kc in range(8)])],
                             [tsa] + tQA, [tp])
                        b.tt(ar(2 + c), p[:], sgA[:, j, :], ALU.mult, [tp, tSgA], [tA[2 + c]])
                    sr, tsr = wget(wi); wi += 1
                    for c in range(4):
                        j = nb * 4 + c
                        p, tp = ps[2 + c % 2], tP[2 + c % 2]
                        b.mm([(p[:], [(sr[:, kc, c * 128:(c + 1) * 128], QtT[:, kc, :]) for kc in range(8)])],
                             [tsr] + tQt, [tp])
                        b.tt(m2, p[:], sgH[:, j, :], ALU.mult, [tp, tSgH], [tA[1]])
                        b.tt(hT[:, j, :], ar(2 + c), m2, ALU.add, [tA[2 + c], tA[1]], [tHT])
                return wi

            def stage_out(wi, grp):
                k = 0
                for nb in range(8):
                    so, tso = wget(wi); wi += 1
                    for tt in range(4):
                        r0 = grp * G + tt * 128
                        xp, txp = (ar(2), tA[2]) if k % 2 == 0 else (ar(3), tA[3])
                        op, top = (ar(4), tA[4]) if k % 2 == 0 else (ar(5), tA[5])
                        k += 1
                        b.dma("sp", xp[:, 0:256], x_own[r0:r0 + 128, nb * 256:(nb + 1) * 256], (), [txp])
                        p, tp = nextps()
                        b.mm([(p[:, 0:256], [(hT[:, kc, tt * 128:(tt + 1) * 128], so[:, kc, :]) for kc in range(KC)])],
                             [tso, tHT], [tp])
                        b.tt(op[:, 0:256], p[:, 0:256], xp[:, 0:256], ALU.add, [tp, txp], [top])
                        b.dma("sp", xmid[r0:r0 + 128, nb * 256:(nb + 1) * 256], op[:, 0:256], [top],
                              [tXm[grp * 4 + tt]], merge=True)
                return wi

            def stage_route(grp):
                b.dma("sp", gbc[:], g2_bc, (), [tGbc])
                for tt in range(4):
                    ti = grp * 4 + tt
                    r0 = ti * 128
                    b.dma("sp", xt0[:], xmid[r0:r0 + 128, :], [tXm[ti]], [tX0])
                    rms_tile(xt0[:], tX0, gbc[:], tGbc, xt1[:], tX1, hb[:], tHb)
                    b.copy("pool", hb[:], xt1[:], [tX1], [tHb])
                    pr, tpr = ps[5], tP[5]
                    for rd in range(4):
                        p, tp = ps[2 + rd % 2], tP[2 + rd % 2]
                        h2T, th2T = (ar(0), tA[0]) if rd % 2 == 0 else (ar(1), tA[1])
                        b.tr([(p[:, j * 128:(j + 1) * 128], xt1[:, (rd * 4 + j) * 128:(rd * 4 + j + 1) * 128], ident_f[:])
                              for j in range(4)], [tX1, tC], [tp])
                        b.evac(h2T, p[:], [tp], [th2T])
                        h2v = h2T.rearrange("p (j t) -> p j t", j=4)

                        def fn(pe, h2v=h2v, rd=rd, pr=pr):
                            ins = None
                            for j in range(4):
                                kc = rd * 4 + j
                                ins = pe.matmul(pr[:, 0:36], h2v[:, j, :], wr_sb[:, kc, :], start=(kc == 0), stop=(kc == 15))
                            return ins
                        S.op("pe", fn, [th2T, tK], [tpr])
                    sm = smallr
                    LG = sm[:, 48:84]
                    b.tt(LG, pr[:, 0:36], b_bc[:], ALU.add, [tpr, tK], [tSr])
                    gl = sm[:, 48:52]
                    el = sm[:, 52:84].rearrange("p (g e) -> p g e", g=4)
                    gmax, ngmax, gsum, gw = sm[:, 84:85], sm[:, 85:86], sm[:, 86:87], sm[:, 87:88]
                    goh = sm[:, 88:92]
                    b.reduce(gmax, gl, ALU.max, [tSr], [tSr])
                    b.ts(goh, gl, gmax, None, ALU.is_equal, None, [tSr], [tSr])
                    b.ts(ngmax, gmax, -1.0, None, ALU.mult, None, [tSr], [tSr])
                    b.act(sm[:, 92:96], gl, AF.Exp, [tSr], [tSr], bias=ngmax, accum_out=gsum)
                    b.recip(gw, gsum, [tSr], [tSr])
                    tmp32 = sm[:, 96:128].rearrange("p (g e) -> p g e", g=4)
                    b.tt(tmp32, el, goh.unsqueeze(2).broadcast_to([128, 4, 8]), ALU.mult, [tSr], [tSr])
                    esel = sm[:, 128:136]
                    b.reduce(esel, tmp32.rearrange("p g e -> p e g"), ALU.add, [tSr], [tSr])
                    top8 = sm[:, 136:144]

                    def fmax(e, top8=top8, esel=esel):
                        return e.max(out=top8, in_=esel)
                    S.op("dve", fmax, [tSr], [tSr])
                    oh1, oh2 = sm[:, 144:152], sm[:, 152:160]
                    b.ts(oh1, esel, top8[:, 0:1], None, ALU.is_equal, None, [tSr], [tSr])
                    b.ts(oh2, esel, top8[:, 1:2], None, ALU.is_equal, None, [tSr], [tSr])
                    s2 = small
                    b.ts(s2[:, 8:9], top8[:, 0:1], -1.0, None, ALU.mult, None, [tSr], [tSm])
                    b.act(s2[:, 9:10], top8[:, 1:2], AF.Exp, [tSr, tSm], [tSm], bias=s2[:, 8:9])
                    b.ts(s2[:, 10:11], s2[:, 9:10], 1.0, None, ALU.add, None, [tSm], [tSm])
                    b.recip(s2[:, 10:11], s2[:, 10:11], [tSm], [tSm])
                    b.tt(s2[:, 11:12], s2[:, 10:11], gw, ALU.mult, [tSm, tSr], [tSm])
                    b.tt(s2[:, 12:13], s2[:, 11:12], s2[:, 9:10], ALU.mult, [tSm], [tSm])
                    M1 = ar(2)[:, 0:32].rearrange("p (g e) -> p g e", g=4)
                    M2 = ar(2)[:, 32:64].rearrange("p (g e) -> p g e", g=4)
                    gohb = goh.unsqueeze(2).broadcast_to([128, 4, 8])
                    b.tt(M1, gohb, oh1.unsqueeze(1).broadcast_to([128, 4, 8]), ALU.mult, [tSr], [tA[2]])
                    b.tt(M2, gohb, oh2.unsqueeze(1).broadcast_to([128, 4, 8]), ALU.mult, [tSr], [tA[2]])
                    Mb = ar(3).bitcast(BF16)[:, 0:32]
                    b.tt(Mb, ar(2)[:, 0:32], ar(2)[:, 32:64], ALU.add, [tA[2]], [tA[3]])
                    pp, tpp = ps[4], tP[4]
                    b.mm([(pp[:, 0:32], [(lstrict[:], Mb), (ones_bf[:], macc[:])])], [tA[3], tC, tC2, tMacc], [tpp])
                    b.tt(macc[:], macc[:], Mb, ALU.add, [tA[3], tMacc], [tMacc], eng="pool")
                    pos = ar(2)[:, 64:96]
                    b.copy("dve", pos, pp[:, 0:32], [tpp], [tA[2]])
                    tm = ar(2)[:, 96:128]
                    for kk, Mk in enumerate((ar(2)[:, 0:32], ar(2)[:, 32:64])):
                        pk, ek, okf, dk = (s2[:, 16 + 8 * kk + i:17 + 8 * kk + i] for i in range(4))
                        b.tt(tm, pos, Mk, ALU.mult, [tA[2]], [tA[2]])
                        b.reduce(pk, tm, ALU.add, [tA[2]], [tSm])
                        b.tt(tm, ebase[:], Mk, ALU.mult, [tA[2], tC], [tA[2]])
                        b.reduce(ek, tm, ALU.add, [tA[2]], [tSm])
                        b.ts(okf, pk, float(CAP), None, ALU.is_lt, None, [tSm], [tSm])
                        b.ts(dk, okf, -1.0e6, 1.0e6, ALU.mult, ALU.add, [tSm], [tSm])
                        b.tt(dk, dk, pk, ALU.add, [tSm], [tSm])
                        b.tt(dk, dk, ek, ALU.add, [tSm], [tSm])
                        b.copy("dve", dest_i[:, ti, kk:kk + 1], dk, [tSm], [tDest[ti]])
                        b.tt(wts[:, ti, kk:kk + 1], s2[:, 11 + kk:12 + kk], okf, ALU.mult, [tSm], [tDest[ti]])
                    for kk in range(2):
                        def fsc(g_, ti=ti, kk=kk):
                            return g_.indirect_dma_start(
                                out=Xg, out_offset=bass.IndirectOffsetOnAxis(ap=dest_i[:, ti, kk:kk + 1], axis=0),
                                in_=hb[:, :], in_offset=None, bounds_check=S.breg, oob_is_err=False)
                        S.dma("pool", fsc, [tHb, tDest[ti]], [tXg], merge=True)

            plan = []
            for pg in range(NG):
                base = len(wlist)
                if pg == NG - 1:
                    wq_win(O_KA); wq_win(O_VA)
                for blk in range(4):
                    wq_win(O_FH + blk * 256)
                for blk in range(4):
                    wq_win(O_IH + blk * 256)
                plan.append(base)
            for og in range(NG):
                base = len(wlist)
                for blk in range(4):
                    wq_win(O_QA + blk * 256)
                wq_win(O_KA); wq_win(O_VA)
                for sec in (O_QH, O_FH, O_IH, O_GH):
                    for blk in range(4):
                        wq_win(sec + blk * 256)
                for sec in (O_GA, O_GB):
                    for blk in range(8):
                        wq_win(sec + blk * 256)
                for nb in range(4):
                    wlist.append((w_a_v[:, :, nb * 512:(nb + 1) * 512], 8, 512))
                    wlist.append((w_r_v[:, :, nb * 512:(nb + 1) * 512], 8, 512))
                for nb in range(8):
                    wlist.append((w_o_v[:, :, nb * 256:(nb + 1) * 256], 16, 256))
                plan.append(base)

            S.mark("init")
            for pg in range(NG - DBG_NGP, NG):
                stage_norm(x_pre, pg)
                S.mark("pnorm%d" % pg)
                stage_proj(False, pg == NG - 1, plan[pg])
                S.mark("pproj%d" % pg)
                stage_hloop(False)
                S.mark("phloop%d" % pg)
            b.copy("pool", kbuf[:, :, 0:128], kbuf[:, :, G:G + 128], [tKb], [tKb])
            b.copy("pool", vbuf[:, 0], vbuf[:, 4], [tVb], [tVb])
            for og in range(DBG_NGO):
                stage_norm(x_own, og)
                S.mark("norm%d" % og)
                wi = stage_proj(True, False, plan[NG + og])
                S.mark("proj%d" % og)
                stage_attn(og == 0)
                S.mark("attn%d" % og)
                stage_hloop(True)
                S.mark("hloop%d" % og)
                wi = stage_branch(wi)
                S.mark("branch%d" % og)
                wi = stage_out(wi, og)
                S.mark("out%d" % og)
                stage_route(og)
                S.mark("route%d" % og)
            S.mark("phase1")
            S.barrier()
            S.emit(final=(dbg == 2))
            if dbg == 2:
                return nc

        with ExitStack() as e2:
            wg = [sb(e2, "wg%d" % i, [128, KC, 512], BF16) for i in range(2)]
            wu = [sb(e2, "wu%d" % i, [128, KC, 512], BF16) for i in range(2)]
            wd = [sb(e2, "wd%d" % i, [128, 4, D], BF16) for i in range(2)]
            tWg = [T("wg%d" % i) for i in range(2)]
            tWu = [T("wu%d" % i) for i in range(2)]
            tWd = [T("wd%d" % i) for i in range(2)]
            xg_t = [sb(e2, "xg%d" % i, [128, D], BF16) for i in range(2)]
            tXgt = [T("xgt%d" % i) for i in range(2)]
            XgT = sb(e2, "XgT", [128, KC, CAP], BF16)
            tXgT = T("XgT")
            hTe = sb(e2, "hTe", [128, 4, CAP], BF16)
            tHe = T("hTe")
            sgt = [sb(e2, "sgt%d" % i, [128, CAP], F32) for i in range(2)]
            tSg = [T("sgt%d" % i) for i in range(2)]
            ysb = [sb(e2, "ysb%d" % i, [128, D], F32) for i in range(2)]
            tYs = [T("ysb%d" % i) for i in range(2)]

            def wload(e):
                i = e % 2
                b.dma("pool", wg[i][:], w_gate[e].rearrange("(kc p) n -> p kc n", p=128), (), [tWg[i]])
                b.dma("pool", wu[i][:], w_up[e].rearrange("(kc p) n -> p kc n", p=128), (), [tWu[i]])
                b.dma("pool", wd[i][:], w_down[e].rearrange("(fc p) n -> p fc n", p=128), (), [tWd[i]])

            wload(0)
            for e in range(DBG_NEX):
                i = e % 2
                if e + 1 < DBG_NEX:
                    wload(e + 1)
                for st in range(2):
                    r0 = e * CAP + st * 128
                    b.dma("sp", xg_t[st][:], Xg[r0:r0 + 128, :], [tXg], [tXgt[st]])
                    for rd in range(2):
                        p, tp = pb[rd], tPb[rd]
                        b.tr([(p[:, j, :], xg_t[st][:, (rd * 8 + j) * 128:(rd * 8 + j + 1) * 128], ident_bf[:])
                              for j in range(8)], [tXgt[st], tC], [tp])
                        b.evac(XgT[:, rd * 8:rd * 8 + 8, st * 128:(st + 1) * 128], p[:], [tp], [tXgT])
                for fc in range(4):
                    p, tp = ps[fc % 2], tP[fc % 2]
                    b.mm([(p[:, 0:CAP], [(wg[i][:, kc, fc * 128:(fc + 1) * 128], XgT[:, kc, :]) for kc in range(KC)]),
                          (p[:, CAP:2 * CAP], [(wu[i][:, kc, fc * 128:(fc + 1) * 128], XgT[:, kc, :]) for kc in range(KC)])],
                         [tWg[i], tWu[i], tXgT], [tp])
                    b.act(sgt[fc % 2][:], p[:, 0:CAP], AF.Silu, [tp], [tSg[fc % 2]])
                    b.tt(hTe[:, fc, :], sgt[fc % 2][:], p[:, CAP:2 * CAP], ALU.mult, [tSg[fc % 2], tp], [tHe])
                for st in range(2):
                    for nb in range(4):
                        p, tp = ps[2 + nb % 2], tP[2 + nb % 2]
                        b.mm([(p[:], [(hTe[:, fc, st * 128:(st + 1) * 128], wd[i][:, fc, nb * 512:(nb + 1) * 512])
                                      for fc in range(4)])], [tHe, tWd[i]], [tp])
                        b.evac(ysb[st][:, nb * 512:(nb + 1) * 512], p[:], [tp], [tYs[st]])
                    r0 = e * CAP + st * 128
                    b.dma("sp", Yg[r0:r0 + 128, :], ysb[st][:], [tYs[st]], [tYg], merge=True)
            S.barrier()
            S.emit()

        with ExitStack() as e3:
            gf = sb(e3, "gf", [128, D], F32)
            tGf = T("gf")
            b.dma("sp", gf[:], gf_bc, (), [tGf])
            xm_t = [sb(e3, "xm_t%d" % i, [128, D], F32) for i in range(2)]
            y1_t = [sb(e3, "y1_t%d" % i, [128, D], F32) for i in range(2)]
            y2_t = [sb(e3, "y2_t%d" % i, [128, D], F32) for i in range(2)]
            o_t = [sb(e3, "o_t%d" % i, [128, D], F32) for i in range(2)]
            jk = sb(e3, "jk", [128, D], BF16)
            sm3 = sb(e3, "sm3", [128, 8], F32)
            tXt = [T("xm_t%d" % i) for i in range(2)]
            tY1 = [T("y1_%d" % i) for i in range(2)]
            tY2 = [T("y2_%d" % i) for i in range(2)]
            tO = [T("o_%d" % i) for i in range(2)]
            tJ = T("jk")
            tS3 = T("sm3")
            for i in range(2):
                b.memset("pool", y1_t[i][:], 0.0, [tY1[i]])
                b.memset("pool", y2_t[i][:], 0.0, [tY2[i]])
            for ti in range(DBG_NT3):
                i = ti % 2
                r0 = ti * 128
                b.dma("sp", xm_t[i][:], xmid[r0:r0 + 128, :], [tXm[ti]], [tXt[i]])
                for kk, (yt, ty) in enumerate(((y1_t[i], tY1[i]), (y2_t[i], tY2[i]))):
                    def fga(g_, yt=yt, ti=ti, kk=kk):
                        return g_.indirect_dma_start(
                            out=yt[:, :], out_offset=None, in_=Yg,
                            in_offset=bass.IndirectOffsetOnAxis(ap=dest_i[:, ti, kk:kk + 1], axis=0),
                            bounds_check=S.breg, oob_is_err=False)
                    S.dma("pool", fga, [tYg, tDest[ti]], [ty])
                b.stt(xm_t[i][:], y1_t[i][:], wts[:, ti, 0:1], xm_t[i][:], ALU.mult, ALU.add,
                      [tY1[i], tDest[ti], tXt[i]], [tXt[i]])
                b.stt(xm_t[i][:], y2_t[i][:], wts[:, ti, 1:2], xm_t[i][:], ALU.mult, ALU.add,
                      [tY2[i], tDest[ti], tXt[i]], [tXt[i]])
                b.act(jk[:], xm_t[i][:], AF.Square, [tXt[i]], [tJ, tS3], accum_out=sm3[:, 0:1])
                b.ts(sm3[:, 1:2], sm3[:, 0:1], 1.0 / D, EPS, ALU.mult, ALU.add, [tS3], [tS3])
                b.act(sm3[:, 1:2], sm3[:, 1:2], AF.Ln, [tS3], [tS3])
                b.act(sm3[:, 2:3], sm3[:, 1:2], AF.Exp, [tS3], [tS3], scale=-0.5)
                b.stt(o_t[i][:], xm_t[i][:], sm3[:, 2:3], gf[:], ALU.mult, ALU.mult, [tXt[i], tS3, tGf], [tO[i]])
                b.dma("sp", out_d[r0:r0 + 128, :], o_t[i][:], [tO[i]], [])
            S.mark("phase3")
            S.barrier()
            S.emit(final=True)
            DBG_MARKS[:] = S.marks
    return nc


def _t5_bucket(n):
    import math
    max_exact = 16
    nf = np.maximum(n, 1).astype(np.float32)
    large = max_exact + (np.log(nf / max_exact) / math.log(128 / max_exact) * (32 - max_exact)).astype(np.int32)
    large = np.minimum(large, 31)
    return np.where(n < max_exact, n, large)


_NC_CACHE = {}


def _consts():
    ident = np.eye(128, dtype=np.float32)
    s_ = np.arange(128)[:, None] % 64
    t_ = np.arange(64)[None, :]
    triu = (s_ <= t_).astype(np.float32)
    lstrict = (np.arange(128)[:, None] < np.arange(128)[None, :]).astype(np.float32)
    scanmask = np.ones((128, 512), np.float32)
    scanmask[:, ::64] = 0.0
    ebase = np.broadcast_to((np.arange(32) * CAP).astype(np.float32)[None, :], (128, 32)).copy()
    return dict(c_ident=ident, c_triu=triu, c_lstrict=lstrict, c_scanmask=scanmask, c_ebase=ebase)


def make_in_maps(inputs, ncores=8):
    f = lambda a: np.ascontiguousarray(np.asarray(a, dtype=np.float32))
    x = f(inputs["x"])
    bc = lambda v: np.ascontiguousarray(np.broadcast_to(f(v).reshape(1, -1), (128, f(v).size)))
    rel = f(inputs["rel_bias"])
    k_ = np.arange(128)[:, None]
    j_ = np.arange(256)[None, :]
    dist = j_ - k_
    bucket = _t5_bucket(np.clip(dist, 0, None))
    valid = (dist >= 0) & (dist < 128)
    tbl = rel[bucket]
    tbl = np.where(valid[:, :, None], tbl, np.float32(NEG)).astype(np.float32)
    biasT = np.ascontiguousarray(tbl.transpose(0, 2, 1))
    lbl = np.ascontiguousarray(f(inputs["hg_lb_logits"]).reshape(2, 8, 128).transpose(2, 1, 0))
    wr = np.ascontiguousarray(np.concatenate([f(inputs["w_group_router"][0]), f(inputs["w_expert_router"][0])], axis=1))
    bb = np.concatenate([f(inputs["b_group_router"][0]), f(inputs["b_expert_router"][0])])
    shared = dict(
        w_in=f(inputs["w_in"][0]), w_a=f(inputs["w_attn_branch"][0]), w_r=f(inputs["w_hg_branch"][0]),
        w_o=f(inputs["w_out"][0]), w_gate=f(inputs["w_gate"][0]), w_up=f(inputs["w_up"][0]),
        w_down=f(inputs["w_down"][0]), g1_bc=bc(inputs["norm1_g"][0]), g2_bc=bc(inputs["norm2_g"][0]),
        gf_bc=bc(inputs["final_g"]), gn_bc=bc(np.tile(f(inputs["hg_norm_g"][0]), 8)), biasT=biasT,
        sinks_bc=bc(inputs["attn_sinks"][0]), lbl=lbl, wr=wr, b_bc=bc(bb), **_consts())
    maps = []
    for c in range(ncores):
        bi, hf = c // 2, c % 2
        m = dict(shared)
        m["x_own"] = np.ascontiguousarray(x[bi, hf * TOK:(hf + 1) * TOK])
        m["x_pre"] = np.ascontiguousarray(x[bi, 0:TOK]) if hf == 1 else np.zeros((TOK, D), np.float32)
        m["flag0"] = np.full((128, 1), float(hf), np.float32)
        maps.append(m)
    return maps


def kernel(**inputs):
    if "nc" not in _NC_CACHE:
        _NC_CACHE["nc"] = build_program()
    nc = _NC_CACHE["nc"]
    maps = make_in_maps(inputs, 8)
    res = run_bass_kernel_spmd(nc, maps, core_ids=list(range(8)))
    out = np.empty((4, 4096, D), np.float32)
    for c in range(8):
        out[c // 2, (c % 2) * TOK:(c % 2 + 1) * TOK] = res.results[c]["out"]
    return out
```

```python
import numpy as np
from contextlib import ExitStack
import concourse.bass as bass
import concourse.mybir as mybir
from concourse.bass_utils import run_bass_kernel_spmd

F32 = mybir.dt.float32
BF16 = mybir.dt.bfloat16
I32 = mybir.dt.int32
AF = mybir.ActivationFunctionType
ALU = mybir.AluOpType
AX = mybir.AxisListType

SAME_ENG_SYNC = True
D = 2048
KC = 16
TOK = 2048
G = 512
NG = TOK // G
INW = 9728
NE = 32
CAP = 256
EPS = 1e-6
NEG = -30000.0
WPD = 2
DBG_NGP = NG
DBG_NGO = NG
DBG_NEX = NE
DBG_NT3 = 16
DBG_LIMIT = 10 ** 9
DBG_MARKS = []
NRING = 3

O_QA, O_KA, O_VA, O_QH, O_FH, O_IH, O_GH, O_GA, O_GB = 0, 1024, 1280, 1536, 2560, 3584, 4608, 5632, 7680


class T:
    __slots__ = ("name", "w", "r")

    def __init__(self, name):
        self.name = name
        self.w = {}
        self.r = {}


class Sched:
    ENG = ("pe", "act", "dve", "pool", "sp")
    NDMA = 8

    def __init__(self, nc, es):
        self.nc = nc
        self.nrec = 0
        self.marks = []
        self.prog = {k: [] for k in self.ENG}
        self.sems = {}
        self.cnt = {}
        self.seen = {k: {} for k in self.ENG}
        for k in self.ENG:
            self.sems[k] = es.enter_context(nc.semaphore("s_" + k))
            self.cnt[k] = 0
        self.breg = es.enter_context(nc.gpsimd.register("bnd"))
        self.dma_pool = {}
        self.dma_i = {}
        for q in ("sp", "act", "pool"):
            keys = []
            for i in range(self.NDMA):
                k = "d_%s%d" % (q, i)
                self.sems[k] = es.enter_context(nc.semaphore(k))
                self.cnt[k] = 0
                keys.append(k)
            self.dma_pool[q] = keys
            self.dma_i[q] = 0

    def _deps(self, e, reads, writes, merge):
        need = {}
        for t in reads:
            for k, v in t.w.items():
                need[k] = max(need.get(k, 0), v)
        for t in writes:
            if not merge:
                for k, v in t.w.items():
                    need[k] = max(need.get(k, 0), v)
            for k, v in t.r.items():
                need[k] = max(need.get(k, 0), v)
        waits = []
        for k, v in need.items():
            if k == e and (e == "pe" or not SAME_ENG_SYNC):
                continue
            if self.seen[e].get(k, 0) < v:
                self.seen[e][k] = v
                waits.append((k, v))
        return waits

    def mark(self, name):
        self.marks.append((name, self.nrec, dict(self.cnt)))

    def op(self, e, fn, reads=(), writes=()):
        if self.nrec >= DBG_LIMIT:
            return
        self.nrec += 1
        waits = self._deps(e, reads, writes, False)
        self.cnt[e] += 1
        v = self.cnt[e]
        self.prog[e].append((waits, fn, (e, 1)))
        for t in reads:
            t.r[e] = v
        for t in writes:
            t.w = {e: v}
            t.r = {}

    def dma(self, q, fn, reads=(), writes=(), merge=False):
        if self.nrec >= DBG_LIMIT:
            return
        self.nrec += 1
        i = self.dma_i[q]
        self.dma_i[q] += 1
        k = self.dma_pool[q][i % self.NDMA]
        waits = self._deps(q, reads, writes, merge)
        if self.cnt[k] > self.seen[q].get(k, 0):
            waits.append((k, self.cnt[k]))
            self.seen[q][k] = self.cnt[k]
        self.cnt[k] += 16
        v = self.cnt[k]
        self.prog[q].append((waits, fn, (k, 16)))
        for t in reads:
            t.r[k] = v
        for t in writes:
            if merge:
                t.w[k] = v
            else:
                t.w = {k: v}
                t.r = {}

    def barrier(self):
        for e in self.ENG:
            waits = []
            for k, v in self.cnt.items():
                if k != e and v > self.seen[e].get(k, 0):
                    waits.append((k, v))
                    self.seen[e][k] = v
            self.prog[e].append((waits, None, None))

    def emit(self, final=False):
        sems = self.sems
        prog = self.prog
        fin = [(k, v) for k, v in self.cnt.items() if v > 0 and k != "sp"] if final else []

        def mk(e):
            plist = prog[e]

            def body(engobj):
                if e == "pool" and self.breg is not None:
                    engobj.reg_mov(self.breg, NE * CAP - 1)
                for waits, fn, inc in plist:
                    for (wk, wv) in waits:
                        engobj.wait_ge(sems[wk], wv)
                    if fn is not None:
                        ins = fn(engobj)
                        ins.then_inc(sems[inc[0]], inc[1])
                if e == "sp":
                    for (wk, wv) in fin:
                        engobj.wait_ge(sems[wk], wv)
            return body

        with self.nc.Block() as block:
            block.tensor(mk("pe"))
            block.scalar(mk("act"))
            block.vector(mk("dve"))
            block.gpsimd(mk("pool"))
            block.sync(mk("sp"))
        self.prog = {k: [] for k in self.ENG}


class B:
    def __init__(self, S):
        self.S = S
        self.flip = 0

    def mm(self, groups, reads, writes):
        groups = [(o, list(ops)) for o, ops in groups]

        def fn(pe):
            ins = None
            for o, ops in groups:
                n = len(ops)
                for i, (l, r) in enumerate(ops):
                    ins = pe.matmul(o, l, r, start=(i == 0), stop=(i == n - 1))
            return ins
        self.S.op("pe", fn, reads, writes)

    def tr(self, items, reads, writes):
        items = list(items)

        def fn(pe):
            ins = None
            for o, i_, idt in items:
                ins = pe.transpose(o, i_, idt)
            return ins
        self.S.op("pe", fn, reads, writes)

    def act(self, out, in_, func, reads, writes, **kw):
        def fn(e):
            return e.activation(out=out, in_=in_, func=func, **kw)
        self.S.op("act", fn, reads, writes)

    def tt(self, out, in0, in1, op, reads, writes, eng="dve"):
        def fn(e):
            return e.tensor_tensor(out=out, in0=in0, in1=in1, op=op)
        self.S.op(eng, fn, reads, writes)

    def ts(self, out, in0, s1, s2, op0, op1, reads, writes, eng="dve"):
        def fn(e):
            if s2 is None:
                return e.tensor_scalar(out=out, in0=in0, scalar1=s1, scalar2=None, op0=op0)
            return e.tensor_scalar(out=out, in0=in0, scalar1=s1, scalar2=s2, op0=op0, op1=op1)
        self.S.op(eng, fn, reads, writes)

    def stt(self, out, in0, scalar, in1, op0, op1, reads, writes):
        def fn(e):
            return e.scalar_tensor_tensor(out=out, in0=in0, scalar=scalar, in1=in1, op0=op0, op1=op1)
        self.S.op("dve", fn, reads, writes)

    def copy(self, eng, out, in_, reads, writes):
        if eng == "act":
            def fn(e):
                return e.activation(out=out, in_=in_, func=AF.Copy)
        else:
            def fn(e):
                return e.tensor_copy(out=out, in_=in_)
        self.S.op(eng, fn, reads, writes)

    def evac(self, out, in_, reads, writes):
        self.flip ^= 1
        self.copy("act" if self.flip else "dve", out, in_, reads, writes)

    def reduce(self, out, in_, op, reads, writes):
        def fn(e):
            return e.tensor_reduce(out=out, in_=in_, axis=AX.X, op=op)
        self.S.op("dve", fn, reads, writes)

    def recip(self, out, in_, reads, writes):
        def fn(e):
            return e.reciprocal(out=out, in_=in_)
        self.S.op("dve", fn, reads, writes)

    def memset(self, eng, ap, val, writes):
        def fn(e):
            return e.memset(ap, val)
        self.S.op(eng, fn, (), writes)

    def dma(self, q, out, in_, reads, writes, merge=False):
        def fn(e):
            return e.dma_start(out=out, in_=in_)
        self.S.dma(q, fn, reads, writes, merge)

    def scan(self, out, d0, d1, reads, writes):
        def fn(e):
            return e.tensor_tensor_scan(out=out, data0=d0, data1=d1, initial=0.0, op0=ALU.mult, op1=ALU.add)
        self.S.op("dve", fn, reads, writes)


def build_program(dbg=False):
    nc = bass.Bass("TRN2", target_bir_lowering=False)

    def din(name, shape, dt=F32):
        return nc.dram_tensor(name, shape, dt, kind="ExternalInput").ap()

    x_own = din("x_own", [TOK, D])
    x_pre = din("x_pre", [TOK, D])
    w_in = din("w_in", [D, INW])
    w_a = din("w_a", [1024, D])
    w_r = din("w_r", [1024, D])
    w_o = din("w_o", [D, D])
    w_gate = din("w_gate", [NE, D, 512])
    w_up = din("w_up", [NE, D, 512])
    w_down = din("w_down", [NE, 512, D])
    g1_bc = din("g1_bc", [128, D])
    g2_bc = din("g2_bc", [128, D])
    gf_bc = din("gf_bc", [128, D])
    gn_in = din("gn_bc", [128, 1024])
    bias_in = din("biasT", [128, 16, 256])
    flag_in = din("flag0", [128, 1])
    sinks_in = din("sinks_bc", [128, 16])
    lbl_in = din("lbl", [128, 8, 2])
    wr_in = din("wr", [D, 36])
    bb_in = din("b_bc", [128, 36])
    c_ident = din("c_ident", [128, 128])
    c_triu = din("c_triu", [128, 64])
    c_lstrict = din("c_lstrict", [128, 128])
    c_scanmask = din("c_scanmask", [128, 512])
    c_ebase = din("c_ebase", [128, 32])
    out_d = nc.dram_tensor("out", [TOK, D], F32, kind="ExternalOutput").ap()
    if dbg:
        xmid = nc.dram_tensor("xmid", [TOK, D], F32, kind="ExternalOutput").ap()
    else:
        xmid = nc.dram_tensor("xmid", [TOK, D], F32).ap()
    Xg = nc.dram_tensor("Xg", [NE * CAP, D], BF16).ap()
    Yg = nc.dram_tensor("Yg", [NE * CAP, D], F32).ap()
    tXm = [T("xm%d" % i) for i in range(16)]
    tXg = T("Xg")
    tYg = T("Yg")

    w_in_v = w_in.rearrange("(kc p) n -> p kc n", p=128)
    w_a_v = w_a.rearrange("(kc p) n -> p kc n", p=128)
    w_r_v = w_r.rearrange("(kc p) n -> p kc n", p=128)
    w_o_v = w_o.rearrange("(kc p) n -> p kc n", p=128)

    with ExitStack() as ea:
        S = Sched(nc, ea)
        b = B(S)

        def sb(es, name, shape, dt):
            return es.enter_context(nc.sbuf_tensor("sb_" + name, shape, dt))

        ident_bf = sb(ea, "ident_bf", [128, 128], BF16)
        ident_f = sb(ea, "ident_f", [128, 128], F32)
        triu = sb(ea, "triu", [128, 64], BF16)
        lstrict = sb(ea, "lstrict", [128, 128], BF16)
        ones_bf = sb(ea, "ones_bf", [128, 128], BF16)
        ebase = sb(ea, "ebase", [128, 32], F32)
        neghalf = sb(ea, "neghalf", [128, 1], F32)
        dest_i = sb(ea, "dest_i", [128, 16, 2], I32)
        wts = sb(ea, "wts", [128, 16, 2], F32)
        tC = T("consts")
        tDest = [T("dest%d" % i) for i in range(16)]
        ps = []
        tP = []
        for i in range(6):
            ps.append(ea.enter_context(nc.psum_tensor("ps%d" % i, [128, 512], F32)))
            tP.append(T("ps%d" % i))
        pb = []
        tPb = []
        for i in range(2):
            pb.append(ea.enter_context(nc.psum_tensor("pb%d" % i, [128, 8, 128], BF16)))
            tPb.append(T("pb%d" % i))

        b.dma("pool", ident_bf[:], c_ident, (), [tC])
        b.dma("sp", ident_f[:], c_ident, (), [tC], merge=True)
        b.dma("pool", triu[:], c_triu, (), [tC], merge=True)
        b.dma("pool", lstrict[:], c_lstrict, (), [tC], merge=True)
        b.dma("sp", ebase[:], c_ebase, (), [tC], merge=True)
        tC2 = T("consts2")
        b.memset("pool", ones_bf[:], 1.0, [tC2])
        b.memset("pool", neghalf[:], -0.5, [tC2])

        with ExitStack() as e1:
            scanmask = sb(e1, "scanmask", [128, 512], F32)
            biasT = sb(e1, "biasT", [128, 16, 256], F32)
            gn_bc = sb(e1, "gn_bc", [128, 1024], F32)
            wr_sb = sb(e1, "wr_sb", [128, 16, 36], F32)
            b_bc = sb(e1, "b_bc", [128, 36], F32)
            esink = sb(e1, "esink", [128, 16], F32)
            flag0 = sb(e1, "flag0", [128, 1], F32)
            lbl = sb(e1, "lbl", [128, 8, 2], F32)
            lb = sb(e1, "lb", [128, 8], F32)
            oml = sb(e1, "oml", [128, 8], F32)
            ln_oml = sb(e1, "ln_oml", [128, 8], F32)
            macc = sb(e1, "macc", [128, 32], BF16)
            tK = T("consts_p1")
            b.dma("sp", scanmask[:], c_scanmask, (), [tK])
            b.dma("sp", biasT[:], bias_in, (), [tK], merge=True)
            b.dma("sp", gn_bc[:], gn_in, (), [tK], merge=True)
            b.dma("sp", wr_sb[:], wr_in.rearrange("(kc p) n -> p kc n", p=128), (), [tK], merge=True)
            b.dma("sp", b_bc[:], bb_in, (), [tK], merge=True)
            b.dma("sp", esink[:], sinks_in, (), [tK], merge=True)
            b.dma("sp", flag0[:], flag_in, (), [tK], merge=True)
            b.dma("sp", lbl[:], lbl_in, (), [tK], merge=True)
            tL = T("lb")
            b.tt(lb[:], lbl[:, :, 1], lbl[:, :, 0], ALU.subtract, [tK], [tL])
            b.act(lb[:], lb[:], AF.Exp, [tL], [tL])
            b.ts(lb[:], lb[:], 1.0, None, ALU.add, None, [tL], [tL])
            b.recip(lb[:], lb[:], [tL], [tL])
            b.ts(oml[:], lb[:], -1.0, 1.0, ALU.mult, ALU.add, [tL], [tL])
            b.act(ln_oml[:], oml[:], AF.Ln, [tL], [tL])
            tEs = T("esink")
            b.act(esink[:], esink[:], AF.Exp, [tK], [tEs])
            tMacc = T("macc")
            b.memset("pool", macc[:], 0.0, [tMacc])

            NR = NRING
            wring = [sb(e1, "wring%d" % i, [128, 4096], BF16) for i in range(NR)]
            tW = [T("wring%d" % i) for i in range(NR)]
            hT = sb(e1, "hT", [128, KC, G], BF16)
            tHT = T("hT")
            qaT = sb(e1, "qaT", [128, 8, G], BF16)
            tQA = [T("qa%d" % i) for i in range(4)]
            kbuf = sb(e1, "kbuf", [128, 4, 128 + G], BF16)
            tKb = T("kbuf")
            vbuf = sb(e1, "vbuf", [128, 5, 4, 65], BF16)
            tVb = T("vbuf")
            QtT = sb(e1, "QtT", [128, 8, G], BF16)
            tQt = [T("Qt%d" % i) for i in range(4)]
            KtT = sb(e1, "KtT", [128, 8, G], BF16)
            tKt = T("KtT")
            vh = sb(e1, "vh", [128, 4, 1024], BF16)
            tVh = T("vh")
            gsg = sb(e1, "gsg", [128, 4, 1024], BF16)
            tGs = T("gsg")
            sgA = sb(e1, "sgA", [128, 16, G], BF16)
            sgH = sb(e1, "sgH", [128, 16, G], BF16)
            tSgA = T("sgA")
            tSgH = T("sgH")
            S32 = sb(e1, "S32", [128, 8, 128], F32)
            Sbf = sb(e1, "Sbf", [128, 8, 128], BF16)
            tS32 = T("S32")
            tSbf = T("Sbf")
            dec = sb(e1, "dec", [128, 8, 8], F32)
            tDec = T("dec")
            xt0 = sb(e1, "xt0", [128, D], F32)
            xt1 = sb(e1, "xt1", [128, D], F32)
            gbc = sb(e1, "gbc", [128, D], F32)
            hb = sb(e1, "hb", [128, D], BF16)
            tX0, tX1, tGbc, tHb = T("xt0"), T("xt1"), T("gbc"), T("hb")
            arena = sb(e1, "arena", [128, 6 * 512], F32)
            tA = [T("ar%d" % i) for i in range(6)]
            small = sb(e1, "small", [128, 64], F32)
            tSm = T("small")
            smallr = sb(e1, "smallr", [128, 160], F32)
            tSr = T("smallr")

            def ar(i, n=1):
                return arena[:, i * 512:(i + n) * 512]

            b.memset("pool", S32[:], 0.0, [tS32])
            b.memset("pool", Sbf[:], 0.0, [tSbf])
            b.memset("pool", vbuf[:], 1.0, [tVb])
            b.memset("pool", kbuf[:], 0.0, [tKb])

            b.memset("pool", hb[:], 0.0, [tHb])
            for i in range(NE * CAP // 128):
                b.dma("act", Xg[i * 128:(i + 1) * 128, :], hb[:], [tHb], [tXg], merge=True)

            wlist = []

            def wq_win(col0):
                wlist.append((w_in_v[:, :, col0:col0 + 256], 16, 256))
                return len(wlist) - 1
            wstate = {"issued": 0}

            def wissue(upto):
                while wstate["issued"] <= min(upto, len(wlist) - 1):
                    i = wstate["issued"]
                    src, kc, n = wlist[i]
                    slot = wring[i % NR][:, 0:kc * n].rearrange("p (k n) -> p k n", k=kc)
                    b.dma("pool", slot, src, (), [tW[i % NR]])
                    wstate["issued"] += 1

            def wget(i):
                wissue(i + WPD)
                src, kc, n = wlist[i]
                return wring[i % NR][:, 0:kc * n].rearrange("p (k n) -> p k n", k=kc), tW[i % NR]

            pstate = {"i": 0}

            def nextps():
                pstate["i"] ^= 1
                return ps[pstate["i"]], tP[pstate["i"]]

            def rms_tile(xt, tXt, gtile, tG, out, tOut, junk, tJunk):
                b.act(junk, xt, AF.Square, [tXt], [tJunk, tSm], accum_out=small[:, 0:1])
                b.ts(small[:, 1:2], small[:, 0:1], 1.0 / D, EPS, ALU.mult, ALU.add, [tSm], [tSm])
                b.act(small[:, 1:2], small[:, 1:2], AF.Ln, [tSm], [tSm])
                b.act(small[:, 2:3], small[:, 1:2], AF.Exp, [tSm], [tSm], scale=-0.5)
                b.stt(out, xt, small[:, 2:3], gtile, ALU.mult, ALU.mult, [tXt, tSm, tG, tJunk], [tOut])

            def stage_norm(xsrc, grp):
                b.dma("sp", gbc[:], g1_bc, (), [tGbc])
                for tt in range(4):
                    xt, tXt = (xt0, tX0) if tt % 2 == 0 else (xt1, tX1)
                    r0 = grp * G + tt * 128
                    b.dma("sp", xt[:], xsrc[r0:r0 + 128, :], (), [tXt])
                    rms_tile(xt[:], tXt, gbc[:], tGbc, hb[:], tHb, hb[:], tHb)
                    for rd in range(2):
                        p, tp = pb[rd], tPb[rd]
                        b.tr([(p[:, j, :], hb[:, (rd * 8 + j) * 128:(rd * 8 + j + 1) * 128], ident_bf[:])
                              for j in range(8)], [tHb, tC], [tp])
                        b.evac(hT[:, rd * 8:rd * 8 + 8, tt * 128:(tt + 1) * 128], p[:], [tp], [tHT])

            def proj_fm(slot, tSlot, c):
                p, tp = nextps()
                b.mm([(p[:], [(slot[:, kc, c * 128:(c + 1) * 128], hT[:, kc, :]) for kc in range(KC)])],
                     [tSlot, tHT], [tp])
                return p, tp

            def proj_tm(slot, tSlot, tt, n):
                p, tp = nextps()
                b.mm([(p[:, 0:n], [(hT[:, kc, tt * 128:(tt + 1) * 128], slot[:, kc, 0:n]) for kc in range(KC)])],
                     [tSlot, tHT], [tp])
                return p, tp

            def chain(h, p, tp, own):
                E, L1, L2, bb_ = ar(0), ar(1), ar(2), ar(3)
                b.act(E, p[:], AF.Exp, [tp], [tA[0]])
                b.act(L1, E, AF.Ln, [tA[0], tL], [tA[1]], bias=lb[:, h:h + 1])
                b.act(L2, E, AF.Ln, [tA[0]], [tA[2]], bias=1.0)
                b.tt(L1, L1, L2, ALU.subtract, [tA[1], tA[2]], [tA[1]])
                b.scan(bb_, scanmask[:], L1, [tK, tA[1]], [tA[3]])
                b.act(E, bb_, AF.Exp, [tA[3]], [tA[0]])
                if own:
                    b.tt(QtT[:, h, :], QtT[:, h, :], E, ALU.mult, [tA[0]] + tQt, tQt)
                b.tt(L2, L2, bb_, ALU.add, [tA[2], tA[3]], [tA[2]])
                b.act(KtT[:, h, :], L2, AF.Exp, [tA[2], tL], [tKt], scale=-1.0, bias=ln_oml[:, h:h + 1])
                b.copy("pool", dec[:, h, :], E.rearrange("p (c t) -> p c t", t=64)[:, :, 63], [tA[0]], [tDec])

            def stage_proj(own, last_prefix, wbase):
                wi = wbase
                if own:
                    for blk in range(4):
                        slot, tsl = wget(wi); wi += 1
                        for c in range(2):
                            p, tp = proj_fm(slot, tsl, c)
                            b.act(qaT[:, blk * 2 + c, :], p[:], AF.Copy, [tp], tQA, scale=0.125)
                if own or last_prefix:
                    slot, tsl = wget(wi); wi += 1
                    for g in range(4):
                        p, tp = nextps()
                        b.mm([(p[0:64, :], [(slot[:, kc, g * 64:(g + 1) * 64], hT[:, kc, :]) for kc in range(KC)]),
                              (p[64:128, :], [(slot[:, kc, g * 64:(g + 1) * 64], hT[:, kc, :]) for kc in range(KC)])],
                             [tsl, tHT], [tp])
                        b.evac(kbuf[:, g, 128:128 + G], p[:], [tp], [tKb])
                    slot, tsl = wget(wi); wi += 1
                    for tt in range(4):
                        p, tp = proj_tm(slot, tsl, tt, 256)
                        b.evac(vbuf[:, 1 + tt, :, 0:64], p[:, 0:256].rearrange("p (g d) -> p g d", g=4), [tp], [tVb])
                if own:
                    for blk in range(4):
                        slot, tsl = wget(wi); wi += 1
                        for c in range(2):
                            p, tp = proj_fm(slot, tsl, c)
                            b.evac(QtT[:, blk * 2 + c, :], p[:], [tp], tQt)
                for blk in range(4):
                    slot, tsl = wget(wi); wi += 1
                    for c in range(2):
                        p, tp = proj_fm(slot, tsl, c)
                        chain(blk * 2 + c, p, tp, own)
                for blk in range(4):
                    slot, tsl = wget(wi); wi += 1
                    for tt in range(4):
                        p, tp = proj_tm(slot, tsl, tt, 256)
                        b.evac(vh[:, tt, blk * 256:(blk + 1) * 256], p[:, 0:256], [tp], [tVh])
                if own:
                    for blk in range(4):
                        slot, tsl = wget(wi); wi += 1
                        for tt in range(4):
                            p, tp = proj_tm(slot, tsl, tt, 256)
                            tmp = ar(4)[:, 0:256]
                            b.act(tmp, p[:, 0:256], AF.Silu, [tp], [tA[4]])
                            b.tt(gsg[:, tt, blk * 256:(blk + 1) * 256], tmp, gn_bc[:, blk * 256:(blk + 1) * 256],
                                 ALU.mult, [tA[4], tK], [tGs])
                    for blk in range(8):
                        slot, tsl = wget(wi); wi += 1
                        for c in range(2):
                            p, tp = proj_fm(slot, tsl, c)
                            b.act(sgA[:, blk * 2 + c, :], p[:], AF.Sigmoid, [tp], [tSgA])
                    for blk in range(8):
                        slot, tsl = wget(wi); wi += 1
                        for c in range(2):
                            p, tp = proj_fm(slot, tsl, c)
                            b.act(sgH[:, blk * 2 + c, :], p[:], AF.Sigmoid, [tp], [tSgH])
                return wi

            def stage_attn(first_group):
                sE = ar(0, 2).rearrange("p (a n) -> p a n", a=2)
                pT = ar(2).bitcast(BF16).rearrange("p (a h q) -> p a h q", a=2, h=4)
                a_tok = ar(3).bitcast(BF16)
                for tt in range(4):
                    for g in range(4):
                        banks = [[(ps[2], tP[2]), (ps[3], tP[3])], [(ps[0], tP[0]), (ps[1], tP[1])]]
                        sE4 = sE.rearrange("p a (j two q) -> p a j two q", j=2, two=2)
                        for kb in range(2):
                            for par in range(2):
                                pbk, tpbk = banks[kb][par]
                                p0 = par * 64
                                groups = []
                                for jj in range(2):
                                    h = g * 4 + jj * 2 + par
                                    groups.append((pbk[:, jj * 128:(jj + 1) * 128],
                                                   [(kbuf[p0:p0 + 64, g, (tt + kb) * 128:(tt + kb + 1) * 128],
                                                     qaT[p0:p0 + 64, h // 2, tt * 128:(tt + 1) * 128])]))
                                b.mm(groups, [tKb, tQA[tt]], [tpbk])
                                bcol = slice(128, 256) if kb == 0 else slice(0, 128)
                                bsl = biasT[:, g * 4:(g + 1) * 4, bcol].rearrange("p (j two) q -> p j two q", two=2)[:, :, par, :]
                                b.stt(sE4[:, kb, :, par, :], pbk[:, 0:256].rearrange("p (j q) -> p j q", j=2), 60.0, bsl,
                                      ALU.min, ALU.add, [tpbk, tK], [tA[kb]])
                        b.act(pT, sE.rearrange("p a (h q) -> p a h q", h=4), AF.Exp, [tA[0], tA[1]], [tA[2]])
                        if first_group and tt == 0:
                            b.ts(pT[:, 0], pT[:, 0], flag0[:, 0:1], None, ALU.mult, None, [tA[2], tK], [tA[2]])
                        po, tpo = ps[4], tP[4]
                        groups = []
                        for hh in range(4):
                            groups.append((po[:, hh * 65:(hh + 1) * 65],
                                           [(pT[:, kb, hh, :], vbuf[:, tt + kb, g, :]) for kb in range(2)]))
                        b.mm(groups, [tA[2], tVb], [tpo])
                        pov = po[:, 0:260].rearrange("p (h d) -> p h d", h=4)
                        b.tt(smallr[:, 0:4], pov[:, :, 64], esink[:, g * 4:(g + 1) * 4], ALU.add, [tpo, tEs], [tSr])
                        b.recip(smallr[:, 4:8], smallr[:, 0:4], [tSr], [tSr])
                        b.tt(a_tok[:, g * 256:(g + 1) * 256].rearrange("p (h d) -> p h d", h=4), pov[:, :, 0:64],
                             smallr[:, 4:8].unsqueeze(2).broadcast_to([128, 4, 64]), ALU.mult, [tpo, tSr], [tA[3]])
                    p, tp = pb[tt % 2], tPb[tt % 2]
                    b.tr([(p[:, j, :], a_tok[:, j * 128:(j + 1) * 128], ident_bf[:]) for j in range(8)],
                         [tA[3], tC], [tp])
                    b.evac(qaT[:, :, tt * 128:(tt + 1) * 128], p[:], [tp], [tQA[tt]])
                b.copy("pool", kbuf[:, :, 0:128], kbuf[:, :, G:G + 128], [tKb], [tKb])
                b.copy("pool", vbuf[:, 0], vbuf[:, 4], [tVb], [tVb])

            def stage_hloop(own):
                attm = ar(0).bitcast(BF16)[:, 0:512].rearrange("p (h t) -> p h t", h=8)
                r_tok = ar(1).bitcast(BF16)
                junk = ar(2).bitcast(BF16)
                Kt = ar(3).bitcast(BF16).rearrange("p (h k) -> p h k", h=8)
                for tt in range(4):
                    p, tp = pb[tt % 2], tPb[tt % 2]
                    b.tr([(p[:, h, :], KtT[:, h, tt * 128:(tt + 1) * 128], ident_bf[:]) for h in range(8)],
                         [tKt, tC], [tp])
                    b.evac(Kt, p[:], [tp], [tA[3]])
                    for cc in range(2):
                        c = tt * 2 + cc
                        p0 = cc * 64
                        cs = slice(c * 64, (c + 1) * 64)
                        if own:
                            pa, tpa = ps[4], tP[4]
                            b.mm([(pa[p0:p0 + 64, h * 64:(h + 1) * 64], [(KtT[:, h, cs], QtT[:, h, cs])])
                                  for h in range(8)], [tKt, tQt[tt]], [tpa])
                            b.tt(attm[p0:p0 + 64], pa[p0:p0 + 64, :].rearrange("p (h t) -> p h t", h=8),
                                 triu[p0:p0 + 64, :].unsqueeze(1).broadcast_to([64, 8, 64]), ALU.mult,
                                 [tpa, tC], [tA[0]])
                            groups = []
                            for h in range(8):
                                po = ps[2] if h < 4 else ps[3]
                                o_ap = po[p0:p0 + 64, (h % 4) * 128:(h % 4 + 1) * 128]
                                groups.append((o_ap, [(QtT[:, h, cs], Sbf[:, h, :]),
                                                      (attm[p0:p0 + 64, h, :], vh[p0:p0 + 64, tt, h * 128:(h + 1) * 128])]))
                            b.mm(groups, [tQt[tt], tSbf, tA[0], tVh], [tP[2], tP[3]])
                        groups = []
                        for h in range(8):
                            po = ps[0] if h < 4 else ps[1]
                            groups.append((po[:, (h % 4) * 128:(h % 4 + 1) * 128],
                                           [(Kt[p0:p0 + 64, h, :], vh[p0:p0 + 64, tt, h * 128:(h + 1) * 128])]))
                        b.mm(groups, [tA[3], tVh], [tP[0], tP[1]])
                        b.tt(S32[:, 0:4, :], ps[0][:].rearrange("p (h v) -> p h v", h=4), S32[:, 0:4, :], ALU.add,
                             [tP[0], tS32], [tS32])
                        b.tt(S32[:, 4:8, :], ps[1][:].rearrange("p (h v) -> p h v", h=4), S32[:, 4:8, :], ALU.add,
                             [tP[1], tS32], [tS32])
                        b.tt(S32[:], S32[:], dec[:, :, c].unsqueeze(2).broadcast_to([128, 8, 128]), ALU.mult,
                             [tS32, tDec], [tS32])
                        b.copy("act", Sbf[:], S32[:], [tS32], [tSbf])
                    if own:
                        for h in range(8):
                            po = ps[2] if h < 4 else ps[3]
                            b.act(junk[:, 0:128], po[:, (h % 4) * 128:(h % 4 + 1) * 128], AF.Square,
                                  [tP[2], tP[3]], [tA[2], tSr], accum_out=smallr[:, 16 + h:17 + h])
                        b.ts(smallr[:, 32:40], smallr[:, 16:24], 1.0 / 128, EPS, ALU.mult, ALU.add, [tSr], [tSr])
                        b.act(smallr[:, 32:40], smallr[:, 32:40], AF.Ln, [tSr], [tSr])
                        b.act(smallr[:, 40:48], smallr[:, 32:40], AF.Exp, [tSr], [tSr], scale=-0.5)
                        for h in range(8):
                            po = ps[2] if h < 4 else ps[3]
                            b.stt(r_tok[:, h * 128:(h + 1) * 128], po[:, (h % 4) * 128:(h % 4 + 1) * 128],
                                  smallr[:, 40 + h:41 + h], gsg[:, tt, h * 128:(h + 1) * 128], ALU.mult, ALU.mult,
                                  [tP[2], tP[3], tSr, tGs], [tA[1]])
                        p, tp = pb[(tt + 1) % 2], tPb[(tt + 1) % 2]
                        b.tr([(p[:, j, :], r_tok[:, j * 128:(j + 1) * 128], ident_bf[:]) for j in range(8)],
                             [tA[1], tC], [tp])
                        b.evac(QtT[:, :, tt * 128:(tt + 1) * 128], p[:], [tp], [tQt[tt]])

            def stage_branch(wi):
                m2 = ar(1)
                for nb in range(4):
                    sa, tsa = wget(wi); wi += 1
                    for c in range(4):
                        j = nb * 4 + c
                        p, tp = ps[2 + c % 2], tP[2 + c % 2]
                        b.mm([(p[:], [(sa[:, kc, c * 128:(c + 1) * 128], qaT[:, kc, :]) for kc in range(8)])],
                             [tsa] + tQA, [tp])
                        b.tt(ar(2 + c), p[:], sgA[:, j, :], ALU.mult, [tp, tSgA], [tA[2 + c]])
                    sr, tsr = wget(wi); wi += 1
                    for c in range(4):
                        j = nb * 4 + c
                        p, tp = ps[2 + c % 2], tP[2 + c % 2]
                        b.mm([(p[:], [(sr[:, kc, c * 128:(c + 1) * 128], QtT[:, kc, :]) for kc in range(8)])],
                             [tsr] + tQt, [tp])
                        b.tt(m2, p[:], sgH[:, j, :], ALU.mult, [tp, tSgH], [tA[1]])
                        b.tt(hT[:, j, :], ar(2 + c), m2, ALU.add, [tA[2 + c], tA[1]], [tHT])
                return wi

            def stage_out(wi, grp):
                xs = [xt0[:, 0:1024].rearrange("p (t c) -> p t c", t=4), xt0[:, 1024:2048].rearrange("p (t c) -> p t c", t=4)]
                os_ = [xt1[:, 0:1024].rearrange("p (t c) -> p t c", t=4), xt1[:, 1024:2048].rearrange("p (t c) -> p t c", t=4)]
                txs = [tA[2], tA[3]]
                tos = [tA[4], tA[5]]
                rows = slice(grp * G, (grp + 1) * G)

                def xload(nb):
                    b.dma("sp", xs[nb % 2], x_own[rows, nb * 256:(nb + 1) * 256].rearrange("(t p) c -> p t c", p=128),
                          [], [txs[nb % 2], tX0])
                xload(0)
                for nb in range(8):
                    so, tso = wget(wi); wi += 1
                    if nb + 1 < 8:
                        xload(nb + 1)
                    for tt in range(4):
                        p, tp = nextps()
                        b.mm([(p[:, 0:256], [(hT[:, kc, tt * 128:(tt + 1) * 128], so[:, kc, :]) for kc in range(KC)])],
                             [tso, tHT], [tp])
                        b.tt(os_[nb % 2][:, tt, :], p[:, 0:256], xs[nb % 2][:, tt, :], ALU.add,
                             [tp, txs[nb % 2]], [tos[nb % 2], tX1])
                    b.dma("sp", xmid[rows, nb * 256:(nb + 1) * 256].rearrange("(t p) c -> p t c", p=128), os_[nb % 2],
                          [tos[nb % 2]], [tXm[grp * 4 + t_] for t_ in range(4)], merge=True)
                return wi

            def stage_route(grp):
                b.dma("sp", gbc[:], g2_bc, (), [tGbc])
                for tt in range(4):
                    ti = grp * 4 + tt
                    r0 = ti * 128
                    b.dma("sp", xt0[:], xmid[r0:r0 + 128, :], [tXm[ti]], [tX0])
                    rms_tile(xt0[:], tX0, gbc[:], tGbc, xt1[:], tX1, hb[:], tHb)
                    b.copy("pool", hb[:], xt1[:], [tX1], [tHb])
                    pr, tpr = ps[5], tP[5]
                    for rd in range(4):
                        p, tp = ps[2 + rd % 2], tP[2 + rd % 2]
                        h2T, th2T = (ar(0), tA[0]) if rd % 2 == 0 else (ar(1), tA[1])
                        b.tr([(p[:, j * 128:(j + 1) * 128], xt1[:, (rd * 4 + j) * 128:(rd * 4 + j + 1) * 128], ident_f[:])
                              for j in range(4)], [tX1, tC], [tp])
                        b.evac(h2T, p[:], [tp], [th2T])
                        h2v = h2T.rearrange("p (j t) -> p j t", j=4)

                        def fn(pe, h2v=h2v, rd=rd, pr=pr):
                            ins = None
                            for j in range(4):
                                kc = rd * 4 + j
                                ins = pe.matmul(pr[:, 0:36], h2v[:, j, :], wr_sb[:, kc, :], start=(kc == 0), stop=(kc == 15))
                            return ins
                        S.op("pe", fn, [th2T, tK], [tpr])
                    sm = smallr
                    LG = sm[:, 48:84]
                    b.tt(LG, pr[:, 0:36], b_bc[:], ALU.add, [tpr, tK], [tSr])
                    gl = sm[:, 48:52]
                    el = sm[:, 52:84].rearrange("p (g e) -> p g e", g=4)
                    gmax, ngmax, gsum, gw = sm[:, 84:85], sm[:, 85:86], sm[:, 86:87], sm[:, 87:88]
                    goh = sm[:, 88:92]
                    b.reduce(gmax, gl, ALU.max, [tSr], [tSr])
                    b.ts(goh, gl, gmax, None, ALU.is_equal, None, [tSr], [tSr])
                    b.ts(ngmax, gmax, -1.0, None, ALU.mult, None, [tSr], [tSr])
                    b.act(sm[:, 92:96], gl, AF.Exp, [tSr], [tSr], bias=ngmax, accum_out=gsum)
                    b.recip(gw, gsum, [tSr], [tSr])
                    tmp32 = sm[:, 96:128].rearrange("p (g e) -> p g e", g=4)
                    b.tt(tmp32, el, goh.unsqueeze(2).broadcast_to([128, 4, 8]), ALU.mult, [tSr], [tSr])
                    esel = sm[:, 128:136]
                    b.reduce(esel, tmp32.rearrange("p g e -> p e g"), ALU.add, [tSr], [tSr])
                    top8 = sm[:, 136:144]

                    def fmax(e, top8=top8, esel=esel):
                        return e.max(out=top8, in_=esel)
                    S.op("dve", fmax, [tSr], [tSr])
                    oh1, oh2 = sm[:, 144:152], sm[:, 152:160]
                    b.ts(oh1, esel, top8[:, 0:1], None, ALU.is_equal, None, [tSr], [tSr])
                    b.ts(oh2, esel, top8[:, 1:2], None, ALU.is_equal, None, [tSr], [tSr])
                    s2 = small
                    b.ts(s2[:, 8:9], top8[:, 0:1], -1.0, None, ALU.mult, None, [tSr], [tSm])
                    b.act(s2[:, 9:10], top8[:, 1:2], AF.Exp, [tSr, tSm], [tSm], bias=s2[:, 8:9])
                    b.ts(s2[:, 10:11], s2[:, 9:10], 1.0, None, ALU.add, None, [tSm], [tSm])
                    b.recip(s2[:, 10:11], s2[:, 10:11], [tSm], [tSm])
                    b.tt(s2[:, 11:12], s2[:, 10:11], gw, ALU.mult, [tSm, tSr], [tSm])
                    b.tt(s2[:, 12:13], s2[:, 11:12], s2[:, 9:10], ALU.mult, [tSm], [tSm])
                    M1 = ar(2)[:, 0:32].rearrange("p (g e) -> p g e", g=4)
                    M2 = ar(2)[:, 32:64].rearrange("p (g e) -> p g e", g=4)
                    gohb = goh.unsqueeze(2).broadcast_to([128, 4, 8])
                    b.tt(M1, gohb, oh1.unsqueeze(1).broadcast_to([128, 4, 8]), ALU.mult, [tSr], [tA[2]])
                    b.tt(M2, gohb, oh2.unsqueeze(1).broadcast_to([128, 4, 8]), ALU.mult, [tSr], [tA[2]])
                    Mb = ar(3).bitcast(BF16)[:, 0:32]
                    b.tt(Mb, ar(2)[:, 0:32], ar(2)[:, 32:64], ALU.add, [tA[2]], [tA[3]])
                    pp, tpp = ps[4], tP[4]
                    b.mm([(pp[:, 0:32], [(lstrict[:], Mb), (ones_bf[:], macc[:])])], [tA[3], tC, tC2, tMacc], [tpp])
                    b.tt(macc[:], macc[:], Mb, ALU.add, [tA[3], tMacc], [tMacc], eng="pool")
                    pos = ar(2)[:, 64:96]
                    b.copy("dve", pos, pp[:, 0:32], [tpp], [tA[2]])
                    tm = ar(2)[:, 96:128]
                    for kk, Mk in enumerate((ar(2)[:, 0:32], ar(2)[:, 32:64])):
                        pk, ek, okf, dk = (s2[:, 16 + 8 * kk + i:17 + 8 * kk + i] for i in range(4))
                        b.tt(tm, pos, Mk, ALU.mult, [tA[2]], [tA[2]])
                        b.reduce(pk, tm, ALU.add, [tA[2]], [tSm])
                        b.tt(tm, ebase[:], Mk, ALU.mult, [tA[2], tC], [tA[2]])
                        b.reduce(ek, tm, ALU.add, [tA[2]], [tSm])
                        b.ts(okf, pk, float(CAP), None, ALU.is_lt, None, [tSm], [tSm])
                        b.ts(dk, okf, -1.0e6, 1.0e6, ALU.mult, ALU.add, [tSm], [tSm])
                        b.tt(dk, dk, pk, ALU.add, [tSm], [tSm])
                        b.tt(dk, dk, ek, ALU.add, [tSm], [tSm])
                        b.copy("dve", dest_i[:, ti, kk:kk + 1], dk, [tSm], [tDest[ti]])
                        b.tt(wts[:, ti, kk:kk + 1], s2[:, 11 + kk:12 + kk], okf, ALU.mult, [tSm], [tDest[ti]])
                    for kk in range(2):
                        def fsc(g_, ti=ti, kk=kk):
                            return g_.indirect_dma_start(
                                out=Xg, out_offset=bass.IndirectOffsetOnAxis(ap=dest_i[:, ti, kk:kk + 1], axis=0),
                                in_=hb[:, :], in_offset=None, bounds_check=S.breg, oob_is_err=False)
                        S.dma("pool", fsc, [tHb, tDest[ti]], [tXg], merge=True)

            plan = []
            for pg in range(NG):
                base = len(wlist)
                if pg == NG - 1:
                    wq_win(O_KA); wq_win(O_VA)
                for blk in range(4):
                    wq_win(O_FH + blk * 256)
                for blk in range(4):
                    wq_win(O_IH + blk * 256)
                plan.append(base)
            for og in range(NG):
                base = len(wlist)
                for blk in range(4):
                    wq_win(O_QA + blk * 256)
                wq_win(O_KA); wq_win(O_VA)
                for sec in (O_QH, O_FH, O_IH, O_GH):
                    for blk in range(4):
                        wq_win(sec + blk * 256)
                for sec in (O_GA, O_GB):
                    for blk in range(8):
                        wq_win(sec + blk * 256)
                for nb in range(4):
                    wlist.append((w_a_v[:, :, nb * 512:(nb + 1) * 512], 8, 512))
                    wlist.append((w_r_v[:, :, nb * 512:(nb + 1) * 512], 8, 512))
                for nb in range(8):
                    wlist.append((w_o_v[:, :, nb * 256:(nb + 1) * 256], 16, 256))
                plan.append(base)

            S.mark("init")
            for pg in range(NG - DBG_NGP, NG):
                stage_norm(x_pre, pg)
                S.mark("pnorm%d" % pg)
                stage_proj(False, pg == NG - 1, plan[pg])
                S.mark("pproj%d" % pg)
                stage_hloop(False)
                S.mark("phloop%d" % pg)
            b.copy("pool", kbuf[:, :, 0:128], kbuf[:, :, G:G + 128], [tKb], [tKb])
            b.copy("pool", vbuf[:, 0], vbuf[:, 4], [tVb], [tVb])
            for og in range(DBG_NGO):
                stage_norm(x_own, og)
                S.mark("norm%d" % og)
                wi = stage_proj(True, False, plan[NG + og])
                S.mark("proj%d" % og)
                stage_attn(og == 0)
                S.mark("attn%d" % og)
                stage_hloop(True)
                S.mark("hloop%d" % og)
                wi = stage_branch(wi)
                S.mark("branch%d" % og)
                wi = stage_out(wi, og)
                S.mark("out%d" % og)
                stage_route(og)
                S.mark("route%d" % og)
            S.mark("phase1")
            S.barrier()
            S.emit(final=(dbg == 2))
            if dbg == 2:
                return nc

        with ExitStack() as e2:
            wg = [sb(e2, "wg%d" % i, [128, KC, 512], BF16) for i in range(2)]
            wu = [sb(e2, "wu%d" % i, [128, KC, 512], BF16) for i in range(2)]
            wd = [sb(e2, "wd%d" % i, [128, 4, D], BF16) for i in range(2)]
            tWg = [T("wg%d" % i) for i in range(2)]
            tWu = [T("wu%d" % i) for i in range(2)]
            tWd = [T("wd%d" % i) for i in range(2)]
            xg_t = [sb(e2, "xg%d" % i, [128, D], BF16) for i in range(2)]
            tXgt = [T("xgt%d" % i) for i in range(2)]
            XgT = sb(e2, "XgT", [128, KC, CAP], BF16)
            tXgT = T("XgT")
            hTe = sb(e2, "hTe", [128, 4, CAP], BF16)
            tHe = T("hTe")
            sgt = [sb(e2, "sgt%d" % i, [128, CAP], F32) for i in range(2)]
            tSg = [T("sgt%d" % i) for i in range(2)]
            ysb = [sb(e2, "ysb%d" % i, [128, D], F32) for i in range(2)]
            tYs = [T("ysb%d" % i) for i in range(2)]

            def wload(e):
                i = e % 2
                b.dma("pool", wg[i][:], w_gate[e].rearrange("(kc p) n -> p kc n", p=128), (), [tWg[i]])
                b.dma("pool", wu[i][:], w_up[e].rearrange("(kc p) n -> p kc n", p=128), (), [tWu[i]])
                b.dma("pool", wd[i][:], w_down[e].rearrange("(fc p) n -> p fc n", p=128), (), [tWd[i]])

            wload(0)
            for e in range(DBG_NEX):
                i = e % 2
                if e + 1 < DBG_NEX:
                    wload(e + 1)
                for st in range(2):
                    r0 = e * CAP + st * 128
                    b.dma("sp", xg_t[st][:], Xg[r0:r0 + 128, :], [tXg], [tXgt[st]])
                    for rd in range(2):
                        p, tp = pb[rd], tPb[rd]
                        b.tr([(p[:, j, :], xg_t[st][:, (rd * 8 + j) * 128:(rd * 8 + j + 1) * 128], ident_bf[:])
                              for j in range(8)], [tXgt[st], tC], [tp])
                        b.evac(XgT[:, rd * 8:rd * 8 + 8, st * 128:(st + 1) * 128], p[:], [tp], [tXgT])
                for fc in range(4):
                    p, tp = ps[fc % 2], tP[fc % 2]
                    b.mm([(p[:, 0:CAP], [(wg[i][:, kc, fc * 128:(fc + 1) * 128], XgT[:, kc, :]) for kc in range(KC)]),
                          (p[:, CAP:2 * CAP], [(wu[i][:, kc, fc * 128:(fc + 1) * 128], XgT[:, kc, :]) for kc in range(KC)])],
                         [tWg[i], tWu[i], tXgT], [tp])
                    b.act(sgt[fc % 2][:], p[:, 0:CAP], AF.Silu, [tp], [tSg[fc % 2]])
                    b.tt(hTe[:, fc, :], sgt[fc % 2][:], p[:, CAP:2 * CAP], ALU.mult, [tSg[fc % 2], tp], [tHe])
                for st in range(2):
                    for nb in range(4):
                        p, tp = ps[2 + nb % 2], tP[2 + nb % 2]
                        b.mm([(p[:], [(hTe[:, fc, st * 128:(st + 1) * 128], wd[i][:, fc, nb * 512:(nb + 1) * 512])
                                      for fc in range(4)])], [tHe, tWd[i]], [tp])
                        b.evac(ysb[st][:, nb * 512:(nb + 1) * 512], p[:], [tp], [tYs[st]])
                    r0 = e * CAP + st * 128
                    b.dma("sp", Yg[r0:r0 + 128, :], ysb[st][:], [tYs[st]], [tYg], merge=True)
            S.barrier()
            S.emit()

        with ExitStack() as e3:
            gf = sb(e3, "gf", [128, D], F32)
            tGf = T("gf")
            b.dma("sp", gf[:], gf_bc, (), [tGf])
            xm_t = [sb(e3, "xm_t%d" % i, [128, D], F32) for i in range(2)]
            y1_t = [sb(e3, "y1_t%d" % i, [128, D], F32) for i in range(2)]
            y2_t = [sb(e3, "y2_t%d" % i, [128, D], F32) for i in range(2)]
            o_t = [sb(e3, "o_t%d" % i, [128, D], F32) for i in range(2)]
            jk = sb(e3, "jk", [128, D], BF16)
            sm3 = sb(e3, "sm3", [128, 8], F32)
            tXt = [T("xm_t%d" % i) for i in range(2)]
            tY1 = [T("y1_%d" % i) for i in range(2)]
            tY2 = [T("y2_%d" % i) for i in range(2)]
            tO = [T("o_%d" % i) for i in range(2)]
            tJ = T("jk")
            tS3 = T("sm3")
            for i in range(2):
                b.memset("pool", y1_t[i][:], 0.0, [tY1[i]])
                b.memset("pool", y2_t[i][:], 0.0, [tY2[i]])
            for ti in range(DBG_NT3):
                i = ti % 2
                r0 = ti * 128
                b.dma("sp", xm_t[i][:], xmid[r0:r0 + 128, :], [tXm[ti]], [tXt[i]])
                for kk, (yt, ty) in enumerate(((y1_t[i], tY1[i]), (y2_t[i], tY2[i]))):
                    def fga(g_, yt=yt, ti=ti, kk=kk):
                        return g_.indirect_dma_start(
                            out=yt[:, :], out_offset=None, in_=Yg,
                            in_offset=bass.IndirectOffsetOnAxis(ap=dest_i[:, ti, kk:kk + 1], axis=0),
                            bounds_check=S.breg, oob_is_err=False)
                    S.dma("pool", fga, [tYg, tDest[ti]], [ty])
                b.stt(xm_t[i][:], y1_t[i][:], wts[:, ti, 0:1], xm_t[i][:], ALU.mult, ALU.add,
                      [tY1[i], tDest[ti], tXt[i]], [tXt[i]])
                b.stt(xm_t[i][:], y2_t[i][:], wts[:, ti, 1:2], xm_t[i][:], ALU.mult, ALU.add,
                      [tY2[i], tDest[ti], tXt[i]], [tXt[i]])
                b.act(jk[:], xm_t[i][:], AF.Square, [tXt[i]], [tJ, tS3], accum_out=sm3[:, 0:1])
                b.ts(sm3[:, 1:2], sm3[:, 0:1], 1.0 / D, EPS, ALU.mult, ALU.add, [tS3], [tS3])
                b.act(sm3[:, 1:2], sm3[:, 1:2], AF.Ln, [tS3], [tS3])
                b.act(sm3[:, 2:3], sm3[:, 1:2], AF.Exp, [tS3], [tS3], scale=-0.5)
                b.stt(o_t[i][:], xm_t[i][:], sm3[:, 2:3], gf[:], ALU.mult, ALU.mult, [tXt[i], tS3, tGf], [tO[i]])
                b.dma("sp", out_d[r0:r0 + 128, :], o_t[i][:], [tO[i]], [])
            S.mark("phase3")
            S.barrier()
            S.emit(final=True)
            DBG_MARKS[:] = S.marks
    return nc


def _t5_bucket(n):
    import math
    max_exact = 16
    nf = np.maximum(n, 1).astype(np.float32)
    large = max_exact + (np.log(nf / max_exact) / math.log(128 / max_exact) * (32 - max_exact)).astype(np.int32)
    large = np.minimum(large, 31)
    return np.where(n < max_exact, n, large)


_NC_CACHE = {}


def _consts():
    ident = np.eye(128, dtype=np.float32)
    s_ = np.arange(128)[:, None] % 64
    t_ = np.arange(64)[None, :]
    triu = (s_ <= t_).astype(np.float32)
    lstrict = (np.arange(128)[:, None] < np.arange(128)[None, :]).astype(np.float32)
    scanmask = np.ones((128, 512), np.float32)
    scanmask[:, ::64] = 0.0
    ebase = np.broadcast_to((np.arange(32) * CAP).astype(np.float32)[None, :], (128, 32)).copy()
    return dict(c_ident=ident, c_triu=triu, c_lstrict=lstrict, c_scanmask=scanmask, c_ebase=ebase)


def make_in_maps(inputs, ncores=8):
    f = lambda a: np.ascontiguousarray(np.asarray(a, dtype=np.float32))
    x = f(inputs["x"])
    bc = lambda v: np.ascontiguousarray(np.broadcast_to(f(v).reshape(1, -1), (128, f(v).size)))
    rel = f(inputs["rel_bias"])
    k_ = np.arange(128)[:, None]
    j_ = np.arange(256)[None, :]
    dist = j_ - k_
    bucket = _t5_bucket(np.clip(dist, 0, None))
    valid = (dist >= 0) & (dist < 128)
    tbl = rel[bucket]
    tbl = np.where(valid[:, :, None], tbl, np.float32(NEG)).astype(np.float32)
    biasT = np.ascontiguousarray(tbl.transpose(0, 2, 1))
    lbl = np.ascontiguousarray(f(inputs["hg_lb_logits"]).reshape(2, 8, 128).transpose(2, 1, 0))
    wr = np.ascontiguousarray(np.concatenate([f(inputs["w_group_router"][0]), f(inputs["w_expert_router"][0])], axis=1))
    bb = np.concatenate([f(inputs["b_group_router"][0]), f(inputs["b_expert_router"][0])])
    shared = dict(
        w_in=f(inputs["w_in"][0]), w_a=f(inputs["w_attn_branch"][0]), w_r=f(inputs["w_hg_branch"][0]),
        w_o=f(inputs["w_out"][0]), w_gate=f(inputs["w_gate"][0]), w_up=f(inputs["w_up"][0]),
        w_down=f(inputs["w_down"][0]), g1_bc=bc(inputs["norm1_g"][0]), g2_bc=bc(inputs["norm2_g"][0]),
        gf_bc=bc(inputs["final_g"]), gn_bc=bc(np.tile(f(inputs["hg_norm_g"][0]), 8)), biasT=biasT,
        sinks_bc=bc(inputs["attn_sinks"][0]), lbl=lbl, wr=wr, b_bc=bc(bb), **_consts())
    maps = []
    for c in range(ncores):
        bi, hf = c // 2, c % 2
        m = dict(shared)
        m["x_own"] = np.ascontiguousarray(x[bi, hf * TOK:(hf + 1) * TOK])
        m["x_pre"] = np.ascontiguousarray(x[bi, 0:TOK]) if hf == 1 else np.zeros((TOK, D), np.float32)
        m["flag0"] = np.full((128, 1), float(hf), np.float32)
        maps.append(m)
    return maps


def kernel(**inputs):
    if "nc" not in _NC_CACHE:
        _NC_CACHE["nc"] = build_program()
    nc = _NC_CACHE["nc"]
    maps = make_in_maps(inputs, 8)
    res = run_bass_kernel_spmd(nc, maps, core_ids=list(range(8)))
    out = np.empty((4, 4096, D), np.float32)
    for c in range(8):
        out[c // 2, (c % 2) * TOK:(c % 2 + 1) * TOK] = res.results[c]["out"]
    return out
```

```python
import numpy as np
from contextlib import ExitStack
import concourse.bass as bass
import concourse.mybir as mybir
from concourse.bass_utils import run_bass_kernel_spmd

F32 = mybir.dt.float32
BF16 = mybir.dt.bfloat16
I32 = mybir.dt.int32
AF = mybir.ActivationFunctionType
ALU = mybir.AluOpType
AX = mybir.AxisListType

SAME_ENG_SYNC = True
D = 2048
KC = 16
TOK = 2048
G = 512
NG = TOK // G
INW = 9728
NE = 32
CAP = 256
EPS = 1e-6
NEG = -30000.0
WPD = 2
DBG_NGP = NG
DBG_NGO = NG
DBG_NEX = NE
DBG_NT3 = 16
DBG_LIMIT = 10 ** 9
DBG_MARKS = []
NRING = 3

O_QA, O_KA, O_VA, O_QH, O_FH, O_IH, O_GH, O_GA, O_GB = 0, 1024, 1280, 1536, 2560, 3584, 4608, 5632, 7680


class T:
    __slots__ = ("name", "w", "r")

    def __init__(self, name):
        self.name = name
        self.w = {}
        self.r = {}


class Sched:
    ENG = ("pe", "act", "dve", "pool", "sp")
    NDMA = 8

    def __init__(self, nc, es):
        self.nc = nc
        self.nrec = 0
        self.marks = []
        self.prog = {k: [] for k in self.ENG}
        self.sems = {}
        self.cnt = {}
        self.seen = {k: {} for k in self.ENG}
        for k in self.ENG:
            self.sems[k] = es.enter_context(nc.semaphore("s_" + k))
            self.cnt[k] = 0
        self.breg = es.enter_context(nc.gpsimd.register("bnd"))
        self.dma_pool = {}
        self.dma_i = {}
        for q in ("sp", "act", "pool"):
            keys = []
            for i in range(self.NDMA):
                k = "d_%s%d" % (q, i)
                self.sems[k] = es.enter_context(nc.semaphore(k))
                self.cnt[k] = 0
                keys.append(k)
            self.dma_pool[q] = keys
            self.dma_i[q] = 0

    def _deps(self, e, reads, writes, merge):
        need = {}
        for t in reads:
            for k, v in t.w.items():
                need[k] = max(need.get(k, 0), v)
        for t in writes:
            if not merge:
                for k, v in t.w.items():
                    need[k] = max(need.get(k, 0), v)
            for k, v in t.r.items():
                need[k] = max(need.get(k, 0), v)
        waits = []
        for k, v in need.items():
            if k == e and (e == "pe" or not SAME_ENG_SYNC):
                continue
            if self.seen[e].get(k, 0) < v:
                self.seen[e][k] = v
                waits.append((k, v))
        return waits

    def mark(self, name):
        self.marks.append((name, self.nrec, dict(self.cnt)))

    def op(self, e, fn, reads=(), writes=()):
        if self.nrec >= DBG_LIMIT:
            return
        self.nrec += 1
        waits = self._deps(e, reads, writes, False)
        self.cnt[e] += 1
        v = self.cnt[e]
        self.prog[e].append((waits, fn, (e, 1)))
        for t in reads:
            t.r[e] = v
        for t in writes:
            t.w = {e: v}
            t.r = {}

    def dma(self, q, fn, reads=(), writes=(), merge=False):
        if self.nrec >= DBG_LIMIT:
            return
        self.nrec += 1
        i = self.dma_i[q]
        self.dma_i[q] += 1
        k = self.dma_pool[q][i % self.NDMA]
        waits = self._deps(q, reads, writes, merge)
        if self.cnt[k] > self.seen[q].get(k, 0):
            waits.append((k, self.cnt[k]))
            self.seen[q][k] = self.cnt[k]
        self.cnt[k] += 16
        v = self.cnt[k]
        self.prog[q].append((waits, fn, (k, 16)))
        for t in reads:
            t.r[k] = v
        for t in writes:
            if merge:
                t.w[k] = v
            else:
                t.w = {k: v}
                t.r = {}

    def barrier(self):
        for e in self.ENG:
            waits = []
            for k, v in self.cnt.items():
                if k != e and v > self.seen[e].get(k, 0):
                    waits.append((k, v))
                    self.seen[e][k] = v
            self.prog[e].append((waits, None, None))

    def emit(self, final=False):
        sems = self.sems
        prog = self.prog
        fin = [(k, v) for k, v in self.cnt.items() if v > 0 and k != "sp"] if final else []

        def mk(e):
            plist = prog[e]

            def body(engobj):
                if e == "pool" and self.breg is not None:
                    engobj.reg_mov(self.breg, NE * CAP - 1)
                for waits, fn, inc in plist:
                    for (wk, wv) in waits:
                        engobj.wait_ge(sems[wk], wv)
                    if fn is not None:
                        ins = fn(engobj)
                        ins.then_inc(sems[inc[0]], inc[1])
                if e == "sp":
                    for (wk, wv) in fin:
                        engobj.wait_ge(sems[wk], wv)
            return body

        with self.nc.Block() as block:
            block.tensor(mk("pe"))
            block.scalar(mk("act"))
            block.vector(mk("dve"))
            block.gpsimd(mk("pool"))
            block.sync(mk("sp"))
        self.prog = {k: [] for k in self.ENG}


class B:
    def __init__(self, S):
        self.S = S
        self.flip = 0

    def mm(self, groups, reads, writes):
        groups = [(o, list(ops)) for o, ops in groups]

        def fn(pe):
            ins = None
            for o, ops in groups:
                n = len(ops)
                for i, (l, r) in enumerate(ops):
                    ins = pe.matmul(o, l, r, start=(i == 0), stop=(i == n - 1))
            return ins
        self.S.op("pe", fn, reads, writes)

    def tr(self, items, reads, writes):
        items = list(items)

        def fn(pe):
            ins = None
            for o, i_, idt in items:
                ins = pe.transpose(o, i_, idt)
            return ins
        self.S.op("pe", fn, reads, writes)

    def act(self, out, in_, func, reads, writes, **kw):
        def fn(e):
            return e.activation(out=out, in_=in_, func=func, **kw)
        self.S.op("act", fn, reads, writes)

    def tt(self, out, in0, in1, op, reads, writes, eng="dve"):
        def fn(e):
            return e.tensor_tensor(out=out, in0=in0, in1=in1, op=op)
        self.S.op(eng, fn, reads, writes)

    def ts(self, out, in0, s1, s2, op0, op1, reads, writes, eng="dve"):
        def fn(e):
            if s2 is None:
                return e.tensor_scalar(out=out, in0=in0, scalar1=s1, scalar2=None, op0=op0)
            return e.tensor_scalar(out=out, in0=in0, scalar1=s1, scalar2=s2, op0=op0, op1=op1)
        self.S.op(eng, fn, reads, writes)

    def stt(self, out, in0, scalar, in1, op0, op1, reads, writes):
        def fn(e):
            return e.scalar_tensor_tensor(out=out, in0=in0, scalar=scalar, in1=in1, op0=op0, op1=op1)
        self.S.op("dve", fn, reads, writes)

    def copy(self, eng, out, in_, reads, writes):
        if eng == "act":
            def fn(e):
                return e.activation(out=out, in_=in_, func=AF.Copy)
        else:
            def fn(e):
                return e.tensor_copy(out=out, in_=in_)
        self.S.op(eng, fn, reads, writes)

    def evac(self, out, in_, reads, writes):
        self.flip ^= 1
        self.copy("act" if self.flip else "dve", out, in_, reads, writes)

    def reduce(self, out, in_, op, reads, writes):
        def fn(e):
            return e.tensor_reduce(out=out, in_=in_, axis=AX.X, op=op)
        self.S.op("dve", fn, reads, writes)

    def recip(self, out, in_, reads, writes):
        def fn(e):
            return e.reciprocal(out=out, in_=in_)
        self.S.op("dve", fn, reads, writes)

    def memset(self, eng, ap, val, writes):
        def fn(e):
            return e.memset(ap, val)
        self.S.op(eng, fn, (), writes)

    def dma(self, q, out, in_, reads, writes, merge=False):
        def fn(e):
            return e.dma_start(out=out, in_=in_)
        self.S.dma(q, fn, reads, writes, merge)

    def scan(self, out, d0, d1, reads, writes):
        def fn(e):
            return e.tensor_tensor_scan(out=out, data0=d0, data1=d1, initial=0.0, op0=ALU.mult, op1=ALU.add)
        self.S.op("dve", fn, reads, writes)


def build_program(dbg=False):
    nc = bass.Bass("TRN2", target_bir_lowering=False)

    def din(name, shape, dt=F32):
        return nc.dram_tensor(name, shape, dt, kind="ExternalInput").ap()

    x_own = din("x_own", [TOK, D])
    x_pre = din("x_pre", [TOK, D])
    w_in = din("w_in", [D, INW])
    w_a = din("w_a", [1024, D])
    w_r = din("w_r", [1024, D])
    w_o = din("w_o", [D, D])
    w_gate = din("w_gate", [NE, D, 512])
    w_up = din("w_up", [NE, D, 512])
    w_down = din("w_down", [NE, 512, D])
    g1_bc = din("g1_bc", [128, D])
    g2_bc = din("g2_bc", [128, D])
    gf_bc = din("gf_bc", [128, D])
    gn_in = din("gn_bc", [128, 1024])
    bias_in = din("biasT", [128, 16, 256])
    flag_in = din("flag0", [128, 1])
    sinks_in = din("sinks_bc", [128, 16])
    lbl_in = din("lbl", [128, 8, 2])
    wr_in = din("wr", [D, 36])
    bb_in = din("b_bc", [128, 36])
    c_ident = din("c_ident", [128, 128])
    c_triu = din("c_triu", [128, 64])
    c_lstrict = din("c_lstrict", [128, 128])
    c_scanmask = din("c_scanmask", [128, 512])
    c_ebase = din("c_ebase", [128, 32])
    out_d = nc.dram_tensor("out", [TOK, D], F32, kind="ExternalOutput").ap()
    if dbg:
        xmid = nc.dram_tensor("xmid", [TOK, D], F32, kind="ExternalOutput").ap()
    else:
        xmid = nc.dram_tensor("xmid", [TOK, D], F32).ap()
    Xg = nc.dram_tensor("Xg", [NE * CAP, D], BF16).ap()
    Yg = nc.dram_tensor("Yg", [NE * CAP, D], F32).ap()
    tXm = [T("xm%d" % i) for i in range(16)]
    tXg = T("Xg")
    tYg = T("Yg")

    w_in_v = w_in.rearrange("(kc p) n -> p kc n", p=128)
    w_a_v = w_a.rearrange("(kc p) n -> p kc n", p=128)
    w_r_v = w_r.rearrange("(kc p) n -> p kc n", p=128)
    w_o_v = w_o.rearrange("(kc p) n -> p kc n", p=128)

    with ExitStack() as ea:
        S = Sched(nc, ea)
        b = B(S)

        def sb(es, name, shape, dt):
            return es.enter_context(nc.sbuf_tensor("sb_" + name, shape, dt))

        ident_bf = sb(ea, "ident_bf", [128, 128], BF16)
        ident_f = sb(ea, "ident_f", [128, 128], F32)
        triu = sb(ea, "triu", [128, 64], BF16)
        lstrict = sb(ea, "lstrict", [128, 128], BF16)
        ones_bf = sb(ea, "ones_bf", [128, 128], BF16)
        ebase = sb(ea, "ebase", [128, 32], F32)
        neghalf = sb(ea, "neghalf", [128, 1], F32)
        dest_i = sb(ea, "dest_i", [128, 16, 2], I32)
        wts = sb(ea, "wts", [128, 16, 2], F32)
        tC = T("consts")
        tDest = [T("dest%d" % i) for i in range(16)]
        ps = []
        tP = []
        for i in range(6):
            ps.append(ea.enter_context(nc.psum_tensor("ps%d" % i, [128, 512], F32)))
            tP.append(T("ps%d" % i))
        pb = []
        tPb = []
        for i in range(2):
            pb.append(ea.enter_context(nc.psum_tensor("pb%d" % i, [128, 8, 128], BF16)))
            tPb.append(T("pb%d" % i))

        b.dma("pool", ident_bf[:], c_ident, (), [tC])
        b.dma("sp", ident_f[:], c_ident, (), [tC], merge=True)
        b.dma("pool", triu[:], c_triu, (), [tC], merge=True)
        b.dma("pool", lstrict[:], c_lstrict, (), [tC], merge=True)
        b.dma("sp", ebase[:], c_ebase, (), [tC], merge=True)
        tC2 = T("consts2")
        b.memset("pool", ones_bf[:], 1.0, [tC2])
        b.memset("pool", neghalf[:], -0.5, [tC2])

        with ExitStack() as e1:
            scanmask = sb(e1, "scanmask", [128, 512], F32)
            biasT = sb(e1, "biasT", [128, 16, 256], F32)
            gn_bc = sb(e1, "gn_bc", [128, 1024], F32)
            wr_sb = sb(e1, "wr_sb", [128, 16, 36], F32)
            b_bc = sb(e1, "b_bc", [128, 36], F32)
            esink = sb(e1, "esink", [128, 16], F32)
            flag0 = sb(e1, "flag0", [128, 1], F32)
            lbl = sb(e1, "lbl", [128, 8, 2], F32)
            lb = sb(e1, "lb", [128, 8], F32)
            oml = sb(e1, "oml", [128, 8], F32)
            ln_oml = sb(e1, "ln_oml", [128, 8], F32)
            macc = sb(e1, "macc", [128, 32], BF16)
            tK = T("consts_p1")
            b.dma("sp", scanmask[:], c_scanmask, (), [tK])
            b.dma("sp", biasT[:], bias_in, (), [tK], merge=True)
            b.dma("sp", gn_bc[:], gn_in, (), [tK], merge=True)
            b.dma("sp", wr_sb[:], wr_in.rearrange("(kc p) n -> p kc n", p=128), (), [tK], merge=True)
            b.dma("sp", b_bc[:], bb_in, (), [tK], merge=True)
            b.dma("sp", esink[:], sinks_in, (), [tK], merge=True)
            b.dma("sp", flag0[:], flag_in, (), [tK], merge=True)
            b.dma("sp", lbl[:], lbl_in, (), [tK], merge=True)
            tL = T("lb")
            b.tt(lb[:], lbl[:, :, 1], lbl[:, :, 0], ALU.subtract, [tK], [tL])
            b.act(lb[:], lb[:], AF.Exp, [tL], [tL])
            b.ts(lb[:], lb[:], 1.0, None, ALU.add, None, [tL], [tL])
            b.recip(lb[:], lb[:], [tL], [tL])
            b.ts(oml[:], lb[:], -1.0, 1.0, ALU.mult, ALU.add, [tL], [tL])
            b.act(ln_oml[:], oml[:], AF.Ln, [tL], [tL])
            tEs = T("esink")
            b.act(esink[:], esink[:], AF.Exp, [tK], [tEs])
            tMacc = T("macc")
            b.memset("pool", macc[:], 0.0, [tMacc])

            NR = NRING
            wring = [sb(e1, "wring%d" % i, [128, 4096], BF16) for i in range(NR)]
            tW = [T("wring%d" % i) for i in range(NR)]
            hT = sb(e1, "hT", [128, KC, G], BF16)
            tHT = T("hT")
            qaT = sb(e1, "qaT", [128, 8, G], BF16)
            tQA = [T("qa%d" % i) for i in range(4)]
            kbuf = sb(e1, "kbuf", [128, 4, 128 + G], BF16)
            tKb = T("kbuf")
            vbuf = sb(e1, "vbuf", [128, 5, 4, 65], BF16)
            tVb = T("vbuf")
            QtT = sb(e1, "QtT", [128, 8, G], BF16)
            tQt = [T("Qt%d" % i) for i in range(4)]
            KtT = sb(e1, "KtT", [128, 8, G], BF16)
            tKt = T("KtT")
            vh = sb(e1, "vh", [128, 4, 1024], BF16)
            tVh = T("vh")
            gsg = sb(e1, "gsg", [128, 4, 1024], BF16)
            tGs = T("gsg")
            sgA = sb(e1, "sgA", [128, 16, G], BF16)
            sgH = sb(e1, "sgH", [128, 16, G], BF16)
            tSgA = T("sgA")
            tSgH = T("sgH")
            S32 = sb(e1, "S32", [128, 8, 128], F32)
            Sbf = sb(e1, "Sbf", [128, 8, 128], BF16)
            tS32 = T("S32")
            tSbf = T("Sbf")
            dec = sb(e1, "dec", [128, 8, 8], F32)
            tDec = T("dec")
            xt0 = sb(e1, "xt0", [128, D], F32)
            xt1 = sb(e1, "xt1", [128, D], F32)
            gbc = sb(e1, "gbc", [128, D], F32)
            hb = sb(e1, "hb", [128, D], BF16)
            tX0, tX1, tGbc, tHb = T("xt0"), T("xt1"), T("gbc"), T("hb")
            arena = sb(e1, "arena", [128, 6 * 512], F32)
            tA = [T("ar%d" % i) for i in range(6)]
            small = sb(e1, "small", [128, 64], F32)
            tSm = T("small")
            smallr = sb(e1, "smallr", [128, 160], F32)
            tSr = T("smallr")

            def ar(i, n=1):
                return arena[:, i * 512:(i + n) * 512]

            b.memset("pool", S32[:], 0.0, [tS32])
            b.memset("pool", Sbf[:], 0.0, [tSbf])
            b.memset("pool", vbuf[:], 1.0, [tVb])
            b.memset("pool", kbuf[:], 0.0, [tKb])

            b.memset("pool", hb[:], 0.0, [tHb])
            for i in range(NE * CAP // 128):
                b.dma("act", Xg[i * 128:(i + 1) * 128, :], hb[:], [tHb], [tXg], merge=True)

            wlist = []

            def wq_win(col0):
                wlist.append((w_in_v[:, :, col0:col0 + 256], 16, 256))
                return len(wlist) - 1
            wstate = {"issued": 0}

            def wissue(upto):
                while wstate["issued"] <= min(upto, len(wlist) - 1):
                    i = wstate["issued"]
                    src, kc, n = wlist[i]
                    slot = wring[i % NR][:, 0:kc * n].rearrange("p (k n) -> p k n", k=kc)
                    b.dma("pool", slot, src, (), [tW[i % NR]])
                    wstate["issued"] += 1

            def wget(i):
                wissue(i + WPD)
                src, kc, n = wlist[i]
                return wring[i % NR][:, 0:kc * n].rearrange("p (k n) -> p k n", k=kc), tW[i % NR]

            pstate = {"i": 0}

            def nextps():
                pstate["i"] ^= 1
                return ps[pstate["i"]], tP[pstate["i"]]

            def rms_tile(xt, tXt, gtile, tG, out, tOut, junk, tJunk):
                b.act(junk, xt, AF.Square, [tXt], [tJunk, tSm], accum_out=small[:, 0:1])
                b.ts(small[:, 1:2], small[:, 0:1], 1.0 / D, EPS, ALU.mult, ALU.add, [tSm], [tSm])
                b.act(small[:, 1:2], small[:, 1:2], AF.Ln, [tSm], [tSm])
                b.act(small[:, 2:3], small[:, 1:2], AF.Exp, [tSm], [tSm], scale=-0.5)
                b.stt(out, xt, small[:, 2:3], gtile, ALU.mult, ALU.mult, [tXt, tSm, tG, tJunk], [tOut])

            def stage_norm(xsrc, grp):
                b.dma("sp", gbc[:], g1_bc, (), [tGbc])
                for tt in range(4):
                    xt, tXt = (xt0, tX0) if tt % 2 == 0 else (xt1, tX1)
                    r0 = grp * G + tt * 128
                    b.dma("sp", xt[:], xsrc[r0:r0 + 128, :], (), [tXt])
                    rms_tile(xt[:], tXt, gbc[:], tGbc, hb[:], tHb, hb[:], tHb)
                    for rd in range(2):
                        p, tp = pb[rd], tPb[rd]
                        b.tr([(p[:, j, :], hb[:, (rd * 8 + j) * 128:(rd * 8 + j + 1) * 128], ident_bf[:])
                              for j in range(8)], [tHb, tC], [tp])
                        b.evac(hT[:, rd * 8:rd * 8 + 8, tt * 128:(tt + 1) * 128], p[:], [tp], [tHT])

            def proj_fm(slot, tSlot, c):
                p, tp = nextps()
                b.mm([(p[:], [(slot[:, kc, c * 128:(c + 1) * 128], hT[:, kc, :]) for kc in range(KC)])],
                     [tSlot, tHT], [tp])
                return p, tp

            def proj_tm(slot, tSlot, tt, n):
                p, tp = nextps()
                b.mm([(p[:, 0:n], [(hT[:, kc, tt * 128:(tt + 1) * 128], slot[:, kc, 0:n]) for kc in range(KC)])],
                     [tSlot, tHT], [tp])
                return p, tp

            def chain(h, p, tp, own):
                E, L1, L2, bb_ = ar(0), ar(1), ar(2), ar(3)
                b.act(E, p[:], AF.Exp, [tp], [tA[0]])
                b.act(L1, E, AF.Ln, [tA[0], tL], [tA[1]], bias=lb[:, h:h + 1])
                b.act(L2, E, AF.Ln, [tA[0]], [tA[2]], bias=1.0)
                b.tt(L1, L1, L2, ALU.subtract, [tA[1], tA[2]], [tA[1]])
                b.scan(bb_, scanmask[:], L1, [tK, tA[1]], [tA[3]])
                b.act(E, bb_, AF.Exp, [tA[3]], [tA[0]])
                if own:
                    b.tt(QtT[:, h, :], QtT[:, h, :], E, ALU.mult, [tA[0]] + tQt, tQt)
                b.tt(L2, L2, bb_, ALU.add, [tA[2], tA[3]], [tA[2]])
                b.act(KtT[:, h, :], L2, AF.Exp, [tA[2], tL], [tKt], scale=-1.0, bias=ln_oml[:, h:h + 1])
                b.copy("pool", dec[:, h, :], E.rearrange("p (c t) -> p c t", t=64)[:, :, 63], [tA[0]], [tDec])

            def stage_proj(own, last_prefix, wbase):
                wi = wbase
                if own:
                    for blk in range(4):
                        slot, tsl = wget(wi); wi += 1
                        for c in range(2):
                            p, tp = proj_fm(slot, tsl, c)
                            b.act(qaT[:, blk * 2 + c, :], p[:], AF.Copy, [tp], tQA, scale=0.125)
                if own or last_prefix:
                    slot, tsl = wget(wi); wi += 1
                    for g in range(4):
                        p, tp = nextps()
                        b.mm([(p[0:64, :], [(slot[:, kc, g * 64:(g + 1) * 64], hT[:, kc, :]) for kc in range(KC)]),
                              (p[64:128, :], [(slot[:, kc, g * 64:(g + 1) * 64], hT[:, kc, :]) for kc in range(KC)])],
                             [tsl, tHT], [tp])
                        b.evac(kbuf[:, g, 128:128 + G], p[:], [tp], [tKb])
                    slot, tsl = wget(wi); wi += 1
                    for tt in range(4):
                        p, tp = proj_tm(slot, tsl, tt, 256)
                        b.evac(vbuf[:, 1 + tt, :, 0:64], p[:, 0:256].rearrange("p (g d) -> p g d", g=4), [tp], [tVb])
                if own:
                    for blk in range(4):
                        slot, tsl = wget(wi); wi += 1
                        for c in range(2):
                            p, tp = proj_fm(slot, tsl, c)
                            b.evac(QtT[:, blk * 2 + c, :], p[:], [tp], tQt)
                for blk in range(4):
                    slot, tsl = wget(wi); wi += 1
                    for c in range(2):
                        p, tp = proj_fm(slot, tsl, c)
                        chain(blk * 2 + c, p, tp, own)
                for blk in range(4):
                    slot, tsl = wget(wi); wi += 1
                    for tt in range(4):
                        p, tp = proj_tm(slot, tsl, tt, 256)
                        b.evac(vh[:, tt, blk * 256:(blk + 1) * 256], p[:, 0:256], [tp], [tVh])
                if own:
                    for blk in range(4):
                        slot, tsl = wget(wi); wi += 1
                        for tt in range(4):
                            p, tp = proj_tm(slot, tsl, tt, 256)
                            tmp = ar(4)[:, 0:256]
                            b.act(tmp, p[:, 0:256], AF.Silu, [tp], [tA[4]])
                            b.tt(gsg[:, tt, blk * 256:(blk + 1) * 256], tmp, gn_bc[:, blk * 256:(blk + 1) * 256],
                                 ALU.mult, [tA[4], tK], [tGs])
                    for blk in range(8):
                        slot, tsl = wget(wi); wi += 1
                        for c in range(2):
                            p, tp = proj_fm(slot, tsl, c)
                            b.act(sgA[:, blk * 2 + c, :], p[:], AF.Sigmoid, [tp], [tSgA])
                    for blk in range(8):
                        slot, tsl = wget(wi); wi += 1
                        for c in range(2):
                            p, tp = proj_fm(slot, tsl, c)
                            b.act(sgH[:, blk * 2 + c, :], p[:], AF.Sigmoid, [tp], [tSgH])
                return wi

            def stage_attn(first_group):
                sE = ar(0, 2).rearrange("p (a n) -> p a n", a=2)
                pT = ar(2).bitcast(BF16).rearrange("p (a h q) -> p a h q", a=2, h=4)
                a_tok = ar(3).bitcast(BF16)
                for tt in range(4):
                    for g in range(4):
                        banks = [[(ps[2], tP[2]), (ps[3], tP[3])], [(ps[0], tP[0]), (ps[1], tP[1])]]
                        sE4 = sE.rearrange("p a (j two q) -> p a j two q", j=2, two=2)
                        for kb in range(2):
                            for par in range(2):
                                pbk, tpbk = banks[kb][par]
                                p0 = par * 64
                                groups = []
                                for jj in range(2):
                                    h = g * 4 + jj * 2 + par
                                    groups.append((pbk[:, jj * 128:(jj + 1) * 128],
                                                   [(kbuf[p0:p0 + 64, g, (tt + kb) * 128:(tt + kb + 1) * 128],
                                                     qaT[p0:p0 + 64, h // 2, tt * 128:(tt + 1) * 128])]))
                                b.mm(groups, [tKb, tQA[tt]], [tpbk])
                                bcol = slice(128, 256) if kb == 0 else slice(0, 128)
                                bsl = biasT[:, g * 4:(g + 1) * 4, bcol].rearrange("p (j two) q -> p j two q", two=2)[:, :, par, :]
                                b.stt(sE4[:, kb, :, par, :], pbk[:, 0:256].rearrange("p (j q) -> p j q", j=2), 60.0, bsl,
                                      ALU.min, ALU.add, [tpbk, tK], [tA[kb]])
                        b.act(pT, sE.rearrange("p a (h q) -> p a h q", h=4), AF.Exp, [tA[0], tA[1]], [tA[2]])
                        if first_group and tt == 0:
                            b.ts(pT[:, 0], pT[:, 0], flag0[:, 0:1], None, ALU.mult, None, [tA[2], tK], [tA[2]])
                        po, tpo = ps[4], tP[4]
                        groups = []
                        for hh in range(4):
                            groups.append((po[:, hh * 65:(hh + 1) * 65],
                                           [(pT[:, kb, hh, :], vbuf[:, tt + kb, g, :]) for kb in range(2)]))
                        b.mm(groups, [tA[2], tVb], [tpo])
                        pov = po[:, 0:260].rearrange("p (h d) -> p h d", h=4)
                        b.tt(smallr[:, 0:4], pov[:, :, 64], esink[:, g * 4:(g + 1) * 4], ALU.add, [tpo, tEs], [tSr])
                        b.recip(smallr[:, 4:8], smallr[:, 0:4], [tSr], [tSr])
                        b.tt(a_tok[:, g * 256:(g + 1) * 256].rearrange("p (h d) -> p h d", h=4), pov[:, :, 0:64],
                             smallr[:, 4:8].unsqueeze(2).broadcast_to([128, 4, 64]), ALU.mult, [tpo, tSr], [tA[3]])
                    p, tp = pb[tt % 2], tPb[tt % 2]
                    b.tr([(p[:, j, :], a_tok[:, j * 128:(j + 1) * 128], ident_bf[:]) for j in range(8)],
                         [tA[3], tC], [tp])
                    b.evac(qaT[:, :, tt * 128:(tt + 1) * 128], p[:], [tp], [tQA[tt]])
                b.copy("pool", kbuf[:, :, 0:128], kbuf[:, :, G:G + 128], [tKb], [tKb])
                b.copy("pool", vbuf[:, 0], vbuf[:, 4], [tVb], [tVb])

            def stage_hloop(own):
                attm = ar(0).bitcast(BF16)[:, 0:512].rearrange("p (h t) -> p h t", h=8)
                r_tok = ar(1).bitcast(BF16)
                junk = ar(2).bitcast(BF16)
                Kt = ar(3).bitcast(BF16).rearrange("p (h k) -> p h k", h=8)
                for tt in range(4):
                    p, tp = pb[tt % 2], tPb[tt % 2]
                    b.tr([(p[:, h, :], KtT[:, h, tt * 128:(tt + 1) * 128], ident_bf[:]) for h in range(8)],
                         [tKt, tC], [tp])
                    b.evac(Kt, p[:], [tp], [tA[3]])
                    for cc in range(2):
                        c = tt * 2 + cc
                        p0 = cc * 64
                        cs = slice(c * 64, (c + 1) * 64)
                        if own:
                            pa, tpa = ps[4], tP[4]
                            b.mm([(pa[p0:p0 + 64, h * 64:(h + 1) * 64], [(KtT[:, h, cs], QtT[:, h, cs])])
                                  for h in range(8)], [tKt, tQt[tt]], [tpa])
                            b.tt(attm[p0:p0 + 64], pa[p0:p0 + 64, :].rearrange("p (h t) -> p h t", h=8),
                                 triu[p0:p0 + 64, :].unsqueeze(1).broadcast_to([64, 8, 64]), ALU.mult,
                                 [tpa, tC], [tA[0]])
                            groups = []
                            for h in range(8):
                                po = ps[2] if h < 4 else ps[3]
                                o_ap = po[p0:p0 + 64, (h % 4) * 128:(h % 4 + 1) * 128]
                                groups.append((o_ap, [(QtT[:, h, cs], Sbf[:, h, :]),
                                                      (attm[p0:p0 + 64, h, :], vh[p0:p0 + 64, tt, h * 128:(h + 1) * 128])]))
                            b.mm(groups, [tQt[tt], tSbf, tA[0], tVh], [tP[2], tP[3]])
                        groups = []
                        for h in range(8):
                            po = ps[0] if h < 4 else ps[1]
                            groups.append((po[:, (h % 4) * 128:(h % 4 + 1) * 128],
                                           [(Kt[p0:p0 + 64, h, :], vh[p0:p0 + 64, tt, h * 128:(h + 1) * 128])]))
                        b.mm(groups, [tA[3], tVh], [tP[0], tP[1]])
                        b.tt(S32[:, 0:4, :], ps[0][:].rearrange("p (h v) -> p h v", h=4), S32[:, 0:4, :], ALU.add,
                             [tP[0], tS32], [tS32])
                        b.tt(S32[:, 4:8, :], ps[1][:].rearrange("p (h v) -> p h v", h=4), S32[:, 4:8, :], ALU.add,
                             [tP[1], tS32], [tS32])
                        b.tt(S32[:], S32[:], dec[:, :, c].unsqueeze(2).broadcast_to([128, 8, 128]), ALU.mult,
                             [tS32, tDec], [tS32])
                        b.copy("act", Sbf[:], S32[:], [tS32], [tSbf])
                    if own:
                        for h in range(8):
                            po = ps[2] if h < 4 else ps[3]
                            b.act(junk[:, 0:128], po[:, (h % 4) * 128:(h % 4 + 1) * 128], AF.Square,
                                  [tP[2], tP[3]], [tA[2], tSr], accum_out=smallr[:, 16 + h:17 + h])
                        b.ts(smallr[:, 32:40], smallr[:, 16:24], 1.0 / 128, EPS, ALU.mult, ALU.add, [tSr], [tSr])
                        b.act(smallr[:, 32:40], smallr[:, 32:40], AF.Ln, [tSr], [tSr])
                        b.act(smallr[:, 40:48], smallr[:, 32:40], AF.Exp, [tSr], [tSr], scale=-0.5)
                        for h in range(8):
                            po = ps[2] if h < 4 else ps[3]
                            b.stt(r_tok[:, h * 128:(h + 1) * 128], po[:, (h % 4) * 128:(h % 4 + 1) * 128],
                                  smallr[:, 40 + h:41 + h], gsg[:, tt, h * 128:(h + 1) * 128], ALU.mult, ALU.mult,
                                  [tP[2], tP[3], tSr, tGs], [tA[1]])
                        p, tp = pb[(tt + 1) % 2], tPb[(tt + 1) % 2]
                        b.tr([(p[:, j, :], r_tok[:, j * 128:(j + 1) * 128], ident_bf[:]) for j in range(8)],
                             [tA[1], tC], [tp])
                        b.evac(QtT[:, :, tt * 128:(tt + 1) * 128], p[:], [tp], [tQt[tt]])

            def stage_branch(wi):
                m2 = ar(1)
                for nb in range(4):
                    sa, tsa = wget(wi); wi += 1
                    for c in range(4):
                        j = nb * 4 + c
                        p, tp = ps[2 + c % 2], tP[2 + c % 2]
                        b.mm([(p[:], [(sa[:, kc, c * 128:(c + 1) * 128], qaT[:, kc, :]) for kc in range(8)])],
                             [tsa] + tQA, [tp])
                        b.tt(ar(2 + c), p[:], sgA[:, j, :], ALU.mult, [tp, tSgA], [tA[2 + c]])
                    sr, tsr = wget(wi); wi += 1
                    for c in range(4):
                        j = nb * 4 + c
                        p, tp = ps[2 + c % 2], tP[2 + c % 2]
                        b.mm([(p[:], [(sr[:, kc, c * 128:(c + 1) * 128], QtT[:, kc, :]) for kc in range(8)])],
                             [tsr] + tQt, [tp])
                        b.tt(m2, p[:], sgH[:, j, :], ALU.mult, [tp, tSgH], [tA[1]])
                        b.tt(hT[:, j, :], ar(2 + c), m2, ALU.add, [tA[2 + c], tA[1]], [tHT])
                return wi

            def stage_out(wi, grp):
                xs = [xt0[:, 0:1024].rearrange("p (t c) -> p t c", t=4), xt0[:, 1024:2048].rearrange("p (t c) -> p t c", t=4)]
                os_ = [xt1[:, 0:1024].rearrange("p (t c) -> p t c", t=4), xt1[:, 1024:2048].rearrange("p (t c) -> p t c", t=4)]
                txs = [tA[2], tA[3]]
                tos = [tA[4], tA[5]]
                rows = slice(grp * G, (grp + 1) * G)

                def xload(nb):
                    b.dma("sp", xs[nb % 2], x_own[rows, nb * 256:(nb + 1) * 256].rearrange("(t p) c -> p t c", p=128),
                          [], [txs[nb % 2], tX0])
                xload(0)
                for nb in range(8):
                    so, tso = wget(wi); wi += 1
                    if nb + 1 < 8:
                        xload(nb + 1)
                    for tt in range(4):
                        p, tp = nextps()
                        b.mm([(p[:, 0:256], [(hT[:, kc, tt * 128:(tt + 1) * 128], so[:, kc, :]) for kc in range(KC)])],
                             [tso, tHT], [tp])
                        b.tt(os_[nb % 2][:, tt, :], p[:, 0:256], xs[nb % 2][:, tt, :], ALU.add,
                             [tp, txs[nb % 2]], [tos[nb % 2], tX1])
                    b.dma("sp", xmid[rows, nb * 256:(nb + 1) * 256].rearrange("(t p) c -> p t c", p=128), os_[nb % 2],
                          [tos[nb % 2]], [tXm[grp * 4 + t_] for t_ in range(4)], merge=True)
                return wi

            def stage_route(grp):
                b.dma("sp", gbc[:], g2_bc, (), [tGbc])
                h2b = sgA[:].rearrange("p a n -> p (a n)").rearrange("p (t d) -> p t d", t=4)
                pr, tpr = ps[5], tP[5]
                for tt in range(4):
                    ti = grp * 4 + tt
                    r0 = ti * 128
                    b.dma("sp", xt0[:], xmid[r0:r0 + 128, :], [tXm[ti]], [tX0])
                    rms_tile(xt0[:], tX0, gbc[:], tGbc, xt1[:], tX1, hb[:], tHb)
                    b.copy("pool", h2b[:, tt, :], xt1[:], [tX1], [tSgA])
                    for rd in range(4):
                        p, tp = ps[2 + rd % 2], tP[2 + rd % 2]
                        h2T, th2T = (ar(0), tA[0]) if rd % 2 == 0 else (ar(1), tA[1])
                        b.tr([(p[:, j * 128:(j + 1) * 128], xt1[:, (rd * 4 + j) * 128:(rd * 4 + j + 1) * 128], ident_f[:])
                              for j in range(4)], [tX1, tC], [tp])
                        b.evac(h2T, p[:], [tp], [th2T])
                        h2v = h2T.rearrange("p (j t) -> p j t", j=4)

                        def fn(pe, h2v=h2v, rd=rd, pr=pr, tt=tt):
                            ins = None
                            for j in range(4):
                                kc = rd * 4 + j
                                ins = pe.matmul(pr[:, tt * 36:(tt + 1) * 36], h2v[:, j, :], wr_sb[:, kc, :],
                                                start=(kc == 0), stop=(kc == 15))
                            return ins
                        S.op("pe", fn, [th2T, tK], [tpr])
                A2, A4 = ar(2), ar(4)
                t2_, t3_, t4_ = tA[2], tA[3], tA[4]
                LG = A4[:, 0:144].rearrange("p (t c) -> p t c", t=4)
                b.tt(LG, pr[:, 0:144].rearrange("p (t c) -> p t c", t=4), b_bc[:].unsqueeze(1).broadcast_to([128, 4, 36]),
                     ALU.add, [tpr, tK], [t4_])
                gl = LG[:, :, 0:4]
                el = LG[:, :, 4:36].rearrange("p t (g e) -> p t g e", g=4)
                tmp = A4[:, 144:272].rearrange("p (t g e) -> p t g e", t=4, g=4)
                esel = A4[:, 272:304].rearrange("p (t e) -> p t e", t=4)
                top8 = A4[:, 304:336].rearrange("p (t e) -> p t e", t=4)
                oh1 = A4[:, 336:368].rearrange("p (t e) -> p t e", t=4)
                oh2 = A4[:, 368:400].rearrange("p (t e) -> p t e", t=4)
                goh = A4[:, 400:416].rearrange("p (t g) -> p t g", t=4)
                gd = A4[:, 416:432].rearrange("p (t g) -> p t g", t=4)
                mi = A4[:, 432:512]
                gmax, gsum, gw, dm, dd, w1, w2 = (mi[:, 4 * i:4 * i + 4] for i in range(7))

                def bc3(ap, n):
                    return ap.unsqueeze(2).broadcast_to([128, 4, n])
                b.reduce(gmax, gl, ALU.max, [t4_], [t4_])
                b.tt(goh, gl, bc3(gmax, 4), ALU.is_equal, [t4_], [t4_])
                b.tt(gd, gl, bc3(gmax, 4), ALU.subtract, [t4_], [t4_])
                b.act(gd, gd, AF.Exp, [t4_], [t4_])
                b.reduce(gsum, gd, ALU.add, [t4_], [t4_])
                b.recip(gw, gsum, [t4_], [t4_])
                b.tt(tmp, el, goh.unsqueeze(3).broadcast_to([128, 4, 4, 8]), ALU.mult, [t4_], [t4_])
                b.reduce(esel, tmp.rearrange("p t g e -> p t e g"), ALU.add, [t4_], [t4_])
                for tt in range(4):
                    def fmax(e, o=top8[:, tt, :], i_=esel[:, tt, :]):
                        return e.max(out=o, in_=i_)
                    S.op("dve", fmax, [t4_], [t4_])
                b.tt(oh1, esel, bc3(top8[:, :, 0], 8), ALU.is_equal, [t4_], [t4_])
                b.tt(oh2, esel, bc3(top8[:, :, 1], 8), ALU.is_equal, [t4_], [t4_])
                b.tt(dm, top8[:, :, 1], top8[:, :, 0], ALU.subtract, [t4_], [t4_])
                b.act(dd, dm, AF.Exp, [t4_], [t4_])
                b.ts(dm, dd, 1.0, None, ALU.add, None, [t4_], [t4_])
                b.recip(dm, dm, [t4_], [t4_])
                b.tt(w1, dm, gw, ALU.mult, [t4_], [t4_])
                b.tt(w2, w1, dd, ALU.mult, [t4_], [t4_])
                M1 = A2[:, 0:128].rearrange("p (t g e) -> p t g e", t=4, g=4)
                M2 = A2[:, 128:256].rearrange("p (t g e) -> p t g e", t=4, g=4)
                gohb = goh.unsqueeze(3).broadcast_to([128, 4, 4, 8])
                b.tt(M1, gohb, oh1.unsqueeze(2).broadcast_to([128, 4, 4, 8]), ALU.mult, [t4_], [t2_])
                b.tt(M2, gohb, oh2.unsqueeze(2).broadcast_to([128, 4, 4, 8]), ALU.mult, [t4_], [t2_])
                Mb = ar(3).bitcast(BF16)[:, 0:128].rearrange("p (t e) -> p t e", t=4)
                b.tt(Mb, A2[:, 0:128].rearrange("p (t e) -> p t e", t=4), A2[:, 128:256].rearrange("p (t e) -> p t e", t=4),
                     ALU.add, [t2_], [t3_])
                pp, tpp = ps[4], tP[4]
                groups = []
                for tt in range(4):
                    ops = [(lstrict[:], Mb[:, tt, :]), (ones_bf[:], macc[:])]
                    for t_ in range(tt):
                        ops.append((ones_bf[:], Mb[:, t_, :]))
                    groups.append((pp[:, tt * 32:(tt + 1) * 32], ops))
                b.mm(groups, [t3_, tC, tC2, tMacc], [tpp])
                for tt in range(4):
                    b.tt(macc[:], macc[:], Mb[:, tt, :], ALU.add, [t3_, tMacc], [tMacc], eng="pool")
                pos = A2[:, 256:384].rearrange("p (t e) -> p t e", t=4)
                tm = A2[:, 384:512].rearrange("p (t e) -> p t e", t=4)
                b.copy("dve", pos, pp[:, 0:128].rearrange("p (t e) -> p t e", t=4), [tpp], [t2_])
                s2 = small
                for kk in range(2):
                    Mk = A2[:, kk * 128:(kk + 1) * 128].rearrange("p (t e) -> p t e", t=4)
                    pk, ek, okf, dk = (s2[:, 16 + 16 * kk + 4 * i:20 + 16 * kk + 4 * i] for i in range(4))
                    b.tt(tm, pos, Mk, ALU.mult, [t2_], [t2_])
                    b.reduce(pk, tm, ALU.add, [t2_], [tSm])
                    b.tt(tm, ebase[:].unsqueeze(1).broadcast_to([128, 4, 32]), Mk, ALU.mult, [t2_, tC], [t2_])
                    b.reduce(ek, tm, ALU.add, [t2_], [tSm])
                    b.ts(okf, pk, float(CAP), None, ALU.is_lt, None, [tSm], [tSm])
                    b.ts(dk, okf, -1.0e6, 1.0e6, ALU.mult, ALU.add, [tSm], [tSm])
                    b.tt(dk, dk, pk, ALU.add, [tSm], [tSm])
                    b.tt(dk, dk, ek, ALU.add, [tSm], [tSm])
                    tD = [tDest[grp * 4 + t_] for t_ in range(4)]
                    b.copy("dve", dest_i[:, grp * 4:(grp + 1) * 4, kk], dk, [tSm], tD)
                    b.tt(wts[:, grp * 4:(grp + 1) * 4, kk], (w1 if kk == 0 else w2), okf, ALU.mult, [tSm, t4_], tD)
                for tt in range(4):
                    ti = grp * 4 + tt
                    for kk in range(2):
                        def fsc(g_, ti=ti, kk=kk, tt=tt):
                            return g_.indirect_dma_start(
                                out=Xg, out_offset=bass.IndirectOffsetOnAxis(ap=dest_i[:, ti, kk:kk + 1], axis=0),
                                in_=h2b[:, tt, :], in_offset=None, bounds_check=S.breg, oob_is_err=False)
                        S.dma("pool", fsc, [tSgA, tDest[ti]], [tXg], merge=True)

            plan = []
            for pg in range(NG):
                base = len(wlist)
                if pg == NG - 1:
                    wq_win(O_KA); wq_win(O_VA)
                for blk in range(4):
                    wq_win(O_FH + blk * 256)
                for blk in range(4):
                    wq_win(O_IH + blk * 256)
                plan.append(base)
            for og in range(NG):
                base = len(wlist)
                for blk in range(4):
                    wq_win(O_QA + blk * 256)
                wq_win(O_KA); wq_win(O_VA)
                for sec in (O_QH, O_FH, O_IH, O_GH):
                    for blk in range(4):
                        wq_win(sec + blk * 256)
                for sec in (O_GA, O_GB):
                    for blk in range(8):
                        wq_win(sec + blk * 256)
                for nb in range(4):
                    wlist.append((w_a_v[:, :, nb * 512:(nb + 1) * 512], 8, 512))
                    wlist.append((w_r_v[:, :, nb * 512:(nb + 1) * 512], 8, 512))
                for nb in range(8):
                    wlist.append((w_o_v[:, :, nb * 256:(nb + 1) * 256], 16, 256))
                plan.append(base)

            S.mark("init")
            for pg in range(NG - DBG_NGP, NG):
                stage_norm(x_pre, pg)
                S.mark("pnorm%d" % pg)
                stage_proj(False, pg == NG - 1, plan[pg])
                S.mark("pproj%d" % pg)
                stage_hloop(False)
                S.mark("phloop%d" % pg)
            b.copy("pool", kbuf[:, :, 0:128], kbuf[:, :, G:G + 128], [tKb], [tKb])
            b.copy("pool", vbuf[:, 0], vbuf[:, 4], [tVb], [tVb])
            for og in range(DBG_NGO):
                stage_norm(x_own, og)
                S.mark("norm%d" % og)
                wi = stage_proj(True, False, plan[NG + og])
                S.mark("proj%d" % og)
                stage_attn(og == 0)
                S.mark("attn%d" % og)
                stage_hloop(True)
                S.mark("hloop%d" % og)
                wi = stage_branch(wi)
                S.mark("branch%d" % og)
                wi = stage_out(wi, og)
                S.mark("out%d" % og)
                stage_route(og)
                S.mark("route%d" % og)
            S.mark("phase1")
            S.barrier()
            S.emit(final=(dbg == 2))
            if dbg == 2:
                return nc

        with ExitStack() as e2:
            wg = [sb(e2, "wg%d" % i, [128, KC, 512], BF16) for i in range(2)]
            wu = [sb(e2, "wu%d" % i, [128, KC, 512], BF16) for i in range(2)]
            wd = [sb(e2, "wd%d" % i, [128, 4, D], BF16) for i in range(2)]
            tWg = [T("wg%d" % i) for i in range(2)]
            tWu = [T("wu%d" % i) for i in range(2)]
            tWd = [T("wd%d" % i) for i in range(2)]
            xg_t = [sb(e2, "xg%d" % i, [128, D], BF16) for i in range(2)]
            tXgt = [T("xgt%d" % i) for i in range(2)]
            XgT = sb(e2, "XgT", [128, KC, CAP], BF16)
            tXgT = T("XgT")
            hTe = sb(e2, "hTe", [128, 4, CAP], BF16)
            tHe = T("hTe")
            sgt = [sb(e2, "sgt%d" % i, [128, CAP], F32) for i in range(2)]
            tSg = [T("sgt%d" % i) for i in range(2)]
            ysb = [sb(e2, "ysb%d" % i, [128, D], F32) for i in range(2)]
            tYs = [T("ysb%d" % i) for i in range(2)]

            def wload(e):
                i = e % 2
                b.dma("pool", wg[i][:], w_gate[e].rearrange("(kc p) n -> p kc n", p=128), (), [tWg[i]])
                b.dma("pool", wu[i][:], w_up[e].rearrange("(kc p) n -> p kc n", p=128), (), [tWu[i]])
                b.dma("pool", wd[i][:], w_down[e].rearrange("(fc p) n -> p fc n", p=128), (), [tWd[i]])

            wload(0)
            for e in range(DBG_NEX):
                i = e % 2
                if e + 1 < DBG_NEX:
                    wload(e + 1)
                for st in range(2):
                    r0 = e * CAP + st * 128
                    b.dma("sp", xg_t[st][:], Xg[r0:r0 + 128, :], [tXg], [tXgt[st]])
                    for rd in range(2):
                        p, tp = pb[rd], tPb[rd]
                        b.tr([(p[:, j, :], xg_t[st][:, (rd * 8 + j) * 128:(rd * 8 + j + 1) * 128], ident_bf[:])
                              for j in range(8)], [tXgt[st], tC], [tp])
                        b.evac(XgT[:, rd * 8:rd * 8 + 8, st * 128:(st + 1) * 128], p[:], [tp], [tXgT])
                for fc in range(4):
                    p, tp = ps[fc % 2], tP[fc % 2]
                    b.mm([(p[:, 0:CAP], [(wg[i][:, kc, fc * 128:(fc + 1) * 128], XgT[:, kc, :]) for kc in range(KC)]),
                          (p[:, CAP:2 * CAP], [(wu[i][:, kc, fc * 128:(fc + 1) * 128], XgT[:, kc, :]) for kc in range(KC)])],
                         [tWg[i], tWu[i], tXgT], [tp])
                    b.act(sgt[fc % 2][:], p[:, 0:CAP], AF.Silu, [tp], [tSg[fc % 2]])
                    b.tt(hTe[:, fc, :], sgt[fc % 2][:], p[:, CAP:2 * CAP], ALU.mult, [tSg[fc % 2], tp], [tHe])
                for st in range(2):
                    for nb in range(4):
                        p, tp = ps[2 + nb % 2], tP[2 + nb % 2]
                        b.mm([(p[:], [(hTe[:, fc, st * 128:(st + 1) * 128], wd[i][:, fc, nb * 512:(nb + 1) * 512])
                                      for fc in range(4)])], [tHe, tWd[i]], [tp])
                        b.evac(ysb[st][:, nb * 512:(nb + 1) * 512], p[:], [tp], [tYs[st]])
                    r0 = e * CAP + st * 128
                    b.dma("sp", Yg[r0:r0 + 128, :], ysb[st][:], [tYs[st]], [tYg], merge=True)
            S.barrier()
            S.emit()

        with ExitStack() as e3:
            gf = sb(e3, "gf", [128, D], F32)
            tGf = T("gf")
            b.dma("sp", gf[:], gf_bc, (), [tGf])
            xm_t = [sb(e3, "xm_t%d" % i, [128, D], F32) for i in range(2)]
            y1_t = [sb(e3, "y1_t%d" % i, [128, D], F32) for i in range(2)]
            y2_t = [sb(e3, "y2_t%d" % i, [128, D], F32) for i in range(2)]
            o_t = [sb(e3, "o_t%d" % i, [128, D], F32) for i in range(2)]
            jk = sb(e3, "jk", [128, D], BF16)
            sm3 = sb(e3, "sm3", [128, 8], F32)
            tXt = [T("xm_t%d" % i) for i in range(2)]
            tY1 = [T("y1_%d" % i) for i in range(2)]
            tY2 = [T("y2_%d" % i) for i in range(2)]
            tO = [T("o_%d" % i) for i in range(2)]
            tJ = T("jk")
            tS3 = T("sm3")
            for i in range(2):
                b.memset("pool", y1_t[i][:], 0.0, [tY1[i]])
                b.memset("pool", y2_t[i][:], 0.0, [tY2[i]])
            for ti in range(DBG_NT3):
                i = ti % 2
                r0 = ti * 128
                b.dma("sp", xm_t[i][:], xmid[r0:r0 + 128, :], [tXm[ti]], [tXt[i]])
                for kk, (yt, ty) in enumerate(((y1_t[i], tY1[i]), (y2_t[i], tY2[i]))):
                    def fga(g_, yt=yt, ti=ti, kk=kk):
                        return g_.indirect_dma_start(
                            out=yt[:, :], out_offset=None, in_=Yg,
                            in_offset=bass.IndirectOffsetOnAxis(ap=dest_i[:, ti, kk:kk + 1], axis=0),
                            bounds_check=S.breg, oob_is_err=False)
                    S.dma("pool", fga, [tYg, tDest[ti]], [ty])
                b.stt(xm_t[i][:], y1_t[i][:], wts[:, ti, 0:1], xm_t[i][:], ALU.mult, ALU.add,
                      [tY1[i], tDest[ti], tXt[i]], [tXt[i]])
                b.stt(xm_t[i][:], y2_t[i][:], wts[:, ti, 1:2], xm_t[i][:], ALU.mult, ALU.add,
                      [tY2[i], tDest[ti], tXt[i]], [tXt[i]])
                b.act(jk[:], xm_t[i][:], AF.Square, [tXt[i]], [tJ, tS3], accum_out=sm3[:, 0:1])
                b.ts(sm3[:, 1:2], sm3[:, 0:1], 1.0 / D, EPS, ALU.mult, ALU.add, [tS3], [tS3])
                b.act(sm3[:, 1:2], sm3[:, 1:2], AF.Ln, [tS3], [tS3])
                b.act(sm3[:, 2:3], sm3[:, 1:2], AF.Exp, [tS3], [tS3], scale=-0.5)
                b.stt(o_t[i][:], xm_t[i][:], sm3[:, 2:3], gf[:], ALU.mult, ALU.mult, [tXt[i], tS3, tGf], [tO[i]])
                b.dma("sp", out_d[r0:r0 + 128, :], o_t[i][:], [tO[i]], [])
            S.mark("phase3")
            S.barrier()
            S.emit(final=True)
            DBG_MARKS[:] = S.marks
    return nc


def _t5_bucket(n):
    import math
    max_exact = 16
    nf = np.maximum(n, 1).astype(np.float32)
    large = max_exact + (np.log(nf / max_exact) / math.log(128 / max_exact) * (32 - max_exact)).astype(np.int32)
    large = np.minimum(large, 31)
    return np.where(n < max_exact, n, large)


_NC_CACHE = {}


def _consts():
    ident = np.eye(128, dtype=np.float32)
    s_ = np.arange(128)[:, None] % 64
    t_ = np.arange(64)[None, :]
    triu = (s_ <= t_).astype(np.float32)
    lstrict = (np.arange(128)[:, None] < np.arange(128)[None, :]).astype(np.float32)
    scanmask = np.ones((128, 512), np.float32)
    scanmask[:, ::64] = 0.0
    ebase = np.broadcast_to((np.arange(32) * CAP).astype(np.float32)[None, :], (128, 32)).copy()
    return dict(c_ident=ident, c_triu=triu, c_lstrict=lstrict, c_scanmask=scanmask, c_ebase=ebase)


def make_in_maps(inputs, ncores=8):
    f = lambda a: np.ascontiguousarray(np.asarray(a, dtype=np.float32))
    x = f(inputs["x"])
    bc = lambda v: np.ascontiguousarray(np.broadcast_to(f(v).reshape(1, -1), (128, f(v).size)))
    rel = f(inputs["rel_bias"])
    k_ = np.arange(128)[:, None]
    j_ = np.arange(256)[None, :]
    dist = j_ - k_
    bucket = _t5_bucket(np.clip(dist, 0, None))
    valid = (dist >= 0) & (dist < 128)
    tbl = rel[bucket]
    tbl = np.where(valid[:, :, None], tbl, np.float32(NEG)).astype(np.float32)
    biasT = np.ascontiguousarray(tbl.transpose(0, 2, 1))
    lbl = np.ascontiguousarray(f(inputs["hg_lb_logits"]).reshape(2, 8, 128).transpose(2, 1, 0))
    wr = np.ascontiguousarray(np.concatenate([f(inputs["w_group_router"][0]), f(inputs["w_expert_router"][0])], axis=1))
    bb = np.concatenate([f(inputs["b_group_router"][0]), f(inputs["b_expert_router"][0])])
    shared = dict(
        w_in=f(inputs["w_in"][0]), w_a=f(inputs["w_attn_branch"][0]), w_r=f(inputs["w_hg_branch"][0]),
        w_o=f(inputs["w_out"][0]), w_gate=f(inputs["w_gate"][0]), w_up=f(inputs["w_up"][0]),
        w_down=f(inputs["w_down"][0]), g1_bc=bc(inputs["norm1_g"][0]), g2_bc=bc(inputs["norm2_g"][0]),
        gf_bc=bc(inputs["final_g"]), gn_bc=bc(np.tile(f(inputs["hg_norm_g"][0]), 8)), biasT=biasT,
        sinks_bc=bc(inputs["attn_sinks"][0]), lbl=lbl, wr=wr, b_bc=bc(bb), **_consts())
    maps = []
    for c in range(ncores):
        bi, hf = c // 2, c % 2
        m = dict(shared)
        m["x_own"] = np.ascontiguousarray(x[bi, hf * TOK:(hf + 1) * TOK])
        m["x_pre"] = np.ascontiguousarray(x[bi, 0:TOK]) if hf == 1 else np.zeros((TOK, D), np.float32)
        m["flag0"] = np.full((128, 1), float(hf), np.float32)
        maps.append(m)
    return maps


def kernel(**inputs):
    if "nc" not in _NC_CACHE:
        _NC_CACHE["nc"] = build_program()
    nc = _NC_CACHE["nc"]
    maps = make_in_maps(inputs, 8)
    res = run_bass_kernel_spmd(nc, maps, core_ids=list(range(8)))
    out = np.empty((4, 4096, D), np.float32)
    for c in range(8):
        out[c // 2, (c % 2) * TOK:(c % 2 + 1) * TOK] = res.results[c]["out"]
    return out
```

```python
import numpy as np
from contextlib import ExitStack
import concourse.bass as bass
import concourse.mybir as mybir
from concourse.bass_utils import run_bass_kernel_spmd

F32 = mybir.dt.float32
BF16 = mybir.dt.bfloat16
I32 = mybir.dt.int32
AF = mybir.ActivationFunctionType
ALU = mybir.AluOpType
AX = mybir.AxisListType

SAME_ENG_SYNC = True
D = 2048
KC = 16
TOK = 2048
G = 512
NG = TOK // G
INW = 9728
NE = 32
CAP = 256
EPS = 1e-6
NEG = -30000.0
WPD = 3
DBG_NGP = NG
DBG_NGO = NG
DBG_NEX = NE
DBG_NT3 = 16
DBG_LIMIT = 10 ** 9
DBG_MARKS = []
NRING = 4

O_QA, O_KA, O_VA, O_QH, O_FH, O_IH, O_GH, O_GA, O_GB = 0, 1024, 1280, 1536, 2560, 3584, 4608, 5632, 7680


class T:
    __slots__ = ("name", "w", "r")

    def __init__(self, name):
        self.name = name
        self.w = {}
        self.r = {}


class Sched:
    ENG = ("pe", "act", "dve", "pool", "sp")
    NDMA = 8

    def __init__(self, nc, es):
        self.nc = nc
        self.nrec = 0
        self.marks = []
        self.prog = {k: [] for k in self.ENG}
        self.sems = {}
        self.cnt = {}
        self.seen = {k: {} for k in self.ENG}
        for k in self.ENG:
            self.sems[k] = es.enter_context(nc.semaphore("s_" + k))
            self.cnt[k] = 0
        self.breg = es.enter_context(nc.gpsimd.register("bnd"))
        self.dma_pool = {}
        self.dma_i = {}
        for q in ("sp", "act", "pool"):
            keys = []
            for i in range(self.NDMA):
                k = "d_%s%d" % (q, i)
                self.sems[k] = es.enter_context(nc.semaphore(k))
                self.cnt[k] = 0
                keys.append(k)
            self.dma_pool[q] = keys
            self.dma_i[q] = 0

    def _deps(self, e, reads, writes, merge):
        need = {}
        for t in reads:
            for k, v in t.w.items():
                need[k] = max(need.get(k, 0), v)
        for t in writes:
            if not merge:
                for k, v in t.w.items():
                    need[k] = max(need.get(k, 0), v)
            for k, v in t.r.items():
                need[k] = max(need.get(k, 0), v)
        waits = []
        for k, v in need.items():
            if k == e and (e == "pe" or not SAME_ENG_SYNC):
                continue
            if self.seen[e].get(k, 0) < v:
                self.seen[e][k] = v
                waits.append((k, v))
        return waits

    def mark(self, name):
        self.marks.append((name, self.nrec, dict(self.cnt)))

    def op(self, e, fn, reads=(), writes=()):
        if self.nrec >= DBG_LIMIT:
            return
        self.nrec += 1
        waits = self._deps(e, reads, writes, False)
        self.cnt[e] += 1
        v = self.cnt[e]
        self.prog[e].append((waits, fn, (e, 1)))
        for t in reads:
            t.r[e] = v
        for t in writes:
            t.w = {e: v}
            t.r = {}

    def dma(self, q, fn, reads=(), writes=(), merge=False):
        if self.nrec >= DBG_LIMIT:
            return
        self.nrec += 1
        i = self.dma_i[q]
        self.dma_i[q] += 1
        k = self.dma_pool[q][i % self.NDMA]
        waits = self._deps(q, reads, writes, merge)
        if self.cnt[k] > self.seen[q].get(k, 0):
            waits.append((k, self.cnt[k]))
            self.seen[q][k] = self.cnt[k]
        self.cnt[k] += 16
        v = self.cnt[k]
        self.prog[q].append((waits, fn, (k, 16)))
        for t in reads:
            t.r[k] = v
        for t in writes:
            if merge:
                t.w[k] = v
            else:
                t.w = {k: v}
                t.r = {}

    def barrier(self):
        for e in self.ENG:
            waits = []
            for k, v in self.cnt.items():
                if k != e and v > self.seen[e].get(k, 0):
                    waits.append((k, v))
                    self.seen[e][k] = v
            self.prog[e].append((waits, None, None))

    def emit(self, final=False):
        sems = self.sems
        prog = self.prog
        fin = [(k, v) for k, v in self.cnt.items() if v > 0 and k != "sp"] if final else []

        def mk(e):
            plist = prog[e]

            def body(engobj):
                if e == "pool" and self.breg is not None:
                    engobj.reg_mov(self.breg, NE * CAP - 1)
                for waits, fn, inc in plist:
                    for (wk, wv) in waits:
                        engobj.wait_ge(sems[wk], wv)
                    if fn is not None:
                        ins = fn(engobj)
                        ins.then_inc(sems[inc[0]], inc[1])
                if e == "sp":
                    for (wk, wv) in fin:
                        engobj.wait_ge(sems[wk], wv)
            return body

        with self.nc.Block() as block:
            block.tensor(mk("pe"))
            block.scalar(mk("act"))
            block.vector(mk("dve"))
            block.gpsimd(mk("pool"))
            block.sync(mk("sp"))
        self.prog = {k: [] for k in self.ENG}


class B:
    def __init__(self, S):
        self.S = S
        self.flip = 0

    def mm(self, groups, reads, writes):
        groups = [(o, list(ops)) for o, ops in groups]

        def fn(pe):
            ins = None
            for o, ops in groups:
                n = len(ops)
                for i, (l, r) in enumerate(ops):
                    ins = pe.matmul(o, l, r, start=(i == 0), stop=(i == n - 1))
            return ins
        self.S.op("pe", fn, reads, writes)

    def tr(self, items, reads, writes):
        items = list(items)

        def fn(pe):
            ins = None
            for o, i_, idt in items:
                ins = pe.transpose(o, i_, idt)
            return ins
        self.S.op("pe", fn, reads, writes)

    def act(self, out, in_, func, reads, writes, **kw):
        def fn(e):
            return e.activation(out=out, in_=in_, func=func, **kw)
        self.S.op("act", fn, reads, writes)

    def tt(self, out, in0, in1, op, reads, writes, eng="dve"):
        def fn(e):
            return e.tensor_tensor(out=out, in0=in0, in1=in1, op=op)
        self.S.op(eng, fn, reads, writes)

    def ts(self, out, in0, s1, s2, op0, op1, reads, writes, eng="dve"):
        def fn(e):
            if s2 is None:
                return e.tensor_scalar(out=out, in0=in0, scalar1=s1, scalar2=None, op0=op0)
            return e.tensor_scalar(out=out, in0=in0, scalar1=s1, scalar2=s2, op0=op0, op1=op1)
        self.S.op(eng, fn, reads, writes)

    def stt(self, out, in0, scalar, in1, op0, op1, reads, writes):
        def fn(e):
            return e.scalar_tensor_tensor(out=out, in0=in0, scalar=scalar, in1=in1, op0=op0, op1=op1)
        self.S.op("dve", fn, reads, writes)

    def copy(self, eng, out, in_, reads, writes):
        if eng == "act":
            def fn(e):
                return e.activation(out=out, in_=in_, func=AF.Copy)
        else:
            def fn(e):
                return e.tensor_copy(out=out, in_=in_)
        self.S.op(eng, fn, reads, writes)

    def evac(self, out, in_, reads, writes):
        self.flip ^= 1
        self.copy("act" if self.flip else "dve", out, in_, reads, writes)

    def reduce(self, out, in_, op, reads, writes):
        def fn(e):
            return e.tensor_reduce(out=out, in_=in_, axis=AX.X, op=op)
        self.S.op("dve", fn, reads, writes)

    def recip(self, out, in_, reads, writes):
        def fn(e):
            return e.reciprocal(out=out, in_=in_)
        self.S.op("dve", fn, reads, writes)

    def memset(self, eng, ap, val, writes):
        def fn(e):
            return e.memset(ap, val)
        self.S.op(eng, fn, (), writes)

    def dma(self, q, out, in_, reads, writes, merge=False):
        def fn(e):
            return e.dma_start(out=out, in_=in_)
        self.S.dma(q, fn, reads, writes, merge)

    def scan(self, out, d0, d1, reads, writes):
        def fn(e):
            return e.tensor_tensor_scan(out=out, data0=d0, data1=d1, initial=0.0, op0=ALU.mult, op1=ALU.add)
        self.S.op("dve", fn, reads, writes)


def build_program(dbg=False):
    nc = bass.Bass("TRN2", target_bir_lowering=False)

    def din(name, shape, dt=F32):
        return nc.dram_tensor(name, shape, dt, kind="ExternalInput").ap()

    x_own = din("x_own", [TOK, D])
    x_pre = din("x_pre", [TOK, D])
    w_in = din("w_in", [D, INW])
    w_a = din("w_a", [1024, D])
    w_r = din("w_r", [1024, D])
    w_o = din("w_o", [D, D])
    w_gate = din("w_gate", [NE, D, 512])
    w_up = din("w_up", [NE, D, 512])
    w_down = din("w_down", [NE, 512, D])
    g1_bc = din("g1_bc", [128, D])
    g2_bc = din("g2_bc", [128, D])
    gf_bc = din("gf_bc", [128, D])
    gn_in = din("gn_bc", [128, 1024])
    bias_in = din("biasT", [128, 16, 256])
    flag_in = din("flag0", [128, 1])
    sinks_in = din("sinks_bc", [128, 16])
    lbl_in = din("lbl", [128, 8, 2])
    wr_in = din("wr", [D, 36])
    bb_in = din("b_bc", [128, 36])
    c_ident = din("c_ident", [128, 128])
    c_triu = din("c_triu", [128, 64])
    c_lstrict = din("c_lstrict", [128, 128])
    c_scanmask = din("c_scanmask", [128, 512])
    c_ebase = din("c_ebase", [128, 32])
    out_d = nc.dram_tensor("out", [TOK, D], F32, kind="ExternalOutput").ap()
    if dbg:
        xmid = nc.dram_tensor("xmid", [TOK, D], F32, kind="ExternalOutput").ap()
    else:
        xmid = nc.dram_tensor("xmid", [TOK, D], F32).ap()
    Xg = nc.dram_tensor("Xg", [NE * CAP, D], BF16).ap()
    Yg = nc.dram_tensor("Yg", [NE * CAP, D], F32).ap()
    tXm = [T("xm%d" % i) for i in range(16)]
    tXg = T("Xg")
    tYg = T("Yg")

    w_in_v = w_in.rearrange("(kc p) n -> p kc n", p=128)
    w_a_v = w_a.rearrange("(kc p) n -> p kc n", p=128)
    w_r_v = w_r.rearrange("(kc p) n -> p kc n", p=128)
    w_o_v = w_o.rearrange("(kc p) n -> p kc n", p=128)

    with ExitStack() as ea:
        S = Sched(nc, ea)
        b = B(S)

        def sb(es, name, shape, dt):
            return es.enter_context(nc.sbuf_tensor("sb_" + name, shape, dt))

        ident_bf = sb(ea, "ident_bf", [128, 128], BF16)
        ident_f = sb(ea, "ident_f", [128, 128], F32)
        triu = sb(ea, "triu", [128, 64], BF16)
        lstrict = sb(ea, "lstrict", [128, 128], BF16)
        ones_bf = sb(ea, "ones_bf", [128, 128], BF16)
        ebase = sb(ea, "ebase", [128, 32], F32)
        neghalf = sb(ea, "neghalf", [128, 1], F32)
        dest_i = sb(ea, "dest_i", [128, 16, 2], I32)
        wts = sb(ea, "wts", [128, 16, 2], F32)
        tC = T("consts")
        tDest = [T("dest%d" % i) for i in range(16)]
        ps = []
        tP = []
        for i in range(6):
            ps.append(ea.enter_context(nc.psum_tensor("ps%d" % i, [128, 512], F32)))
            tP.append(T("ps%d" % i))
        pb = []
        tPb = []
        for i in range(2):
            pb.append(ea.enter_context(nc.psum_tensor("pb%d" % i, [128, 8, 128], BF16)))
            tPb.append(T("pb%d" % i))

        b.dma("pool", ident_bf[:], c_ident, (), [tC])
        b.dma("sp", ident_f[:], c_ident, (), [tC], merge=True)
        b.dma("pool", triu[:], c_triu, (), [tC], merge=True)
        b.dma("pool", lstrict[:], c_lstrict, (), [tC], merge=True)
        b.dma("sp", ebase[:], c_ebase, (), [tC], merge=True)
        tC2 = T("consts2")
        b.memset("pool", ones_bf[:], 1.0, [tC2])
        b.memset("pool", neghalf[:], -0.5, [tC2])

        with ExitStack() as e1:
            scanmask = sb(e1, "scanmask", [128, 512], F32)
            biasT = sb(e1, "biasT", [128, 16, 256], F32)
            gn_bc = sb(e1, "gn_bc", [128, 1024], F32)
            wr_sb = sb(e1, "wr_sb", [128, 16, 36], F32)
            b_bc = sb(e1, "b_bc", [128, 36], F32)
            esink = sb(e1, "esink", [128, 16], F32)
            flag0 = sb(e1, "flag0", [128, 1], F32)
            lbl = sb(e1, "lbl", [128, 8, 2], F32)
            lb = sb(e1, "lb", [128, 8], F32)
            oml = sb(e1, "oml", [128, 8], F32)
            ln_oml = sb(e1, "ln_oml", [128, 8], F32)
            macc = sb(e1, "macc", [128, 32], BF16)
            tK = T("consts_p1")
            b.dma("sp", scanmask[:], c_scanmask, (), [tK])
            b.dma("sp", biasT[:], bias_in, (), [tK], merge=True)
            b.dma("sp", gn_bc[:], gn_in, (), [tK], merge=True)
            b.dma("sp", wr_sb[:], wr_in.rearrange("(kc p) n -> p kc n", p=128), (), [tK], merge=True)
            b.dma("sp", b_bc[:], bb_in, (), [tK], merge=True)
            b.dma("sp", esink[:], sinks_in, (), [tK], merge=True)
            b.dma("sp", flag0[:], flag_in, (), [tK], merge=True)
            b.dma("sp", lbl[:], lbl_in, (), [tK], merge=True)
            tL = T("lb")
            b.tt(lb[:], lbl[:, :, 1], lbl[:, :, 0], ALU.subtract, [tK], [tL])
            b.act(lb[:], lb[:], AF.Exp, [tL], [tL])
            b.ts(lb[:], lb[:], 1.0, None, ALU.add, None, [tL], [tL])
            b.recip(lb[:], lb[:], [tL], [tL])
            b.ts(oml[:], lb[:], -1.0, 1.0, ALU.mult, ALU.add, [tL], [tL])
            b.act(ln_oml[:], oml[:], AF.Ln, [tL], [tL])
            tEs = T("esink")
            b.act(esink[:], esink[:], AF.Exp, [tK], [tEs])
            tMacc = T("macc")
            b.memset("pool", macc[:], 0.0, [tMacc])

            NR = NRING
            wring = [sb(e1, "wring%d" % i, [128, 4096], BF16) for i in range(NR)]
            tW = [T("wring%d" % i) for i in range(NR)]
            hT = sb(e1, "hT", [128, KC, G], BF16)
            tHT = T("hT")
            qaT = sb(e1, "qaT", [128, 8, G], BF16)
            tQA = [T("qa%d" % i) for i in range(4)]
            kbuf = sb(e1, "kbuf", [128, 4, 128 + G], BF16)
            tKb = T("kbuf")
            vbuf = sb(e1, "vbuf", [128, 5, 4, 65], BF16)
            tVb = T("vbuf")
            QtT = sb(e1, "QtT", [128, 8, G], BF16)
            tQt = [T("Qt%d" % i) for i in range(4)]
            KtT = sb(e1, "KtT", [128, 8, G], BF16)
            tKt = T("KtT")
            vh = sb(e1, "vh", [128, 4, 1024], BF16)
            tVh = T("vh")
            gsg = sb(e1, "gsg", [128, 4, 1024], BF16)
            tGs = T("gsg")
            sgA = sb(e1, "sgA", [128, 16, G], BF16)
            sgH = sb(e1, "sgH", [128, 16, G], BF16)
            tSgA = T("sgA")
            tSgH = T("sgH")
            S32 = sb(e1, "S32", [128, 8, 128], F32)
            Sbf = sb(e1, "Sbf", [128, 8, 128], BF16)
            tS32 = T("S32")
            tSbf = T("Sbf")
            dec = sb(e1, "dec", [128, 8, 8], F32)
            tDec = T("dec")
            xt0 = sb(e1, "xt0", [128, D], F32)
            xt1 = sb(e1, "xt1", [128, D], F32)
            gbc = sb(e1, "gbc", [128, D], F32)
            hb = sb(e1, "hb", [128, D], BF16)
            hb2 = sb(e1, "hb2", [128, D], BF16)
            tHb2 = T("hb2")
            tX0, tX1, tGbc, tHb = T("xt0"), T("xt1"), T("gbc"), T("hb")
            arena = sb(e1, "arena", [128, 6 * 512], F32)
            tA = [T("ar%d" % i) for i in range(6)]
            small = sb(e1, "small", [128, 64], F32)
            tSm = T("small")
            smallr = sb(e1, "smallr", [128, 160], F32)
            tSr = T("smallr")

            def ar(i, n=1):
                return arena[:, i * 512:(i + n) * 512]

            b.memset("pool", S32[:], 0.0, [tS32])
            b.memset("pool", Sbf[:], 0.0, [tSbf])
            b.memset("pool", vbuf[:], 1.0, [tVb])
            b.memset("pool", kbuf[:], 0.0, [tKb])

            b.memset("pool", hb[:], 0.0, [tHb])
            for i in range(NE * CAP // 128):
                b.dma("act", Xg[i * 128:(i + 1) * 128, :], hb[:], [tHb], [tXg], merge=True)

            wlist = []

            def wq_win(col0):
                wlist.append((w_in_v[:, :, col0:col0 + 256], 16, 256))
                return len(wlist) - 1
            wstate = {"issued": 0}

            def wissue(upto):
                while wstate["issued"] <= min(upto, len(wlist) - 1):
                    i = wstate["issued"]
                    src, kc, n = wlist[i]
                    slot = wring[i % NR][:, 0:kc * n].rearrange("p (k n) -> p k n", k=kc)
                    b.dma("pool", slot, src, (), [tW[i % NR]])
                    wstate["issued"] += 1

            def wget(i):
                wissue(i + WPD)
                src, kc, n = wlist[i]
                return wring[i % NR][:, 0:kc * n].rearrange("p (k n) -> p k n", k=kc), tW[i % NR]

            pstate = {"i": 0}

            def nextps():
                pstate["i"] ^= 1
                return ps[pstate["i"]], tP[pstate["i"]]

            def rms_tile(xt, tXt, gtile, tG, out, tOut, junk, tJunk):
                b.act(junk, xt, AF.Square, [tXt], [tJunk, tSm], accum_out=small[:, 0:1])
                b.ts(small[:, 1:2], small[:, 0:1], 1.0 / D, EPS, ALU.mult, ALU.add, [tSm], [tSm])
                b.act(small[:, 1:2], small[:, 1:2], AF.Ln, [tSm], [tSm])
                b.act(small[:, 2:3], small[:, 1:2], AF.Exp, [tSm], [tSm], scale=-0.5)
                b.stt(out, xt, small[:, 2:3], gtile, ALU.mult, ALU.mult, [tXt, tSm, tG, tJunk], [tOut])

            def stage_norm(xsrc, grp):
                b.dma("sp", gbc[:], g1_bc, (), [tGbc])
                for tt in range(4):
                    xt, tXt = (xt0, tX0) if tt % 2 == 0 else (xt1, tX1)
                    r0 = grp * G + tt * 128
                    b.dma("sp", xt[:], xsrc[r0:r0 + 128, :], (), [tXt])
                    hbx, thbx = (hb, tHb) if tt % 2 == 0 else (hb2, tHb2)
                    rms_tile(xt[:], tXt, gbc[:], tGbc, hbx[:], thbx, hbx[:], thbx)
                    for rd in range(2):
                        p, tp = pb[rd], tPb[rd]
                        b.tr([(p[:, j, :], hbx[:, (rd * 8 + j) * 128:(rd * 8 + j + 1) * 128], ident_bf[:])
                              for j in range(8)], [thbx, tC], [tp])
                        b.evac(hT[:, rd * 8:rd * 8 + 8, tt * 128:(tt + 1) * 128], p[:], [tp], [tHT])

            def proj_fm(slot, tSlot, c):
                p, tp = nextps()
                b.mm([(p[:], [(slot[:, kc, c * 128:(c + 1) * 128], hT[:, kc, :]) for kc in range(KC)])],
                     [tSlot, tHT], [tp])
                return p, tp

            def proj_tm(slot, tSlot, tt, n):
                p, tp = nextps()
                b.mm([(p[:, 0:n], [(hT[:, kc, tt * 128:(tt + 1) * 128], slot[:, kc, 0:n]) for kc in range(KC)])],
                     [tSlot, tHT], [tp])
                return p, tp

            def chain(h, p, tp, own):
                E, L1, L2, bb_ = ar(0), ar(1), ar(2), ar(3)
                b.act(E, p[:], AF.Exp, [tp], [tA[0]])
                b.act(L1, E, AF.Ln, [tA[0], tL], [tA[1]], bias=lb[:, h:h + 1])
                b.act(L2, E, AF.Ln, [tA[0]], [tA[2]], bias=1.0)
                b.tt(L1, L1, L2, ALU.subtract, [tA[1], tA[2]], [tA[1]])
                b.scan(bb_, scanmask[:], L1, [tK, tA[1]], [tA[3]])
                b.act(E, bb_, AF.Exp, [tA[3]], [tA[0]])
                if own:
                    b.tt(QtT[:, h, :], QtT[:, h, :], E, ALU.mult, [tA[0]] + tQt, tQt)
                b.tt(L2, L2, bb_, ALU.add, [tA[2], tA[3]], [tA[2]])
                b.act(KtT[:, h, :], L2, AF.Exp, [tA[2], tL], [tKt], scale=-1.0, bias=ln_oml[:, h:h + 1])
                b.copy("pool", dec[:, h, :], E.rearrange("p (c t) -> p c t", t=64)[:, :, 63], [tA[0]], [tDec])

            def stage_proj(own, last_prefix, wbase):
                wi = wbase
                if own:
                    for blk in range(4):
                        slot, tsl = wget(wi); wi += 1
                        for c in range(2):
                            p, tp = proj_fm(slot, tsl, c)
                            b.act(qaT[:, blk * 2 + c, :], p[:], AF.Copy, [tp], tQA, scale=0.125)
                if own or last_prefix:
                    slot, tsl = wget(wi); wi += 1
                    for g in range(4):
                        p, tp = nextps()
                        b.mm([(p[0:64, :], [(slot[:, kc, g * 64:(g + 1) * 64], hT[:, kc, :]) for kc in range(KC)]),
                              (p[64:128, :], [(slot[:, kc, g * 64:(g + 1) * 64], hT[:, kc, :]) for kc in range(KC)])],
                             [tsl, tHT], [tp])
                        b.evac(kbuf[:, g, 128:128 + G], p[:], [tp], [tKb])
                    slot, tsl = wget(wi); wi += 1
                    for tt in range(4):
                        p, tp = proj_tm(slot, tsl, tt, 256)
                        b.evac(vbuf[:, 1 + tt, :, 0:64], p[:, 0:256].rearrange("p (g d) -> p g d", g=4), [tp], [tVb])
                if own:
                    for blk in range(4):
                        slot, tsl = wget(wi); wi += 1
                        for c in range(2):
                            p, tp = proj_fm(slot, tsl, c)
                            b.evac(QtT[:, blk * 2 + c, :], p[:], [tp], tQt)
                for blk in range(4):
                    slot, tsl = wget(wi); wi += 1
                    for c in range(2):
                        p, tp = proj_fm(slot, tsl, c)
                        chain(blk * 2 + c, p, tp, own)
                for blk in range(4):
                    slot, tsl = wget(wi); wi += 1
                    for tt in range(4):
                        p, tp = proj_tm(slot, tsl, tt, 256)
                        b.evac(vh[:, tt, blk * 256:(blk + 1) * 256], p[:, 0:256], [tp], [tVh])
                if own:
                    for blk in range(4):
                        slot, tsl = wget(wi); wi += 1
                        for tt in range(4):
                            p, tp = proj_tm(slot, tsl, tt, 256)
                            tmp = ar(4)[:, 0:256]
                            b.act(tmp, p[:, 0:256], AF.Silu, [tp], [tA[4]])
                            b.tt(gsg[:, tt, blk * 256:(blk + 1) * 256], tmp, gn_bc[:, blk * 256:(blk + 1) * 256],
                                 ALU.mult, [tA[4], tK], [tGs])
                    for blk in range(8):
                        slot, tsl = wget(wi); wi += 1
                        for c in range(2):
                            p, tp = proj_fm(slot, tsl, c)
                            b.act(sgA[:, blk * 2 + c, :], p[:], AF.Sigmoid, [tp], [tSgA])
                    for blk in range(8):
                        slot, tsl = wget(wi); wi += 1
                        for c in range(2):
                            p, tp = proj_fm(slot, tsl, c)
                            b.act(sgH[:, blk * 2 + c, :], p[:], AF.Sigmoid, [tp], [tSgH])
                return wi

            def stage_attn(first_group):
                sE = ar(0, 2).rearrange("p (a n) -> p a n", a=2)
                pT = ar(2).bitcast(BF16).rearrange("p (a h q) -> p a h q", a=2, h=4)
                a_tok = ar(3).bitcast(BF16)
                for tt in range(4):
                    for g in range(4):
                        banks = [[(ps[2], tP[2]), (ps[3], tP[3])], [(ps[0], tP[0]), (ps[1], tP[1])]]
                        sE4 = sE.rearrange("p a (j two q) -> p a j two q", j=2, two=2)
                        for kb in range(2):
                            for par in range(2):
                                pbk, tpbk = banks[kb][par]
                                p0 = par * 64
                                groups = []
                                for jj in range(2):
                                    h = g * 4 + jj * 2 + par
                                    groups.append((pbk[:, jj * 128:(jj + 1) * 128],
                                                   [(kbuf[p0:p0 + 64, g, (tt + kb) * 128:(tt + kb + 1) * 128],
                                                     qaT[p0:p0 + 64, h // 2, tt * 128:(tt + 1) * 128])]))
                                b.mm(groups, [tKb, tQA[tt]], [tpbk])
                                bcol = slice(128, 256) if kb == 0 else slice(0, 128)
                                bsl = biasT[:, g * 4:(g + 1) * 4, bcol].rearrange("p (j two) q -> p j two q", two=2)[:, :, par, :]
                                b.stt(sE4[:, kb, :, par, :], pbk[:, 0:256].rearrange("p (j q) -> p j q", j=2), 60.0, bsl,
                                      ALU.min, ALU.add, [tpbk, tK], [tA[kb]])
                        b.act(pT, sE.rearrange("p a (h q) -> p a h q", h=4), AF.Exp, [tA[0], tA[1]], [tA[2]])
                        if first_group and tt == 0:
                            b.ts(pT[:, 0], pT[:, 0], flag0[:, 0:1], None, ALU.mult, None, [tA[2], tK], [tA[2]])
                        po, tpo = ps[4], tP[4]
                        groups = []
                        for hh in range(4):
                            groups.append((po[:, hh * 65:(hh + 1) * 65],
                                           [(pT[:, kb, hh, :], vbuf[:, tt + kb, g, :]) for kb in range(2)]))
                        b.mm(groups, [tA[2], tVb], [tpo])
                        pov = po[:, 0:260].rearrange("p (h d) -> p h d", h=4)
                        b.tt(smallr[:, 0:4], pov[:, :, 64], esink[:, g * 4:(g + 1) * 4], ALU.add, [tpo, tEs], [tSr])
                        b.recip(smallr[:, 4:8], smallr[:, 0:4], [tSr], [tSr])
                        b.tt(a_tok[:, g * 256:(g + 1) * 256].rearrange("p (h d) -> p h d", h=4), pov[:, :, 0:64],
                             smallr[:, 4:8].unsqueeze(2).broadcast_to([128, 4, 64]), ALU.mult, [tpo, tSr], [tA[3]])
                    p, tp = pb[tt % 2], tPb[tt % 2]
                    b.tr([(p[:, j, :], a_tok[:, j * 128:(j + 1) * 128], ident_bf[:]) for j in range(8)],
                         [tA[3], tC], [tp])
                    b.evac(qaT[:, :, tt * 128:(tt + 1) * 128], p[:], [tp], [tQA[tt]])
                b.copy("pool", kbuf[:, :, 0:128], kbuf[:, :, G:G + 128], [tKb], [tKb])
                b.copy("pool", vbuf[:, 0], vbuf[:, 4], [tVb], [tVb])

            def stage_hloop(own):
                attm = ar(0).bitcast(BF16)[:, 0:512].rearrange("p (h t) -> p h t", h=8)
                r_tok = ar(1).bitcast(BF16)
                junk = ar(2).bitcast(BF16)
                Kt = ar(3).bitcast(BF16).rearrange("p (h k) -> p h k", h=8)
                for tt in range(4):
                    p, tp = pb[tt % 2], tPb[tt % 2]
                    b.tr([(p[:, h, :], KtT[:, h, tt * 128:(tt + 1) * 128], ident_bf[:]) for h in range(8)],
                         [tKt, tC], [tp])
                    b.evac(Kt, p[:], [tp], [tA[3]])
                    for cc in range(2):
                        c = tt * 2 + cc
                        p0 = cc * 64
                        cs = slice(c * 64, (c + 1) * 64)
                        if own:
                            pa, tpa = ps[4], tP[4]
                            b.mm([(pa[p0:p0 + 64, h * 64:(h + 1) * 64], [(KtT[:, h, cs], QtT[:, h, cs])])
                                  for h in range(8)], [tKt, tQt[tt]], [tpa])
                            b.tt(attm[p0:p0 + 64], pa[p0:p0 + 64, :].rearrange("p (h t) -> p h t", h=8),
                                 triu[p0:p0 + 64, :].unsqueeze(1).broadcast_to([64, 8, 64]), ALU.mult,
                                 [tpa, tC], [tA[0]])
                            groups = []
                            for h in range(8):
                                po = ps[2] if h < 4 else ps[3]
                                o_ap = po[p0:p0 + 64, (h % 4) * 128:(h % 4 + 1) * 128]
                                groups.append((o_ap, [(QtT[:, h, cs], Sbf[:, h, :]),
                                                      (attm[p0:p0 + 64, h, :], vh[p0:p0 + 64, tt, h * 128:(h + 1) * 128])]))
                            b.mm(groups, [tQt[tt], tSbf, tA[0], tVh], [tP[2], tP[3]])
                        groups = []
                        for h in range(8):
                            po = ps[0] if h < 4 else ps[1]
                            groups.append((po[:, (h % 4) * 128:(h % 4 + 1) * 128],
                                           [(Kt[p0:p0 + 64, h, :], vh[p0:p0 + 64, tt, h * 128:(h + 1) * 128])]))
                        b.mm(groups, [tA[3], tVh], [tP[0], tP[1]])
                        b.tt(S32[:, 0:4, :], ps[0][:].rearrange("p (h v) -> p h v", h=4), S32[:, 0:4, :], ALU.add,
                             [tP[0], tS32], [tS32])
                        b.tt(S32[:, 4:8, :], ps[1][:].rearrange("p (h v) -> p h v", h=4), S32[:, 4:8, :], ALU.add,
                             [tP[1], tS32], [tS32])
                        b.tt(S32[:], S32[:], dec[:, :, c].unsqueeze(2).broadcast_to([128, 8, 128]), ALU.mult,
                             [tS32, tDec], [tS32])
                        b.copy("act", Sbf[:], S32[:], [tS32], [tSbf])
                    if own:
                        for h in range(8):
                            po = ps[2] if h < 4 else ps[3]
                            b.act(junk[:, 0:128], po[:, (h % 4) * 128:(h % 4 + 1) * 128], AF.Square,
                                  [tP[2], tP[3]], [tA[2], tSr], accum_out=smallr[:, 16 + h:17 + h])
                        b.ts(smallr[:, 32:40], smallr[:, 16:24], 1.0 / 128, EPS, ALU.mult, ALU.add, [tSr], [tSr])
                        b.act(smallr[:, 32:40], smallr[:, 32:40], AF.Ln, [tSr], [tSr])
                        b.act(smallr[:, 40:48], smallr[:, 32:40], AF.Exp, [tSr], [tSr], scale=-0.5)
                        for h in range(8):
                            po = ps[2] if h < 4 else ps[3]
                            b.stt(r_tok[:, h * 128:(h + 1) * 128], po[:, (h % 4) * 128:(h % 4 + 1) * 128],
                                  smallr[:, 40 + h:41 + h], gsg[:, tt, h * 128:(h + 1) * 128], ALU.mult, ALU.mult,
                                  [tP[2], tP[3], tSr, tGs], [tA[1]])
                        p, tp = pb[(tt + 1) % 2], tPb[(tt + 1) % 2]
                        b.tr([(p[:, j, :], r_tok[:, j * 128:(j + 1) * 128], ident_bf[:]) for j in range(8)],
                             [tA[1], tC], [tp])
                        b.evac(QtT[:, :, tt * 128:(tt + 1) * 128], p[:], [tp], [tQt[tt]])

            def stage_branch(wi):
                m2 = ar(1)
                for nb in range(4):
                    sa, tsa = wget(wi); wi += 1
                    for c in range(4):
                        j = nb * 4 + c
                        p, tp = ps[2 + c % 2], tP[2 + c % 2]
                        b.mm([(p[:], [(sa[:, kc, c * 128:(c + 1) * 128], qaT[:, kc, :]) for kc in range(8)])],
                             [tsa] + tQA, [tp])
                        b.tt(ar(2 + c), p[:], sgA[:, j, :], ALU.mult, [tp, tSgA], [tA[2 + c]])
                    sr, tsr = wget(wi); wi += 1
                    for c in range(4):
                        j = nb * 4 + c
                        p, tp = ps[2 + c % 2], tP[2 + c % 2]
                        b.mm([(p[:], [(sr[:, kc, c * 128:(c + 1) * 128], QtT[:, kc, :]) for kc in range(8)])],
                             [tsr] + tQt, [tp])
                        b.tt(m2, p[:], sgH[:, j, :], ALU.mult, [tp, tSgH], [tA[1]])
                        b.tt(hT[:, j, :], ar(2 + c), m2, ALU.add, [tA[2 + c], tA[1]], [tHT])
                return wi

            def stage_out(wi, grp):
                xs = [xt0[:, 0:1024].rearrange("p (t c) -> p t c", t=4), xt0[:, 1024:2048].rearrange("p (t c) -> p t c", t=4)]
                os_ = [xt1[:, 0:1024].rearrange("p (t c) -> p t c", t=4), xt1[:, 1024:2048].rearrange("p (t c) -> p t c", t=4)]
                txs = [tA[2], tA[3]]
                tos = [tA[4], tA[5]]
                rows = slice(grp * G, (grp + 1) * G)

                def xload(nb):
                    b.dma("sp", xs[nb % 2], x_own[rows, nb * 256:(nb + 1) * 256].rearrange("(t p) c -> p t c", p=128),
                          [], [txs[nb % 2], tX0])
                xload(0)
                for nb in range(8):
                    so, tso = wget(wi); wi += 1
                    if nb + 1 < 8:
                        xload(nb + 1)
                    for tt in range(4):
                        p, tp = nextps()
                        b.mm([(p[:, 0:256], [(hT[:, kc, tt * 128:(tt + 1) * 128], so[:, kc, :]) for kc in range(KC)])],
                             [tso, tHT], [tp])
                        b.tt(os_[nb % 2][:, tt, :], p[:, 0:256], xs[nb % 2][:, tt, :], ALU.add,
                             [tp, txs[nb % 2]], [tos[nb % 2], tX1])
                    b.dma("sp", xmid[rows, nb * 256:(nb + 1) * 256].rearrange("(t p) c -> p t c", p=128), os_[nb % 2],
                          [tos[nb % 2]], [tXm[grp * 4 + t_] for t_ in range(4)], merge=True)
                return wi

            def stage_route(grp):
                b.dma("sp", gbc[:], g2_bc, (), [tGbc])
                h2b = sgA[:].rearrange("p a n -> p (a n)").rearrange("p (t d) -> p t d", t=4)
                pr, tpr = ps[5], tP[5]
                for tt in range(4):
                    ti = grp * 4 + tt
                    r0 = ti * 128
                    b.dma("sp", xt0[:], xmid[r0:r0 + 128, :], [tXm[ti]], [tX0])
                    rms_tile(xt0[:], tX0, gbc[:], tGbc, xt1[:], tX1, hb[:], tHb)
                    b.copy("pool", h2b[:, tt, :], xt1[:], [tX1], [tSgA])
                    for rd in range(4):
                        p, tp = ps[2 + rd % 2], tP[2 + rd % 2]
                        h2T, th2T = (ar(0), tA[0]) if rd % 2 == 0 else (ar(1), tA[1])
                        b.tr([(p[:, j * 128:(j + 1) * 128], xt1[:, (rd * 4 + j) * 128:(rd * 4 + j + 1) * 128], ident_f[:])
                              for j in range(4)], [tX1, tC], [tp])
                        b.evac(h2T, p[:], [tp], [th2T])
                        h2v = h2T.rearrange("p (j t) -> p j t", j=4)

                        def fn(pe, h2v=h2v, rd=rd, pr=pr, tt=tt):
                            ins = None
                            for j in range(4):
                                kc = rd * 4 + j
                                ins = pe.matmul(pr[:, tt * 36:(tt + 1) * 36], h2v[:, j, :], wr_sb[:, kc, :],
                                                start=(kc == 0), stop=(kc == 15))
                            return ins
                        S.op("pe", fn, [th2T, tK], [tpr])
                A2, A4 = ar(2), ar(4)
                t2_, t3_, t4_ = tA[2], tA[3], tA[4]
                LG = A4[:, 0:144].rearrange("p (t c) -> p t c", t=4)
                b.tt(LG, pr[:, 0:144].rearrange("p (t c) -> p t c", t=4), b_bc[:].unsqueeze(1).broadcast_to([128, 4, 36]),
                     ALU.add, [tpr, tK], [t4_])
                gl = LG[:, :, 0:4]
                el = LG[:, :, 4:36].rearrange("p t (g e) -> p t g e", g=4)
                tmp = A4[:, 144:272].rearrange("p (t g e) -> p t g e", t=4, g=4)
                esel = A4[:, 272:304].rearrange("p (t e) -> p t e", t=4)
                top8 = A4[:, 304:336].rearrange("p (t e) -> p t e", t=4)
                oh1 = A4[:, 336:368].rearrange("p (t e) -> p t e", t=4)
                oh2 = A4[:, 368:400].rearrange("p (t e) -> p t e", t=4)
                goh = A4[:, 400:416].rearrange("p (t g) -> p t g", t=4)
                gd = A4[:, 416:432].rearrange("p (t g) -> p t g", t=4)
                mi = A4[:, 432:512]
                gmax, gsum, gw, dm, dd, w1, w2 = (mi[:, 4 * i:4 * i + 4] for i in range(7))

                def bc3(ap, n):
                    return ap.unsqueeze(2).broadcast_to([128, 4, n])
                b.reduce(gmax, gl, ALU.max, [t4_], [t4_])
                b.tt(goh, gl, bc3(gmax, 4), ALU.is_equal, [t4_], [t4_])
                b.tt(gd, gl, bc3(gmax, 4), ALU.subtract, [t4_], [t4_])
                b.act(gd, gd, AF.Exp, [t4_], [t4_])
                b.reduce(gsum, gd, ALU.add, [t4_], [t4_])
                b.recip(gw, gsum, [t4_], [t4_])
                b.tt(tmp, el, goh.unsqueeze(3).broadcast_to([128, 4, 4, 8]), ALU.mult, [t4_], [t4_])
                b.reduce(esel, tmp.rearrange("p t g e -> p t e g"), ALU.add, [t4_], [t4_])
                for tt in range(4):
                    def fmax(e, o=top8[:, tt, :], i_=esel[:, tt, :]):
                        return e.max(out=o, in_=i_)
                    S.op("dve", fmax, [t4_], [t4_])
                b.tt(oh1, esel, bc3(top8[:, :, 0], 8), ALU.is_equal, [t4_], [t4_])
                b.tt(oh2, esel, bc3(top8[:, :, 1], 8), ALU.is_equal, [t4_], [t4_])
                b.tt(dm, top8[:, :, 1], top8[:, :, 0], ALU.subtract, [t4_], [t4_])
                b.act(dd, dm, AF.Exp, [t4_], [t4_])
                b.ts(dm, dd, 1.0, None, ALU.add, None, [t4_], [t4_])
                b.recip(dm, dm, [t4_], [t4_])
                b.tt(w1, dm, gw, ALU.mult, [t4_], [t4_])
                b.tt(w2, w1, dd, ALU.mult, [t4_], [t4_])
                M1 = A2[:, 0:128].rearrange("p (t g e) -> p t g e", t=4, g=4)
                M2 = A2[:, 128:256].rearrange("p (t g e) -> p t g e", t=4, g=4)
                gohb = goh.unsqueeze(3).broadcast_to([128, 4, 4, 8])
                b.tt(M1, gohb, oh1.unsqueeze(2).broadcast_to([128, 4, 4, 8]), ALU.mult, [t4_], [t2_])
                b.tt(M2, gohb, oh2.unsqueeze(2).broadcast_to([128, 4, 4, 8]), ALU.mult, [t4_], [t2_])
                Mb = ar(3).bitcast(BF16)[:, 0:128].rearrange("p (t e) -> p t e", t=4)
                b.tt(Mb, A2[:, 0:128].rearrange("p (t e) -> p t e", t=4), A2[:, 128:256].rearrange("p (t e) -> p t e", t=4),
                     ALU.add, [t2_], [t3_])
                pp, tpp = ps[4], tP[4]
                groups = []
                for tt in range(4):
                    ops = [(lstrict[:], Mb[:, tt, :]), (ones_bf[:], macc[:])]
                    for t_ in range(tt):
                        ops.append((ones_bf[:], Mb[:, t_, :]))
                    groups.append((pp[:, tt * 32:(tt + 1) * 32], ops))
                b.mm(groups, [t3_, tC, tC2, tMacc], [tpp])
                for tt in range(4):
                    b.tt(macc[:], macc[:], Mb[:, tt, :], ALU.add, [t3_, tMacc], [tMacc], eng="pool")
                pos = A2[:, 256:384].rearrange("p (t e) -> p t e", t=4)
                tm = A2[:, 384:512].rearrange("p (t e) -> p t e", t=4)
                b.copy("dve", pos, pp[:, 0:128].rearrange("p (t e) -> p t e", t=4), [tpp], [t2_])
                s2 = small
                for kk in range(2):
                    Mk = A2[:, kk * 128:(kk + 1) * 128].rearrange("p (t e) -> p t e", t=4)
                    pk, ek, okf, dk = (s2[:, 16 + 16 * kk + 4 * i:20 + 16 * kk + 4 * i] for i in range(4))
                    b.tt(tm, pos, Mk, ALU.mult, [t2_], [t2_])
                    b.reduce(pk, tm, ALU.add, [t2_], [tSm])
                    b.tt(tm, ebase[:].unsqueeze(1).broadcast_to([128, 4, 32]), Mk, ALU.mult, [t2_, tC], [t2_])
                    b.reduce(ek, tm, ALU.add, [t2_], [tSm])
                    b.ts(okf, pk, float(CAP), None, ALU.is_lt, None, [tSm], [tSm])
                    b.ts(dk, okf, -1.0e6, 1.0e6, ALU.mult, ALU.add, [tSm], [tSm])
                    b.tt(dk, dk, pk, ALU.add, [tSm], [tSm])
                    b.tt(dk, dk, ek, ALU.add, [tSm], [tSm])
                    tD = [tDest[grp * 4 + t_] for t_ in range(4)]
                    b.copy("dve", dest_i[:, grp * 4:(grp + 1) * 4, kk], dk, [tSm], tD)
                    b.tt(wts[:, grp * 4:(grp + 1) * 4, kk], (w1 if kk == 0 else w2), okf, ALU.mult, [tSm, t4_], tD)
                for tt in range(4):
                    ti = grp * 4 + tt
                    for kk in range(2):
                        def fsc(g_, ti=ti, kk=kk, tt=tt):
                            return g_.indirect_dma_start(
                                out=Xg, out_offset=bass.IndirectOffsetOnAxis(ap=dest_i[:, ti, kk:kk + 1], axis=0),
                                in_=h2b[:, tt, :], in_offset=None, bounds_check=S.breg, oob_is_err=False)
                        S.dma("pool", fsc, [tSgA, tDest[ti]], [tXg], merge=True)

            plan = []
            for pg in range(NG):
                base = len(wlist)
                if pg == NG - 1:
                    wq_win(O_KA); wq_win(O_VA)
                for blk in range(4):
                    wq_win(O_FH + blk * 256)
                for blk in range(4):
                    wq_win(O_IH + blk * 256)
                plan.append(base)
            for og in range(NG):
                base = len(wlist)
                for blk in range(4):
                    wq_win(O_QA + blk * 256)
                wq_win(O_KA); wq_win(O_VA)
                for sec in (O_QH, O_FH, O_IH, O_GH):
                    for blk in range(4):
                        wq_win(sec + blk * 256)
                for sec in (O_GA, O_GB):
                    for blk in range(8):
                        wq_win(sec + blk * 256)
                for nb in range(4):
                    wlist.append((w_a_v[:, :, nb * 512:(nb + 1) * 512], 8, 512))
                    wlist.append((w_r_v[:, :, nb * 512:(nb + 1) * 512], 8, 512))
                for nb in range(8):
                    wlist.append((w_o_v[:, :, nb * 256:(nb + 1) * 256], 16, 256))
                plan.append(base)

            S.mark("init")
            for pg in range(NG - DBG_NGP, NG):
                stage_norm(x_pre, pg)
                S.mark("pnorm%d" % pg)
                stage_proj(False, pg == NG - 1, plan[pg])
                S.mark("pproj%d" % pg)
                stage_hloop(False)
                S.mark("phloop%d" % pg)
            b.copy("pool", kbuf[:, :, 0:128], kbuf[:, :, G:G + 128], [tKb], [tKb])
            b.copy("pool", vbuf[:, 0], vbuf[:, 4], [tVb], [tVb])
            for og in range(DBG_NGO):
                stage_norm(x_own, og)
                S.mark("norm%d" % og)
                wi = stage_proj(True, False, plan[NG + og])
                S.mark("proj%d" % og)
                stage_attn(og == 0)
                S.mark("attn%d" % og)
                stage_hloop(True)
                S.mark("hloop%d" % og)
                wi = stage_branch(wi)
                S.mark("branch%d" % og)
                wi = stage_out(wi, og)
                S.mark("out%d" % og)
                stage_route(og)
                S.mark("route%d" % og)
            S.mark("phase1")
            S.barrier()
            S.emit(final=(dbg == 2))
            if dbg == 2:
                return nc

        with ExitStack() as e2:
            wg = [sb(e2, "wg%d" % i, [128, KC, 512], BF16) for i in range(2)]
            wu = [sb(e2, "wu%d" % i, [128, KC, 512], BF16) for i in range(2)]
            wd = [sb(e2, "wd%d" % i, [128, 4, D], BF16) for i in range(2)]
            tWg = [T("wg%d" % i) for i in range(2)]
            tWu = [T("wu%d" % i) for i in range(2)]
            tWd = [T("wd%d" % i) for i in range(2)]
            xg_t = [sb(e2, "xg%d" % i, [128, D], BF16) for i in range(2)]
            tXgt = [T("xgt%d" % i) for i in range(2)]
            XgT = sb(e2, "XgT", [128, KC, CAP], BF16)
            tXgT = T("XgT")
            hTe = sb(e2, "hTe", [128, 4, CAP], BF16)
            tHe = T("hTe")
            sgt = [sb(e2, "sgt%d" % i, [128, CAP], F32) for i in range(2)]
            tSg = [T("sgt%d" % i) for i in range(2)]
            ysb = [sb(e2, "ysb%d" % i, [128, D], F32) for i in range(2)]
            tYs = [T("ysb%d" % i) for i in range(2)]

            def wload(e):
                i = e % 2
                b.dma("pool", wg[i][:], w_gate[e].rearrange("(kc p) n -> p kc n", p=128), (), [tWg[i]])
                b.dma("pool", wu[i][:], w_up[e].rearrange("(kc p) n -> p kc n", p=128), (), [tWu[i]])
                b.dma("pool", wd[i][:], w_down[e].rearrange("(fc p) n -> p fc n", p=128), (), [tWd[i]])

            wload(0)
            for e in range(DBG_NEX):
                i = e % 2
                if e + 1 < DBG_NEX:
                    wload(e + 1)
                for st in range(2):
                    r0 = e * CAP + st * 128
                    b.dma("sp", xg_t[st][:], Xg[r0:r0 + 128, :], [tXg], [tXgt[st]])
                    for rd in range(2):
                        p, tp = pb[rd], tPb[rd]
                        b.tr([(p[:, j, :], xg_t[st][:, (rd * 8 + j) * 128:(rd * 8 + j + 1) * 128], ident_bf[:])
                              for j in range(8)], [tXgt[st], tC], [tp])
                        b.evac(XgT[:, rd * 8:rd * 8 + 8, st * 128:(st + 1) * 128], p[:], [tp], [tXgT])
                for fc in range(4):
                    p, tp = ps[fc % 2], tP[fc % 2]
                    b.mm([(p[:, 0:CAP], [(wg[i][:, kc, fc * 128:(fc + 1) * 128], XgT[:, kc, :]) for kc in range(KC)]),
                          (p[:, CAP:2 * CAP], [(wu[i][:, kc, fc * 128:(fc + 1) * 128], XgT[:, kc, :]) for kc in range(KC)])],
                         [tWg[i], tWu[i], tXgT], [tp])
                    b.act(sgt[fc % 2][:], p[:, 0:CAP], AF.Silu, [tp], [tSg[fc % 2]])
                    b.tt(hTe[:, fc, :], sgt[fc % 2][:], p[:, CAP:2 * CAP], ALU.mult, [tSg[fc % 2], tp], [tHe])
                for st in range(2):
                    for nb in range(4):
                        p, tp = ps[2 + nb % 2], tP[2 + nb % 2]
                        b.mm([(p[:], [(hTe[:, fc, st * 128:(st + 1) * 128], wd[i][:, fc, nb * 512:(nb + 1) * 512])
                                      for fc in range(4)])], [tHe, tWd[i]], [tp])
                        b.evac(ysb[st][:, nb * 512:(nb + 1) * 512], p[:], [tp], [tYs[st]])
                    r0 = e * CAP + st * 128
                    b.dma("sp", Yg[r0:r0 + 128, :], ysb[st][:], [tYs[st]], [tYg], merge=True)
            S.barrier()
            S.emit()

        with ExitStack() as e3:
            gf = sb(e3, "gf", [128, D], F32)
            tGf = T("gf")
            b.dma("sp", gf[:], gf_bc, (), [tGf])
            xm_t = [sb(e3, "xm_t%d" % i, [128, D], F32) for i in range(3)]
            y1_t = [sb(e3, "y1_t%d" % i, [128, D], F32) for i in range(3)]
            y2_t = [sb(e3, "y2_t%d" % i, [128, D], F32) for i in range(3)]
            o_t = [sb(e3, "o_t%d" % i, [128, D], F32) for i in range(3)]
            jk = sb(e3, "jk", [128, D], BF16)
            sm3 = sb(e3, "sm3", [128, 8], F32)
            tXt = [T("xm_t%d" % i) for i in range(3)]
            tY1 = [T("y1_%d" % i) for i in range(3)]
            tY2 = [T("y2_%d" % i) for i in range(3)]
            tO = [T("o_%d" % i) for i in range(3)]
            tJ = T("jk")
            tS3 = T("sm3")
            for i in range(3):
                b.memset("pool", y1_t[i][:], 0.0, [tY1[i]])
                b.memset("pool", y2_t[i][:], 0.0, [tY2[i]])
            for ti in range(DBG_NT3):
                i = ti % 3
                r0 = ti * 128
                b.dma("sp", xm_t[i][:], xmid[r0:r0 + 128, :], [tXm[ti]], [tXt[i]])
                for kk, (yt, ty) in enumerate(((y1_t[i], tY1[i]), (y2_t[i], tY2[i]))):
                    def fga(g_, yt=yt, ti=ti, kk=kk):
                        return g_.indirect_dma_start(
                            out=yt[:, :], out_offset=None, in_=Yg,
                            in_offset=bass.IndirectOffsetOnAxis(ap=dest_i[:, ti, kk:kk + 1], axis=0),
                            bounds_check=S.breg, oob_is_err=False)
                    S.dma("pool", fga, [tYg, tDest[ti]], [ty])
                b.stt(xm_t[i][:], y1_t[i][:], wts[:, ti, 0:1], xm_t[i][:], ALU.mult, ALU.add,
                      [tY1[i], tDest[ti], tXt[i]], [tXt[i]])
                b.stt(xm_t[i][:], y2_t[i][:], wts[:, ti, 1:2], xm_t[i][:], ALU.mult, ALU.add,
                      [tY2[i], tDest[ti], tXt[i]], [tXt[i]])
                b.act(jk[:], xm_t[i][:], AF.Square, [tXt[i]], [tJ, tS3], accum_out=sm3[:, 0:1])
                b.ts(sm3[:, 1:2], sm3[:, 0:1], 1.0 / D, EPS, ALU.mult, ALU.add, [tS3], [tS3])
                b.act(sm3[:, 1:2], sm3[:, 1:2], AF.Ln, [tS3], [tS3])
                b.act(sm3[:, 2:3], sm3[:, 1:2], AF.Exp, [tS3], [tS3], scale=-0.5)
                b.stt(o_t[i][:], xm_t[i][:], sm3[:, 2:3], gf[:], ALU.mult, ALU.mult, [tXt[i], tS3, tGf], [tO[i]])
                b.dma("act", out_d[r0:r0 + 128, :], o_t[i][:], [tO[i]], [])
            S.mark("phase3")
            S.barrier()
            S.emit(final=True)
            DBG_MARKS[:] = S.marks
    return nc


def _t5_bucket(n):
    import math
    max_exact = 16
    nf = np.maximum(n, 1).astype(np.float32)
    large = max_exact + (np.log(nf / max_exact) / math.log(128 / max_exact) * (32 - max_exact)).astype(np.int32)
    large = np.minimum(large, 31)
    return np.where(n < max_exact, n, large)


_NC_CACHE = {}


def _consts():
    ident = np.eye(128, dtype=np.float32)
    s_ = np.arange(128)[:, None] % 64
    t_ = np.arange(64)[None, :]
    triu = (s_ <= t_).astype(np.float32)
    lstrict = (np.arange(128)[:, None] < np.arange(128)[None, :]).astype(np.float32)
    scanmask = np.ones((128, 512), np.float32)
    scanmask[:, ::64] = 0.0
    ebase = np.broadcast_to((np.arange(32) * CAP).astype(np.float32)[None, :], (128, 32)).copy()
    return dict(c_ident=ident, c_triu=triu, c_lstrict=lstrict, c_scanmask=scanmask, c_ebase=ebase)


def make_in_maps(inputs, ncores=8):
    f = lambda a: np.ascontiguousarray(np.asarray(a, dtype=np.float32))
    x = f(inputs["x"])
    bc = lambda v: np.ascontiguousarray(np.broadcast_to(f(v).reshape(1, -1), (128, f(v).size)))
    rel = f(inputs["rel_bias"])
    k_ = np.arange(128)[:, None]
    j_ = np.arange(256)[None, :]
    dist = j_ - k_
    bucket = _t5_bucket(np.clip(dist, 0, None))
    valid = (dist >= 0) & (dist < 128)
    tbl = rel[bucket]
    tbl = np.where(valid[:, :, None], tbl, np.float32(NEG)).astype(np.float32)
    biasT = np.ascontiguousarray(tbl.transpose(0, 2, 1))
    lbl = np.ascontiguousarray(f(inputs["hg_lb_logits"]).reshape(2, 8, 128).transpose(2, 1, 0))
    wr = np.ascontiguousarray(np.concatenate([f(inputs["w_group_router"][0]), f(inputs["w_expert_router"][0])], axis=1))
    bb = np.concatenate([f(inputs["b_group_router"][0]), f(inputs["b_expert_router"][0])])
    shared = dict(
        w_in=f(inputs["w_in"][0]), w_a=f(inputs["w_attn_branch"][0]), w_r=f(inputs["w_hg_branch"][0]),
        w_o=f(inputs["w_out"][0]), w_gate=f(inputs["w_gate"][0]), w_up=f(inputs["w_up"][0]),
        w_down=f(inputs["w_down"][0]), g1_bc=bc(inputs["norm1_g"][0]), g2_bc=bc(inputs["norm2_g"][0]),
        gf_bc=bc(inputs["final_g"]), gn_bc=bc(np.tile(f(inputs["hg_norm_g"][0]), 8)), biasT=biasT,
        sinks_bc=bc(inputs["attn_sinks"][0]), lbl=lbl, wr=wr, b_bc=bc(bb), **_consts())
    maps = []
    for c in range(ncores):
        bi, hf = c // 2, c % 2
        m = dict(shared)
        m["x_own"] = np.ascontiguousarray(x[bi, hf * TOK:(hf + 1) * TOK])
        m["x_pre"] = np.ascontiguousarray(x[bi, 0:TOK]) if hf == 1 else np.zeros((TOK, D), np.float32)
        m["flag0"] = np.full((128, 1), float(hf), np.float32)
        maps.append(m)
    return maps


def kernel(**inputs):
    if "nc" not in _NC_CACHE:
        _NC_CACHE["nc"] = build_program()
    nc = _NC_CACHE["nc"]
    maps = make_in_maps(inputs, 8)
    res = run_bass_kernel_spmd(nc, maps, core_ids=list(range(8)))
    out = np.empty((4, 4096, D), np.float32)
    for c in range(8):
        out[c // 2, (c % 2) * TOK:(c % 2 + 1) * TOK] = res.results[c]["out"]
    return out
```

```python
import numpy as np
from contextlib import ExitStack
import concourse.bass as bass
import concourse.mybir as mybir
from concourse.bass_utils import run_bass_kernel_spmd

F32 = mybir.dt.float32
BF16 = mybir.dt.bfloat16
I32 = mybir.dt.int32
AF = mybir.ActivationFunctionType
ALU = mybir.AluOpType
AX = mybir.AxisListType

SAME_ENG_SYNC = True
D = 2048
KC = 16
TOK = 2048
G = 512
NG = TOK // G
INW = 9728
NE = 32
CAP = 256
EPS = 1e-6
NEG = -30000.0
WPD = 2
DBG_NGP = NG
DBG_NGO = NG
DBG_NEX = NE
DBG_NT3 = 16
DBG_LIMIT = 10 ** 9
DBG_MARKS = []
NRING = 3

O_QA, O_KA, O_VA, O_QH, O_FH, O_IH, O_GH, O_GA, O_GB = 0, 1024, 1280, 1536, 2560, 3584, 4608, 5632, 7680


class T:
    __slots__ = ("name", "w", "r")

    def __init__(self, name):
        self.name = name
        self.w = {}
        self.r = {}


class Sched:
    ENG = ("pe", "act", "dve", "pool", "sp")
    NDMA = 8

    def __init__(self, nc, es):
        self.nc = nc
        self.nrec = 0
        self.marks = []
        self.prog = {k: [] for k in self.ENG}
        self.sems = {}
        self.cnt = {}
        self.seen = {k: {} for k in self.ENG}
        for k in self.ENG:
            self.sems[k] = es.enter_context(nc.semaphore("s_" + k))
            self.cnt[k] = 0
        self.breg = es.enter_context(nc.gpsimd.register("bnd"))
        self.dma_pool = {}
        self.dma_i = {}
        for q in ("sp", "act", "pool"):
            keys = []
            for i in range(self.NDMA):
                k = "d_%s%d" % (q, i)
                self.sems[k] = es.enter_context(nc.semaphore(k))
                self.cnt[k] = 0
                keys.append(k)
            self.dma_pool[q] = keys
            self.dma_i[q] = 0

    def _deps(self, e, reads, writes, merge):
        need = {}
        for t in reads:
            for k, v in t.w.items():
                need[k] = max(need.get(k, 0), v)
        for t in writes:
            if not merge:
                for k, v in t.w.items():
                    need[k] = max(need.get(k, 0), v)
            for k, v in t.r.items():
                need[k] = max(need.get(k, 0), v)
        waits = []
        for k, v in need.items():
            if k == e and (e == "pe" or not SAME_ENG_SYNC):
                continue
            if self.seen[e].get(k, 0) < v:
                self.seen[e][k] = v
                waits.append((k, v))
        return waits

    def mark(self, name):
        self.marks.append((name, self.nrec, dict(self.cnt)))

    def op(self, e, fn, reads=(), writes=()):
        if self.nrec >= DBG_LIMIT:
            return
        self.nrec += 1
        waits = self._deps(e, reads, writes, False)
        self.cnt[e] += 1
        v = self.cnt[e]
        self.prog[e].append((waits, fn, (e, 1)))
        for t in reads:
            t.r[e] = v
        for t in writes:
            t.w = {e: v}
            t.r = {}

    def dma(self, q, fn, reads=(), writes=(), merge=False):
        if self.nrec >= DBG_LIMIT:
            return
        self.nrec += 1
        i = self.dma_i[q]
        self.dma_i[q] += 1
        k = self.dma_pool[q][i % self.NDMA]
        waits = self._deps(q, reads, writes, merge)
        if self.cnt[k] > self.seen[q].get(k, 0):
            waits.append((k, self.cnt[k]))
            self.seen[q][k] = self.cnt[k]
        self.cnt[k] += 16
        v = self.cnt[k]
        self.prog[q].append((waits, fn, (k, 16)))
        for t in reads:
            t.r[k] = v
        for t in writes:
            if merge:
                t.w[k] = v
            else:
                t.w = {k: v}
                t.r = {}

    def barrier(self):
        for e in self.ENG:
            waits = []
            for k, v in self.cnt.items():
                if k != e and v > self.seen[e].get(k, 0):
                    waits.append((k, v))
                    self.seen[e][k] = v
            self.prog[e].append((waits, None, None))

    def emit(self, final=False):
        sems = self.sems
        prog = self.prog
        fin = [(k, v) for k, v in self.cnt.items() if v > 0 and k != "sp"] if final else []

        def mk(e):
            plist = prog[e]

            def body(engobj):
                if e == "pool" and self.breg is not None:
                    engobj.reg_mov(self.breg, NE * CAP - 1)
                for waits, fn, inc in plist:
                    for (wk, wv) in waits:
                        engobj.wait_ge(sems[wk], wv)
                    if fn is not None:
                        ins = fn(engobj)
                        ins.then_inc(sems[inc[0]], inc[1])
                if e == "sp":
                    for (wk, wv) in fin:
                        engobj.wait_ge(sems[wk], wv)
            return body

        with self.nc.Block() as block:
            block.tensor(mk("pe"))
            block.scalar(mk("act"))
            block.vector(mk("dve"))
            block.gpsimd(mk("pool"))
            block.sync(mk("sp"))
        self.prog = {k: [] for k in self.ENG}


class B:
    def __init__(self, S):
        self.S = S
        self.flip = 0

    def mm(self, groups, reads, writes):
        groups = [(o, list(ops)) for o, ops in groups]

        def fn(pe):
            ins = None
            for o, ops in groups:
                n = len(ops)
                for i, (l, r) in enumerate(ops):
                    ins = pe.matmul(o, l, r, start=(i == 0), stop=(i == n - 1))
            return ins
        self.S.op("pe", fn, reads, writes)

    def tr(self, items, reads, writes):
        items = list(items)

        def fn(pe):
            ins = None
            for o, i_, idt in items:
                ins = pe.transpose(o, i_, idt)
            return ins
        self.S.op("pe", fn, reads, writes)

    def act(self, out, in_, func, reads, writes, **kw):
        def fn(e):
            return e.activation(out=out, in_=in_, func=func, **kw)
        self.S.op("act", fn, reads, writes)

    def tt(self, out, in0, in1, op, reads, writes, eng="dve"):
        def fn(e):
            return e.tensor_tensor(out=out, in0=in0, in1=in1, op=op)
        self.S.op(eng, fn, reads, writes)

    def ts(self, out, in0, s1, s2, op0, op1, reads, writes, eng="dve"):
        def fn(e):
            if s2 is None:
                return e.tensor_scalar(out=out, in0=in0, scalar1=s1, scalar2=None, op0=op0)
            return e.tensor_scalar(out=out, in0=in0, scalar1=s1, scalar2=s2, op0=op0, op1=op1)
        self.S.op(eng, fn, reads, writes)

    def stt(self, out, in0, scalar, in1, op0, op1, reads, writes):
        def fn(e):
            return e.scalar_tensor_tensor(out=out, in0=in0, scalar=scalar, in1=in1, op0=op0, op1=op1)
        self.S.op("dve", fn, reads, writes)

    def copy(self, eng, out, in_, reads, writes):
        if eng == "act":
            def fn(e):
                return e.activation(out=out, in_=in_, func=AF.Copy)
        else:
            def fn(e):
                return e.tensor_copy(out=out, in_=in_)
        self.S.op(eng, fn, reads, writes)

    def evac(self, out, in_, reads, writes):
        self.flip ^= 1
        self.copy("act" if self.flip else "dve", out, in_, reads, writes)

    def reduce(self, out, in_, op, reads, writes):
        def fn(e):
            return e.tensor_reduce(out=out, in_=in_, axis=AX.X, op=op)
        self.S.op("dve", fn, reads, writes)

    def recip(self, out, in_, reads, writes):
        def fn(e):
            return e.reciprocal(out=out, in_=in_)
        self.S.op("dve", fn, reads, writes)

    def memset(self, eng, ap, val, writes):
        def fn(e):
            return e.memset(ap, val)
        self.S.op(eng, fn, (), writes)

    def dma(self, q, out, in_, reads, writes, merge=False):
        def fn(e):
            return e.dma_start(out=out, in_=in_)
        self.S.dma(q, fn, reads, writes, merge)

    def scan(self, out, d0, d1, reads, writes):
        def fn(e):
            return e.tensor_tensor_scan(out=out, data0=d0, data1=d1, initial=0.0, op0=ALU.mult, op1=ALU.add)
        self.S.op("dve", fn, reads, writes)


def build_program(dbg=False):
    nc = bass.Bass("TRN2", target_bir_lowering=False)

    def din(name, shape, dt=F32):
        return nc.dram_tensor(name, shape, dt, kind="ExternalInput").ap()

    x_own = din("x_own", [TOK, D])
    x_pre = din("x_pre", [TOK, D])
    w_in = din("w_in", [D, INW])
    w_a = din("w_a", [1024, D])
    w_r = din("w_r", [1024, D])
    w_o = din("w_o", [D, D])
    w_gate = din("w_gate", [NE, D, 512])
    w_up = din("w_up", [NE, D, 512])
    w_down = din("w_down", [NE, 512, D])
    g1_bc = din("g1_bc", [128, D])
    g2_bc = din("g2_bc", [128, D])
    gf_bc = din("gf_bc", [128, D])
    gn_in = din("gn_bc", [128, 1024])
    bias_in = din("biasT", [128, 16, 256])
    flag_in = din("flag0", [128, 1])
    sinks_in = din("sinks_bc", [128, 16])
    lbl_in = din("lbl", [128, 8, 2])
    wr_in = din("wr", [D, 36])
    bb_in = din("b_bc", [128, 36])
    c_ident = din("c_ident", [128, 128])
    c_triu = din("c_triu", [128, 64])
    c_lstrict = din("c_lstrict", [128, 128])
    c_scanmask = din("c_scanmask", [128, 512])
    c_ebase = din("c_ebase", [128, 32])
    out_d = nc.dram_tensor("out", [TOK, D], F32, kind="ExternalOutput").ap()
    if dbg:
        xmid = nc.dram_tensor("xmid", [TOK, D], F32, kind="ExternalOutput").ap()
    else:
        xmid = nc.dram_tensor("xmid", [TOK, D], F32).ap()
    Xg = nc.dram_tensor("Xg", [NE * CAP, D], BF16).ap()
    Yg = nc.dram_tensor("Yg", [NE * CAP, D], F32).ap()
    tXm = [T("xm%d" % i) for i in range(16)]
    tXg = T("Xg")
    tYg = T("Yg")

    w_in_v = w_in.rearrange("(kc p) n -> p kc n", p=128)
    w_a_v = w_a.rearrange("(kc p) n -> p kc n", p=128)
    w_r_v = w_r.rearrange("(kc p) n -> p kc n", p=128)
    w_o_v = w_o.rearrange("(kc p) n -> p kc n", p=128)

    with ExitStack() as ea:
        S = Sched(nc, ea)
        b = B(S)

        def sb(es, name, shape, dt):
            return es.enter_context(nc.sbuf_tensor("sb_" + name, shape, dt))

        ident_bf = sb(ea, "ident_bf", [128, 128], BF16)
        ident_f = sb(ea, "ident_f", [128, 128], F32)
        triu = sb(ea, "triu", [128, 64], BF16)
        lstrict = sb(ea, "lstrict", [128, 128], BF16)
        ones_bf = sb(ea, "ones_bf", [128, 128], BF16)
        ebase = sb(ea, "ebase", [128, 32], F32)
        neghalf = sb(ea, "neghalf", [128, 1], F32)
        dest_i = sb(ea, "dest_i", [128, 16, 2], I32)
        wts = sb(ea, "wts", [128, 16, 2], F32)
        tC = T("consts")
        tDest = [T("dest%d" % i) for i in range(16)]
        ps = []
        tP = []
        for i in range(6):
            ps.append(ea.enter_context(nc.psum_tensor("ps%d" % i, [128, 512], F32)))
            tP.append(T("ps%d" % i))
        pb = []
        tPb = []
        for i in range(2):
            pb.append(ea.enter_context(nc.psum_tensor("pb%d" % i, [128, 8, 128], BF16)))
            tPb.append(T("pb%d" % i))

        b.dma("pool", ident_bf[:], c_ident, (), [tC])
        b.dma("sp", ident_f[:], c_ident, (), [tC], merge=True)
        b.dma("pool", triu[:], c_triu, (), [tC], merge=True)
        b.dma("pool", lstrict[:], c_lstrict, (), [tC], merge=True)
        b.dma("sp", ebase[:], c_ebase, (), [tC], merge=True)
        tC2 = T("consts2")
        b.memset("pool", ones_bf[:], 1.0, [tC2])
        b.memset("pool", neghalf[:], -0.5, [tC2])

        with ExitStack() as e1:
            scanmask = sb(e1, "scanmask", [128, 512], F32)
            biasT = sb(e1, "biasT", [128, 16, 256], F32)
            gn_bc = sb(e1, "gn_bc", [128, 1024], F32)
            wr_sb = sb(e1, "wr_sb", [128, 16, 36], F32)
            b_bc = sb(e1, "b_bc", [128, 36], F32)
            esink = sb(e1, "esink", [128, 16], F32)
            flag0 = sb(e1, "flag0", [128, 1], F32)
            lbl = sb(e1, "lbl", [128, 8, 2], F32)
            lb = sb(e1, "lb", [128, 8], F32)
            oml = sb(e1, "oml", [128, 8], F32)
            ln_oml = sb(e1, "ln_oml", [128, 8], F32)
            macc = sb(e1, "macc", [128, 32], BF16)
            tK = T("consts_p1")
            b.dma("sp", scanmask[:], c_scanmask, (), [tK])
            b.dma("sp", biasT[:], bias_in, (), [tK], merge=True)
            b.dma("sp", gn_bc[:], gn_in, (), [tK], merge=True)
            b.dma("sp", wr_sb[:], wr_in.rearrange("(kc p) n -> p kc n", p=128), (), [tK], merge=True)
            b.dma("sp", b_bc[:], bb_in, (), [tK], merge=True)
            b.dma("sp", esink[:], sinks_in, (), [tK], merge=True)
            b.dma("sp", flag0[:], flag_in, (), [tK], merge=True)
            b.dma("sp", lbl[:], lbl_in, (), [tK], merge=True)
            tL = T("lb")
            b.tt(lb[:], lbl[:, :, 1], lbl[:, :, 0], ALU.subtract, [tK], [tL])
            b.act(lb[:], lb[:], AF.Exp, [tL], [tL])
            b.ts(lb[:], lb[:], 1.0, None, ALU.add, None, [tL], [tL])
            b.recip(lb[:], lb[:], [tL], [tL])
            b.ts(oml[:], lb[:], -1.0, 1.0, ALU.mult, ALU.add, [tL], [tL])
            b.act(ln_oml[:], oml[:], AF.Ln, [tL], [tL])
            tEs = T("esink")
            b.act(esink[:], esink[:], AF.Exp, [tK], [tEs])
            tMacc = T("macc")
            b.memset("pool", macc[:], 0.0, [tMacc])

            NR = NRING
            wring = [sb(e1, "wring%d" % i, [128, 4096], BF16) for i in range(NR)]
            tW = [T("wring%d" % i) for i in range(NR)]
            hT = sb(e1, "hT", [128, KC, G], BF16)
            tHT = T("hT")
            qaT = sb(e1, "qaT", [128, 8, G], BF16)
            tQA = [T("qa%d" % i) for i in range(4)]
            kbuf = sb(e1, "kbuf", [128, 4, 128 + G], BF16)
            tKb = T("kbuf")
            vbuf = sb(e1, "vbuf", [128, 5, 4, 65], BF16)
            tVb = T("vbuf")
            QtT = sb(e1, "QtT", [128, 8, G], BF16)
            tQt = [T("Qt%d" % i) for i in range(4)]
            KtT = sb(e1, "KtT", [128, 8, G], BF16)
            tKt = T("KtT")
            vh = sb(e1, "vh", [128, 4, 1024], BF16)
            tVh = T("vh")
            gsg = sb(e1, "gsg", [128, 4, 1024], BF16)
            tGs = T("gsg")
            sgA = sb(e1, "sgA", [128, 16, G], BF16)
            sgH = sb(e1, "sgH", [128, 16, G], BF16)
            tSgA = T("sgA")
            tSgH = T("sgH")
            S32 = sb(e1, "S32", [128, 8, 128], F32)
            Sbf = sb(e1, "Sbf", [128, 8, 128], BF16)
            tS32 = T("S32")
            tSbf = T("Sbf")
            dec = sb(e1, "dec", [128, 8, 8], F32)
            tDec = T("dec")
            xt0 = sb(e1, "xt0", [128, D], F32)
            xt1 = sb(e1, "xt1", [128, D], F32)
            gbc = sb(e1, "gbc", [128, D], F32)
            hb = sb(e1, "hb", [128, D], BF16)
            hb2 = sb(e1, "hb2", [128, D], BF16)
            tHb2 = T("hb2")
            tX0, tX1, tGbc, tHb = T("xt0"), T("xt1"), T("gbc"), T("hb")
            arena = sb(e1, "arena", [128, 6 * 512], F32)
            tA = [T("ar%d" % i) for i in range(6)]
            small = sb(e1, "small", [128, 64], F32)
            tSm = T("small")
            smallr = sb(e1, "smallr", [128, 160], F32)
            tSr = T("smallr")

            def ar(i, n=1):
                return arena[:, i * 512:(i + n) * 512]

            b.memset("pool", S32[:], 0.0, [tS32])
            b.memset("pool", Sbf[:], 0.0, [tSbf])
            b.memset("pool", vbuf[:], 1.0, [tVb])
            b.memset("pool", kbuf[:], 0.0, [tKb])

            b.memset("pool", hb[:], 0.0, [tHb])
            for i in range(NE * CAP // 128):
                b.dma("act", Xg[i * 128:(i + 1) * 128, :], hb[:], [tHb], [tXg], merge=True)

            wlist = []

            def wq_win(col0):
                wlist.append((w_in_v[:, :, col0:col0 + 256], 16, 256))
                return len(wlist) - 1
            wstate = {"issued": 0}

            def wissue(upto):
                while wstate["issued"] <= min(upto, len(wlist) - 1):
                    i = wstate["issued"]
                    src, kc, n = wlist[i]
                    slot = wring[i % NR][:, 0:kc * n].rearrange("p (k n) -> p k n", k=kc)
                    b.dma("pool", slot, src, (), [tW[i % NR]])
                    wstate["issued"] += 1

            def wget(i):
                wissue(i + WPD)
                src, kc, n = wlist[i]
                return wring[i % NR][:, 0:kc * n].rearrange("p (k n) -> p k n", k=kc), tW[i % NR]

            pstate = {"i": 0}

            def nextps():
                pstate["i"] ^= 1
                return ps[pstate["i"]], tP[pstate["i"]]

            def rms_tile(xt, tXt, gtile, tG, out, tOut, junk, tJunk):
                b.act(junk, xt, AF.Square, [tXt], [tJunk, tSm], accum_out=small[:, 0:1])
                b.ts(small[:, 1:2], small[:, 0:1], 1.0 / D, EPS, ALU.mult, ALU.add, [tSm], [tSm])
                b.act(small[:, 1:2], small[:, 1:2], AF.Ln, [tSm], [tSm])
                b.act(small[:, 2:3], small[:, 1:2], AF.Exp, [tSm], [tSm], scale=-0.5)
                b.stt(out, xt, small[:, 2:3], gtile, ALU.mult, ALU.mult, [tXt, tSm, tG, tJunk], [tOut])

            def stage_norm(xsrc, grp):
                b.dma("sp", gbc[:], g1_bc, (), [tGbc])
                for tt in range(4):
                    xt, tXt = (xt0, tX0) if tt % 2 == 0 else (xt1, tX1)
                    r0 = grp * G + tt * 128
                    b.dma("sp", xt[:], xsrc[r0:r0 + 128, :], (), [tXt])
                    hbx, thbx = (hb, tHb) if tt % 2 == 0 else (hb2, tHb2)
                    rms_tile(xt[:], tXt, gbc[:], tGbc, hbx[:], thbx, hbx[:], thbx)
                    for rd in range(2):
                        p, tp = pb[rd], tPb[rd]
                        b.tr([(p[:, j, :], hbx[:, (rd * 8 + j) * 128:(rd * 8 + j + 1) * 128], ident_bf[:])
                              for j in range(8)], [thbx, tC], [tp])
                        b.evac(hT[:, rd * 8:rd * 8 + 8, tt * 128:(tt + 1) * 128], p[:], [tp], [tHT])

            def proj_fm(slot, tSlot, c):
                p, tp = nextps()
                b.mm([(p[:], [(slot[:, kc, c * 128:(c + 1) * 128], hT[:, kc, :]) for kc in range(KC)])],
                     [tSlot, tHT], [tp])
                return p, tp

            def proj_tm(slot, tSlot, tt, n):
                p, tp = nextps()
                b.mm([(p[:, 0:n], [(hT[:, kc, tt * 128:(tt + 1) * 128], slot[:, kc, 0:n]) for kc in range(KC)])],
                     [tSlot, tHT], [tp])
                return p, tp

            def chain(h, p, tp, own):
                E, L1, L2, bb_ = ar(0), ar(1), ar(2), ar(3)
                b.act(E, p[:], AF.Exp, [tp], [tA[0]])
                b.act(L1, E, AF.Ln, [tA[0], tL], [tA[1]], bias=lb[:, h:h + 1])
                b.act(L2, E, AF.Ln, [tA[0]], [tA[2]], bias=1.0)
                b.tt(L1, L1, L2, ALU.subtract, [tA[1], tA[2]], [tA[1]])
                b.scan(bb_, scanmask[:], L1, [tK, tA[1]], [tA[3]])
                b.act(E, bb_, AF.Exp, [tA[3]], [tA[0]])
                if own:
                    b.tt(QtT[:, h, :], QtT[:, h, :], E, ALU.mult, [tA[0]] + tQt, tQt)
                b.tt(L2, L2, bb_, ALU.add, [tA[2], tA[3]], [tA[2]])
                b.act(KtT[:, h, :], L2, AF.Exp, [tA[2], tL], [tKt], scale=-1.0, bias=ln_oml[:, h:h + 1])
                b.copy("pool", dec[:, h, :], E.rearrange("p (c t) -> p c t", t=64)[:, :, 63], [tA[0]], [tDec])

            def stage_proj(own, last_prefix, wbase):
                wi = wbase
                if own:
                    for blk in range(4):
                        slot, tsl = wget(wi); wi += 1
                        for c in range(2):
                            p, tp = proj_fm(slot, tsl, c)
                            b.act(qaT[:, blk * 2 + c, :], p[:], AF.Copy, [tp], tQA, scale=0.125)
                if own or last_prefix:
                    slot, tsl = wget(wi); wi += 1
                    for g in range(4):
                        p, tp = nextps()
                        b.mm([(p[0:64, :], [(slot[:, kc, g * 64:(g + 1) * 64], hT[:, kc, :]) for kc in range(KC)]),
                              (p[64:128, :], [(slot[:, kc, g * 64:(g + 1) * 64], hT[:, kc, :]) for kc in range(KC)])],
                             [tsl, tHT], [tp])
                        b.evac(kbuf[:, g, 128:128 + G], p[:], [tp], [tKb])
                    slot, tsl = wget(wi); wi += 1
                    for tt in range(4):
                        p, tp = proj_tm(slot, tsl, tt, 256)
                        b.evac(vbuf[:, 1 + tt, :, 0:64], p[:, 0:256].rearrange("p (g d) -> p g d", g=4), [tp], [tVb])
                if own:
                    for blk in range(4):
                        slot, tsl = wget(wi); wi += 1
                        for c in range(2):
                            p, tp = proj_fm(slot, tsl, c)
                            b.evac(QtT[:, blk * 2 + c, :], p[:], [tp], tQt)
                for blk in range(4):
                    slot, tsl = wget(wi); wi += 1
                    for c in range(2):
                        p, tp = proj_fm(slot, tsl, c)
                        chain(blk * 2 + c, p, tp, own)
                for blk in range(4):
                    slot, tsl = wget(wi); wi += 1
                    for tt in range(4):
                        p, tp = proj_tm(slot, tsl, tt, 256)
                        b.evac(vh[:, tt, blk * 256:(blk + 1) * 256], p[:, 0:256], [tp], [tVh])
                if own:
                    for blk in range(4):
                        slot, tsl = wget(wi); wi += 1
                        for tt in range(4):
                            p, tp = proj_tm(slot, tsl, tt, 256)
                            tmp = ar(4)[:, 0:256]
                            b.act(tmp, p[:, 0:256], AF.Silu, [tp], [tA[4]])
                            b.tt(gsg[:, tt, blk * 256:(blk + 1) * 256], tmp, gn_bc[:, blk * 256:(blk + 1) * 256],
                                 ALU.mult, [tA[4], tK], [tGs])
                    for blk in range(8):
                        slot, tsl = wget(wi); wi += 1
                        for c in range(2):
                            p, tp = proj_fm(slot, tsl, c)
                            b.act(sgA[:, blk * 2 + c, :], p[:], AF.Sigmoid, [tp], [tSgA])
                    for blk in range(8):
                        slot, tsl = wget(wi); wi += 1
                        for c in range(2):
                            p, tp = proj_fm(slot, tsl, c)
                            b.act(sgH[:, blk * 2 + c, :], p[:], AF.Sigmoid, [tp], [tSgH])
                return wi

            def stage_attn(first_group):
                sE = ar(0, 2).rearrange("p (a n) -> p a n", a=2)
                pT = ar(2).bitcast(BF16).rearrange("p (a h q) -> p a h q", a=2, h=4)
                a_tok = ar(3).bitcast(BF16)
                for tt in range(4):
                    for g in range(4):
                        banks = [[(ps[2], tP[2]), (ps[3], tP[3])], [(ps[0], tP[0]), (ps[1], tP[1])]]
                        sE4 = sE.rearrange("p a (j two q) -> p a j two q", j=2, two=2)
                        for kb in range(2):
                            for par in range(2):
                                pbk, tpbk = banks[kb][par]
                                p0 = par * 64
                                groups = []
                                for jj in range(2):
                                    h = g * 4 + jj * 2 + par
                                    groups.append((pbk[:, jj * 128:(jj + 1) * 128],
                                                   [(kbuf[p0:p0 + 64, g, (tt + kb) * 128:(tt + kb + 1) * 128],
                                                     qaT[p0:p0 + 64, h // 2, tt * 128:(tt + 1) * 128])]))
                                b.mm(groups, [tKb, tQA[tt]], [tpbk])
                                bcol = slice(128, 256) if kb == 0 else slice(0, 128)
                                bsl = biasT[:, g * 4:(g + 1) * 4, bcol].rearrange("p (j two) q -> p j two q", two=2)[:, :, par, :]
                                b.stt(sE4[:, kb, :, par, :], pbk[:, 0:256].rearrange("p (j q) -> p j q", j=2), 60.0, bsl,
                                      ALU.min, ALU.add, [tpbk, tK], [tA[kb]])
                        b.act(pT, sE.rearrange("p a (h q) -> p a h q", h=4), AF.Exp, [tA[0], tA[1]], [tA[2]])
                        if first_group and tt == 0:
                            b.ts(pT[:, 0], pT[:, 0], flag0[:, 0:1], None, ALU.mult, None, [tA[2], tK], [tA[2]])
                        po, tpo = ps[4], tP[4]
                        groups = []
                        for hh in range(4):
                            groups.append((po[:, hh * 65:(hh + 1) * 65],
                                           [(pT[:, kb, hh, :], vbuf[:, tt + kb, g, :]) for kb in range(2)]))
                        b.mm(groups, [tA[2], tVb], [tpo])
                        pov = po[:, 0:260].rearrange("p (h d) -> p h d", h=4)
                        b.tt(smallr[:, 0:4], pov[:, :, 64], esink[:, g * 4:(g + 1) * 4], ALU.add, [tpo, tEs], [tSr])
                        b.recip(smallr[:, 4:8], smallr[:, 0:4], [tSr], [tSr])
                        b.tt(a_tok[:, g * 256:(g + 1) * 256].rearrange("p (h d) -> p h d", h=4), pov[:, :, 0:64],
                             smallr[:, 4:8].unsqueeze(2).broadcast_to([128, 4, 64]), ALU.mult, [tpo, tSr], [tA[3]])
                    p, tp = pb[tt % 2], tPb[tt % 2]
                    b.tr([(p[:, j, :], a_tok[:, j * 128:(j + 1) * 128], ident_bf[:]) for j in range(8)],
                         [tA[3], tC], [tp])
                    b.evac(qaT[:, :, tt * 128:(tt + 1) * 128], p[:], [tp], [tQA[tt]])
                b.copy("pool", kbuf[:, :, 0:128], kbuf[:, :, G:G + 128], [tKb], [tKb])
                b.copy("pool", vbuf[:, 0], vbuf[:, 4], [tVb], [tVb])

            def stage_hloop(own):
                attm = ar(0).bitcast(BF16)[:, 0:512].rearrange("p (h t) -> p h t", h=8)
                r_tok = ar(1).bitcast(BF16)
                junk = ar(2).bitcast(BF16)
                Kt = ar(3).bitcast(BF16).rearrange("p (h k) -> p h k", h=8)
                for tt in range(4):
                    p, tp = pb[tt % 2], tPb[tt % 2]
                    b.tr([(p[:, h, :], KtT[:, h, tt * 128:(tt + 1) * 128], ident_bf[:]) for h in range(8)],
                         [tKt, tC], [tp])
                    b.evac(Kt, p[:], [tp], [tA[3]])
                    for cc in range(2):
                        c = tt * 2 + cc
                        p0 = cc * 64
                        cs = slice(c * 64, (c + 1) * 64)
                        if own:
                            pa, tpa = ps[4], tP[4]
                            b.mm([(pa[p0:p0 + 64, h * 64:(h + 1) * 64], [(KtT[:, h, cs], QtT[:, h, cs])])
                                  for h in range(8)], [tKt, tQt[tt]], [tpa])
                            b.tt(attm[p0:p0 + 64], pa[p0:p0 + 64, :].rearrange("p (h t) -> p h t", h=8),
                                 triu[p0:p0 + 64, :].unsqueeze(1).broadcast_to([64, 8, 64]), ALU.mult,
                                 [tpa, tC], [tA[0]])
                            groups = []
                            for h in range(8):
                                po = ps[2] if h < 4 else ps[3]
                                o_ap = po[p0:p0 + 64, (h % 4) * 128:(h % 4 + 1) * 128]
                                groups.append((o_ap, [(QtT[:, h, cs], Sbf[:, h, :]),
                                                      (attm[p0:p0 + 64, h, :], vh[p0:p0 + 64, tt, h * 128:(h + 1) * 128])]))
                            b.mm(groups, [tQt[tt], tSbf, tA[0], tVh], [tP[2], tP[3]])
                        groups = []
                        for h in range(8):
                            po = ps[0] if h < 4 else ps[1]
                            groups.append((po[:, (h % 4) * 128:(h % 4 + 1) * 128],
                                           [(Kt[p0:p0 + 64, h, :], vh[p0:p0 + 64, tt, h * 128:(h + 1) * 128])]))
                        b.mm(groups, [tA[3], tVh], [tP[0], tP[1]])
                        b.tt(S32[:, 0:4, :], ps[0][:].rearrange("p (h v) -> p h v", h=4), S32[:, 0:4, :], ALU.add,
                             [tP[0], tS32], [tS32])
                        b.tt(S32[:, 4:8, :], ps[1][:].rearrange("p (h v) -> p h v", h=4), S32[:, 4:8, :], ALU.add,
                             [tP[1], tS32], [tS32])
                        b.tt(S32[:], S32[:], dec[:, :, c].unsqueeze(2).broadcast_to([128, 8, 128]), ALU.mult,
                             [tS32, tDec], [tS32])
                        b.copy("act", Sbf[:], S32[:], [tS32], [tSbf])
                    if own:
                        for h in range(8):
                            po = ps[2] if h < 4 else ps[3]
                            b.act(junk[:, 0:128], po[:, (h % 4) * 128:(h % 4 + 1) * 128], AF.Square,
                                  [tP[2], tP[3]], [tA[2], tSr], accum_out=smallr[:, 16 + h:17 + h])
                        b.ts(smallr[:, 32:40], smallr[:, 16:24], 1.0 / 128, EPS, ALU.mult, ALU.add, [tSr], [tSr])
                        b.act(smallr[:, 32:40], smallr[:, 32:40], AF.Ln, [tSr], [tSr])
                        b.act(smallr[:, 40:48], smallr[:, 32:40], AF.Exp, [tSr], [tSr], scale=-0.5)
                        for h in range(8):
                            po = ps[2] if h < 4 else ps[3]
                            b.stt(r_tok[:, h * 128:(h + 1) * 128], po[:, (h % 4) * 128:(h % 4 + 1) * 128],
                                  smallr[:, 40 + h:41 + h], gsg[:, tt, h * 128:(h + 1) * 128], ALU.mult, ALU.mult,
                                  [tP[2], tP[3], tSr, tGs], [tA[1]])
                        p, tp = pb[(tt + 1) % 2], tPb[(tt + 1) % 2]
                        b.tr([(p[:, j, :], r_tok[:, j * 128:(j + 1) * 128], ident_bf[:]) for j in range(8)],
                             [tA[1], tC], [tp])
                        b.evac(QtT[:, :, tt * 128:(tt + 1) * 128], p[:], [tp], [tQt[tt]])

            def stage_branch(wi):
                m2 = ar(1)
                for nb in range(4):
                    sa, tsa = wget(wi); wi += 1
                    for c in range(4):
                        j = nb * 4 + c
                        p, tp = ps[2 + c % 2], tP[2 + c % 2]
                        b.mm([(p[:], [(sa[:, kc, c * 128:(c + 1) * 128], qaT[:, kc, :]) for kc in range(8)])],
                             [tsa] + tQA, [tp])
                        b.tt(ar(2 + c), p[:], sgA[:, j, :], ALU.mult, [tp, tSgA], [tA[2 + c]])
                    sr, tsr = wget(wi); wi += 1
                    for c in range(4):
                        j = nb * 4 + c
                        p, tp = ps[2 + c % 2], tP[2 + c % 2]
                        b.mm([(p[:], [(sr[:, kc, c * 128:(c + 1) * 128], QtT[:, kc, :]) for kc in range(8)])],
                             [tsr] + tQt, [tp])
                        b.tt(m2, p[:], sgH[:, j, :], ALU.mult, [tp, tSgH], [tA[1]])
                        b.tt(hT[:, j, :], ar(2 + c), m2, ALU.add, [tA[2 + c], tA[1]], [tHT])
                return wi

            def stage_out(wi, grp):
                xs = [xt0[:, 0:1024].rearrange("p (t c) -> p t c", t=4), xt0[:, 1024:2048].rearrange("p (t c) -> p t c", t=4)]
                os_ = [xt1[:, 0:1024].rearrange("p (t c) -> p t c", t=4), xt1[:, 1024:2048].rearrange("p (t c) -> p t c", t=4)]
                txs = [tA[2], tA[3]]
                tos = [tA[4], tA[5]]
                rows = slice(grp * G, (grp + 1) * G)

                def xload(nb):
                    b.dma("sp", xs[nb % 2], x_own[rows, nb * 256:(nb + 1) * 256].rearrange("(t p) c -> p t c", p=128),
                          [], [txs[nb % 2], tX0])
                xload(0)
                for nb in range(8):
                    so, tso = wget(wi); wi += 1
                    if nb + 1 < 8:
                        xload(nb + 1)
                    for tt in range(4):
                        p, tp = nextps()
                        b.mm([(p[:, 0:256], [(hT[:, kc, tt * 128:(tt + 1) * 128], so[:, kc, :]) for kc in range(KC)])],
                             [tso, tHT], [tp])
                        b.tt(os_[nb % 2][:, tt, :], p[:, 0:256], xs[nb % 2][:, tt, :], ALU.add,
                             [tp, txs[nb % 2]], [tos[nb % 2], tX1])
                    b.dma("sp", xmid[rows, nb * 256:(nb + 1) * 256].rearrange("(t p) c -> p t c", p=128), os_[nb % 2],
                          [tos[nb % 2]], [tXm[grp * 4 + t_] for t_ in range(4)], merge=True)
                return wi

            def stage_route(grp):
                b.dma("sp", gbc[:], g2_bc, (), [tGbc])
                h2b = sgA[:].rearrange("p a n -> p (a n)").rearrange("p (t d) -> p t d", t=4)
                pr, tpr = ps[5], tP[5]
                for tt in range(4):
                    ti = grp * 4 + tt
                    r0 = ti * 128
                    b.dma("sp", xt0[:], xmid[r0:r0 + 128, :], [tXm[ti]], [tX0])
                    rms_tile(xt0[:], tX0, gbc[:], tGbc, xt1[:], tX1, hb[:], tHb)
                    b.copy("pool", h2b[:, tt, :], xt1[:], [tX1], [tSgA])
                    for rd in range(4):
                        p, tp = ps[2 + rd % 2], tP[2 + rd % 2]
                        h2T, th2T = (ar(0), tA[0]) if rd % 2 == 0 else (ar(1), tA[1])
                        b.tr([(p[:, j * 128:(j + 1) * 128], xt1[:, (rd * 4 + j) * 128:(rd * 4 + j + 1) * 128], ident_f[:])
                              for j in range(4)], [tX1, tC], [tp])
                        b.evac(h2T, p[:], [tp], [th2T])
                        h2v = h2T.rearrange("p (j t) -> p j t", j=4)

                        def fn(pe, h2v=h2v, rd=rd, pr=pr, tt=tt):
                            ins = None
                            for j in range(4):
                                kc = rd * 4 + j
                                ins = pe.matmul(pr[:, tt * 36:(tt + 1) * 36], h2v[:, j, :], wr_sb[:, kc, :],
                                                start=(kc == 0), stop=(kc == 15))
                            return ins
                        S.op("pe", fn, [th2T, tK], [tpr])
                A2, A4 = ar(2), ar(4)
                t2_, t3_, t4_ = tA[2], tA[3], tA[4]
                LG = A4[:, 0:144].rearrange("p (t c) -> p t c", t=4)
                b.tt(LG, pr[:, 0:144].rearrange("p (t c) -> p t c", t=4), b_bc[:].unsqueeze(1).broadcast_to([128, 4, 36]),
                     ALU.add, [tpr, tK], [t4_])
                gl = LG[:, :, 0:4]
                el = LG[:, :, 4:36].rearrange("p t (g e) -> p t g e", g=4)
                tmp = A4[:, 144:272].rearrange("p (t g e) -> p t g e", t=4, g=4)
                esel = A4[:, 272:304].rearrange("p (t e) -> p t e", t=4)
                top8 = A4[:, 304:336].rearrange("p (t e) -> p t e", t=4)
                oh1 = A4[:, 336:368].rearrange("p (t e) -> p t e", t=4)
                oh2 = A4[:, 368:400].rearrange("p (t e) -> p t e", t=4)
                goh = A4[:, 400:416].rearrange("p (t g) -> p t g", t=4)
                gd = A4[:, 416:432].rearrange("p (t g) -> p t g", t=4)
                mi = A4[:, 432:512]
                gmax, gsum, gw, dm, dd, w1, w2 = (mi[:, 4 * i:4 * i + 4] for i in range(7))

                def bc3(ap, n):
                    return ap.unsqueeze(2).broadcast_to([128, 4, n])
                b.reduce(gmax, gl, ALU.max, [t4_], [t4_])
                b.tt(goh, gl, bc3(gmax, 4), ALU.is_equal, [t4_], [t4_])
                b.tt(gd, gl, bc3(gmax, 4), ALU.subtract, [t4_], [t4_])
                b.act(gd, gd, AF.Exp, [t4_], [t4_])
                b.reduce(gsum, gd, ALU.add, [t4_], [t4_])
                b.recip(gw, gsum, [t4_], [t4_])
                b.tt(tmp, el, goh.unsqueeze(3).broadcast_to([128, 4, 4, 8]), ALU.mult, [t4_], [t4_])
                b.reduce(esel, tmp.rearrange("p t g e -> p t e g"), ALU.add, [t4_], [t4_])
                for tt in range(4):
                    def fmax(e, o=top8[:, tt, :], i_=esel[:, tt, :]):
                        return e.max(out=o, in_=i_)
                    S.op("dve", fmax, [t4_], [t4_])
                b.tt(oh1, esel, bc3(top8[:, :, 0], 8), ALU.is_equal, [t4_], [t4_])
                b.tt(oh2, esel, bc3(top8[:, :, 1], 8), ALU.is_equal, [t4_], [t4_])
                b.tt(dm, top8[:, :, 1], top8[:, :, 0], ALU.subtract, [t4_], [t4_])
                b.act(dd, dm, AF.Exp, [t4_], [t4_])
                b.ts(dm, dd, 1.0, None, ALU.add, None, [t4_], [t4_])
                b.recip(dm, dm, [t4_], [t4_])
                b.tt(w1, dm, gw, ALU.mult, [t4_], [t4_])
                b.tt(w2, w1, dd, ALU.mult, [t4_], [t4_])
                M1 = A2[:, 0:128].rearrange("p (t g e) -> p t g e", t=4, g=4)
                M2 = A2[:, 128:256].rearrange("p (t g e) -> p t g e", t=4, g=4)
                gohb = goh.unsqueeze(3).broadcast_to([128, 4, 4, 8])
                b.tt(M1, gohb, oh1.unsqueeze(2).broadcast_to([128, 4, 4, 8]), ALU.mult, [t4_], [t2_])
                b.tt(M2, gohb, oh2.unsqueeze(2).broadcast_to([128, 4, 4, 8]), ALU.mult, [t4_], [t2_])
                Mb = ar(3).bitcast(BF16)[:, 0:128].rearrange("p (t e) -> p t e", t=4)
                b.tt(Mb, A2[:, 0:128].rearrange("p (t e) -> p t e", t=4), A2[:, 128:256].rearrange("p (t e) -> p t e", t=4),
                     ALU.add, [t2_], [t3_])
                pp, tpp = ps[4], tP[4]
                groups = []
                for tt in range(4):
                    ops = [(lstrict[:], Mb[:, tt, :]), (ones_bf[:], macc[:])]
                    for t_ in range(tt):
                        ops.append((ones_bf[:], Mb[:, t_, :]))
                    groups.append((pp[:, tt * 32:(tt + 1) * 32], ops))
                b.mm(groups, [t3_, tC, tC2, tMacc], [tpp])
                for tt in range(4):
                    b.tt(macc[:], macc[:], Mb[:, tt, :], ALU.add, [t3_, tMacc], [tMacc], eng="pool")
                pos = A2[:, 256:384].rearrange("p (t e) -> p t e", t=4)
                tm = A2[:, 384:512].rearrange("p (t e) -> p t e", t=4)
                b.copy("dve", pos, pp[:, 0:128].rearrange("p (t e) -> p t e", t=4), [tpp], [t2_])
                s2 = small
                for kk in range(2):
                    Mk = A2[:, kk * 128:(kk + 1) * 128].rearrange("p (t e) -> p t e", t=4)
                    pk, ek, okf, dk = (s2[:, 16 + 16 * kk + 4 * i:20 + 16 * kk + 4 * i] for i in range(4))
                    b.tt(tm, pos, Mk, ALU.mult, [t2_], [t2_])
                    b.reduce(pk, tm, ALU.add, [t2_], [tSm])
                    b.tt(tm, ebase[:].unsqueeze(1).broadcast_to([128, 4, 32]), Mk, ALU.mult, [t2_, tC], [t2_])
                    b.reduce(ek, tm, ALU.add, [t2_], [tSm])
                    b.ts(okf, pk, float(CAP), None, ALU.is_lt, None, [tSm], [tSm])
                    b.ts(dk, okf, -1.0e6, 1.0e6, ALU.mult, ALU.add, [tSm], [tSm])
                    b.tt(dk, dk, pk, ALU.add, [tSm], [tSm])
                    b.tt(dk, dk, ek, ALU.add, [tSm], [tSm])
                    tD = [tDest[grp * 4 + t_] for t_ in range(4)]
                    b.copy("dve", dest_i[:, grp * 4:(grp + 1) * 4, kk], dk, [tSm], tD)
                    b.tt(wts[:, grp * 4:(grp + 1) * 4, kk], (w1 if kk == 0 else w2), okf, ALU.mult, [tSm, t4_], tD)
                for tt in range(4):
                    ti = grp * 4 + tt
                    for kk in range(2):
                        def fsc(g_, ti=ti, kk=kk, tt=tt):
                            return g_.indirect_dma_start(
                                out=Xg, out_offset=bass.IndirectOffsetOnAxis(ap=dest_i[:, ti, kk:kk + 1], axis=0),
                                in_=h2b[:, tt, :], in_offset=None, bounds_check=S.breg, oob_is_err=False)
                        S.dma("pool", fsc, [tSgA, tDest[ti]], [tXg], merge=True)

            plan = []
            for pg in range(NG):
                base = len(wlist)
                if pg == NG - 1:
                    wq_win(O_KA); wq_win(O_VA)
                for blk in range(4):
                    wq_win(O_FH + blk * 256)
                for blk in range(4):
                    wq_win(O_IH + blk * 256)
                plan.append(base)
            for og in range(NG):
                base = len(wlist)
                for blk in range(4):
                    wq_win(O_QA + blk * 256)
                wq_win(O_KA); wq_win(O_VA)
                for sec in (O_QH, O_FH, O_IH, O_GH):
                    for blk in range(4):
                        wq_win(sec + blk * 256)
                for sec in (O_GA, O_GB):
                    for blk in range(8):
                        wq_win(sec + blk * 256)
                for nb in range(4):
                    wlist.append((w_a_v[:, :, nb * 512:(nb + 1) * 512], 8, 512))
                    wlist.append((w_r_v[:, :, nb * 512:(nb + 1) * 512], 8, 512))
                for nb in range(8):
                    wlist.append((w_o_v[:, :, nb * 256:(nb + 1) * 256], 16, 256))
                plan.append(base)

            S.mark("init")
            for pg in range(NG - DBG_NGP, NG):
                stage_norm(x_pre, pg)
                S.mark("pnorm%d" % pg)
                stage_proj(False, pg == NG - 1, plan[pg])
                S.mark("pproj%d" % pg)
                stage_hloop(False)
                S.mark("phloop%d" % pg)
            b.copy("pool", kbuf[:, :, 0:128], kbuf[:, :, G:G + 128], [tKb], [tKb])
            b.copy("pool", vbuf[:, 0], vbuf[:, 4], [tVb], [tVb])
            for og in range(DBG_NGO):
                stage_norm(x_own, og)
                S.mark("norm%d" % og)
                wi = stage_proj(True, False, plan[NG + og])
                S.mark("proj%d" % og)
                stage_attn(og == 0)
                S.mark("attn%d" % og)
                stage_hloop(True)
                S.mark("hloop%d" % og)
                wi = stage_branch(wi)
                S.mark("branch%d" % og)
                wi = stage_out(wi, og)
                S.mark("out%d" % og)
                stage_route(og)
                S.mark("route%d" % og)
            S.mark("phase1")
            S.barrier()
            S.emit(final=(dbg == 2))
            if dbg == 2:
                return nc

        with ExitStack() as e2:
            wg = [sb(e2, "wg%d" % i, [128, KC, 512], BF16) for i in range(2)]
            wu = [sb(e2, "wu%d" % i, [128, KC, 512], BF16) for i in range(2)]
            wd = [sb(e2, "wd%d" % i, [128, 4, D], BF16) for i in range(2)]
            tWg = [T("wg%d" % i) for i in range(2)]
            tWu = [T("wu%d" % i) for i in range(2)]
            tWd = [T("wd%d" % i) for i in range(2)]
            xg_t = [sb(e2, "xg%d" % i, [128, D], BF16) for i in range(2)]
            tXgt = [T("xgt%d" % i) for i in range(2)]
            XgT = sb(e2, "XgT", [128, KC, CAP], BF16)
            tXgT = T("XgT")
            hTe = sb(e2, "hTe", [128, 4, CAP], BF16)
            tHe = T("hTe")
            sgt = [sb(e2, "sgt%d" % i, [128, CAP], F32) for i in range(2)]
            tSg = [T("sgt%d" % i) for i in range(2)]
            ysb = [sb(e2, "ysb%d" % i, [128, D], F32) for i in range(2)]
            tYs = [T("ysb%d" % i) for i in range(2)]

            def wload(e):
                i = e % 2
                b.dma("pool", wg[i][:], w_gate[e].rearrange("(kc p) n -> p kc n", p=128), (), [tWg[i]])
                b.dma("pool", wu[i][:], w_up[e].rearrange("(kc p) n -> p kc n", p=128), (), [tWu[i]])
                b.dma("pool", wd[i][:], w_down[e].rearrange("(fc p) n -> p fc n", p=128), (), [tWd[i]])

            wload(0)
            for e in range(DBG_NEX):
                i = e % 2
                if e + 1 < DBG_NEX:
                    wload(e + 1)
                for st in range(2):
                    r0 = e * CAP + st * 128
                    b.dma("sp", xg_t[st][:], Xg[r0:r0 + 128, :], [tXg], [tXgt[st]])
                    for rd in range(2):
                        p, tp = pb[rd], tPb[rd]
                        b.tr([(p[:, j, :], xg_t[st][:, (rd * 8 + j) * 128:(rd * 8 + j + 1) * 128], ident_bf[:])
                              for j in range(8)], [tXgt[st], tC], [tp])
                        b.evac(XgT[:, rd * 8:rd * 8 + 8, st * 128:(st + 1) * 128], p[:], [tp], [tXgT])
                for fc in range(4):
                    p, tp = ps[fc % 2], tP[fc % 2]
                    b.mm([(p[:, 0:CAP], [(wg[i][:, kc, fc * 128:(fc + 1) * 128], XgT[:, kc, :]) for kc in range(KC)]),
                          (p[:, CAP:2 * CAP], [(wu[i][:, kc, fc * 128:(fc + 1) * 128], XgT[:, kc, :]) for kc in range(KC)])],
                         [tWg[i], tWu[i], tXgT], [tp])
                    b.act(sgt[fc % 2][:], p[:, 0:CAP], AF.Silu, [tp], [tSg[fc % 2]])
                    b.tt(hTe[:, fc, :], sgt[fc % 2][:], p[:, CAP:2 * CAP], ALU.mult, [tSg[fc % 2], tp], [tHe])
                for st in range(2):
                    for nb in range(4):
                        p, tp = ps[2 + nb % 2], tP[2 + nb % 2]
                        b.mm([(p[:], [(hTe[:, fc, st * 128:(st + 1) * 128], wd[i][:, fc, nb * 512:(nb + 1) * 512])
                                      for fc in range(4)])], [tHe, tWd[i]], [tp])
                        b.evac(ysb[st][:, nb * 512:(nb + 1) * 512], p[:], [tp], [tYs[st]])
                    r0 = e * CAP + st * 128
                    b.dma("sp", Yg[r0:r0 + 128, :], ysb[st][:], [tYs[st]], [tYg], merge=True)
            S.barrier()
            S.emit()

        with ExitStack() as e3:
            gf = sb(e3, "gf", [128, D], F32)
            tGf = T("gf")
            b.dma("sp", gf[:], gf_bc, (), [tGf])
            xm_t = [sb(e3, "xm_t%d" % i, [128, D], F32) for i in range(3)]
            y1_t = [sb(e3, "y1_t%d" % i, [128, D], F32) for i in range(3)]
            y2_t = [sb(e3, "y2_t%d" % i, [128, D], F32) for i in range(3)]
            o_t = [sb(e3, "o_t%d" % i, [128, D], F32) for i in range(3)]
            jk = sb(e3, "jk", [128, D], BF16)
            sm3 = sb(e3, "sm3", [128, 8], F32)
            tXt = [T("xm_t%d" % i) for i in range(3)]
            tY1 = [T("y1_%d" % i) for i in range(3)]
            tY2 = [T("y2_%d" % i) for i in range(3)]
            tO = [T("o_%d" % i) for i in range(3)]
            tJ = T("jk")
            tS3 = T("sm3")
            for i in range(3):
                b.memset("pool", y1_t[i][:], 0.0, [tY1[i]])
                b.memset("pool", y2_t[i][:], 0.0, [tY2[i]])
            def xmload(ti):
                b.dma("sp", xm_t[ti % 3][:], xmid[ti * 128:(ti + 1) * 128, :], [tXm[ti]], [tXt[ti % 3]])
            for ti in range(min(2, DBG_NT3)):
                xmload(ti)
            for ti in range(DBG_NT3):
                i = ti % 3
                r0 = ti * 128
                if ti + 2 < DBG_NT3:
                    xmload(ti + 2)
                for kk, (yt, ty) in enumerate(((y1_t[i], tY1[i]), (y2_t[i], tY2[i]))):
                    def fga(g_, yt=yt, ti=ti, kk=kk):
                        return g_.indirect_dma_start(
                            out=yt[:, :], out_offset=None, in_=Yg,
                            in_offset=bass.IndirectOffsetOnAxis(ap=dest_i[:, ti, kk:kk + 1], axis=0),
                            bounds_check=S.breg, oob_is_err=False)
                    S.dma("pool", fga, [tYg, tDest[ti]], [ty])
                b.stt(xm_t[i][:], y1_t[i][:], wts[:, ti, 0:1], xm_t[i][:], ALU.mult, ALU.add,
                      [tY1[i], tDest[ti], tXt[i]], [tXt[i]])
                b.stt(xm_t[i][:], y2_t[i][:], wts[:, ti, 1:2], xm_t[i][:], ALU.mult, ALU.add,
                      [tY2[i], tDest[ti], tXt[i]], [tXt[i]])
                b.act(jk[:], xm_t[i][:], AF.Square, [tXt[i]], [tJ, tS3], accum_out=sm3[:, 0:1])
                b.ts(sm3[:, 1:2], sm3[:, 0:1], 1.0 / D, EPS, ALU.mult, ALU.add, [tS3], [tS3])
                b.act(sm3[:, 1:2], sm3[:, 1:2], AF.Ln, [tS3], [tS3])
                b.act(sm3[:, 2:3], sm3[:, 1:2], AF.Exp, [tS3], [tS3], scale=-0.5)
                b.stt(o_t[i][:], xm_t[i][:], sm3[:, 2:3], gf[:], ALU.mult, ALU.mult, [tXt[i], tS3, tGf], [tO[i]])
                b.dma("sp", out_d[r0:r0 + 128, :], o_t[i][:], [tO[i]], [])
            S.mark("phase3")
            S.barrier()
            S.emit(final=True)
            DBG_MARKS[:] = S.marks
    return nc


def _t5_bucket(n):
    import math
    max_exact = 16
    nf = np.maximum(n, 1).astype(np.float32)
    large = max_exact + (np.log(nf / max_exact) / math.log(128 / max_exact) * (32 - max_exact)).astype(np.int32)
    large = np.minimum(large, 31)
    return np.where(n < max_exact, n, large)


_NC_CACHE = {}


def _consts():
    ident = np.eye(128, dtype=np.float32)
    s_ = np.arange(128)[:, None] % 64
    t_ = np.arange(64)[None, :]
    triu = (s_ <= t_).astype(np.float32)
    lstrict = (np.arange(128)[:, None] < np.arange(128)[None, :]).astype(np.float32)
    scanmask = np.ones((128, 512), np.float32)
    scanmask[:, ::64] = 0.0
    ebase = np.broadcast_to((np.arange(32) * CAP).astype(np.float32)[None, :], (128, 32)).copy()
    return dict(c_ident=ident, c_triu=triu, c_lstrict=lstrict, c_scanmask=scanmask, c_ebase=ebase)


def make_in_maps(inputs, ncores=8):
    f = lambda a: np.ascontiguousarray(np.asarray(a, dtype=np.float32))
    x = f(inputs["x"])
    bc = lambda v: np.ascontiguousarray(np.broadcast_to(f(v).reshape(1, -1), (128, f(v).size)))
    rel = f(inputs["rel_bias"])
    k_ = np.arange(128)[:, None]
    j_ = np.arange(256)[None, :]
    dist = j_ - k_
    bucket = _t5_bucket(np.clip(dist, 0, None))
    valid = (dist >= 0) & (dist < 128)
    tbl = rel[bucket]
    tbl = np.where(valid[:, :, None], tbl, np.float32(NEG)).astype(np.float32)
    biasT = np.ascontiguousarray(tbl.transpose(0, 2, 1))
    lbl = np.ascontiguousarray(f(inputs["hg_lb_logits"]).reshape(2, 8, 128).transpose(2, 1, 0))
    wr = np.ascontiguousarray(np.concatenate([f(inputs["w_group_router"][0]), f(inputs["w_expert_router"][0])], axis=1))
    bb = np.concatenate([f(inputs["b_group_router"][0]), f(inputs["b_expert_router"][0])])
    shared = dict(
        w_in=f(inputs["w_in"][0]), w_a=f(inputs["w_attn_branch"][0]), w_r=f(inputs["w_hg_branch"][0]),
        w_o=f(inputs["w_out"][0]), w_gate=f(inputs["w_gate"][0]), w_up=f(inputs["w_up"][0]),
        w_down=f(inputs["w_down"][0]), g1_bc=bc(inputs["norm1_g"][0]), g2_bc=bc(inputs["norm2_g"][0]),
        gf_bc=bc(inputs["final_g"]), gn_bc=bc(np.tile(f(inputs["hg_norm_g"][0]), 8)), biasT=biasT,
        sinks_bc=bc(inputs["attn_sinks"][0]), lbl=lbl, wr=wr, b_bc=bc(bb), **_consts())
    maps = []
    for c in range(ncores):
        bi, hf = c // 2, c % 2
        m = dict(shared)
        m["x_own"] = np.ascontiguousarray(x[bi, hf * TOK:(hf + 1) * TOK])
        m["x_pre"] = np.ascontiguousarray(x[bi, 0:TOK]) if hf == 1 else np.zeros((TOK, D), np.float32)
        m["flag0"] = np.full((128, 1), float(hf), np.float32)
        maps.append(m)
    return maps


def kernel(**inputs):
    if "nc" not in _NC_CACHE:
        _NC_CACHE["nc"] = build_program()
    nc = _NC_CACHE["nc"]
    maps = make_in_maps(inputs, 8)
    res = run_bass_kernel_spmd(nc, maps, core_ids=list(range(8)))
    out = np.empty((4, 4096, D), np.float32)
    for c in range(8):
        out[c // 2, (c % 2) * TOK:(c % 2 + 1) * TOK] = res.results[c]["out"]
    return out
```
